# Optimizing a Trainium2 kernel written in Bass

```python
import math
import jax, jax.numpy as jnp
from jax import lax
import numpy as np

D_MODEL = 2048
BATCH = 1
SEQ = 8192
DEPTH = 1

HEAD_DIM = 128
A_HEADS = 8
DIL_PATTERNS = ((128, 1), (512, 4), (2048, 16))
B_Q_HEADS = 8
B_KV_HEADS = 2
GRID_W = 64
ROPE_THETA = 10000.0
Q_BLOCK = 128
NORM_EPS = 1e-6

PEER_HEADS = 8
PEER_N_KEYS = 128
PEER_N_EXPERTS = PEER_N_KEYS * PEER_N_KEYS
PEER_QUERY_DIM = 256
PEER_TOPK = 16
TOKEN_BLOCK = 128

A_WIDTH = A_HEADS * HEAD_DIM
B_Q_WIDTH = B_Q_HEADS * HEAD_DIM
B_KV_WIDTH = B_KV_HEADS * HEAD_DIM
MIX_WIDTH = A_WIDTH + B_Q_WIDTH
IN_COLS = 3 * A_WIDTH + B_Q_WIDTH + 2 * B_KV_WIDTH

kernel_name = "hybrid_dilated_gqa_peer_encoder"


def rms_norm(x, g):
    xf = x.astype(jnp.float32)
    y = xf * lax.rsqrt(jnp.mean(xf * xf, axis=-1, keepdims=True) + NORM_EPS)
    return (y * g.astype(jnp.float32)).astype(x.dtype)


def dilated_offsets():
    rows = []
    for w, d in DIL_PATTERNS:
        n_side = (w // 2) // d
        rows.append(np.arange(-n_side, n_side + 1, dtype=np.int32) * d)
    return np.stack(rows, axis=0)


def alibi_slopes(n_heads):
    return 2.0 ** (-8.0 * (jnp.arange(n_heads, dtype=jnp.float32) + 1.0) / n_heads)


def dilated_window_attention(q, k, v):
    b, h, s, hd = q.shape
    offs_np = dilated_offsets()
    n_p, n_k = offs_np.shape
    offs = jnp.asarray(offs_np)
    bias = -alibi_slopes(h)[:, None, None] * jnp.abs(offs).astype(jnp.float32)[None]
    scale = HEAD_DIM ** -0.5
    nb = s // Q_BLOCK
    qb = q.reshape(b, h, nb, Q_BLOCK, hd).transpose(2, 0, 1, 3, 4)
    starts = jnp.arange(nb, dtype=jnp.int32) * Q_BLOCK

    def block(args):
        qblk, start = args
        t = start + jnp.arange(Q_BLOCK, dtype=jnp.int32)
        idx = t[:, None, None] + offs[None]
        valid = (idx >= 0) & (idx < s)
        idx_c = jnp.clip(idx, 0, s - 1).reshape(-1)
        kg = jnp.take(k, idx_c, axis=2).reshape(b, h, Q_BLOCK, n_p, n_k, hd)
        vg = jnp.take(v, idx_c, axis=2).reshape(b, h, Q_BLOCK, n_p, n_k, hd)
        sc = jnp.einsum('bhqd,bhqpkd->bhqpk', qblk, kg).astype(jnp.float32) * scale
        sc = sc + bias[None, :, None]
        sc = jnp.where(valid[None, None], sc, -jnp.inf)
        lse = jax.nn.logsumexp(sc, axis=-1)
        p = jnp.exp(sc - lse[..., None])
        o = jnp.einsum('bhqpk,bhqpkd->bhqpd', p.astype(v.dtype), vg)
        w = jax.nn.softmax(lse, axis=-1)
        return jnp.einsum('bhqp,bhqpd->bhqd', w.astype(v.dtype), o)

    out = lax.map(block, (qb, starts))
    return out.transpose(1, 2, 0, 3, 4).reshape(b, h, s, hd)


def axial_rope(s):
    rows = s // GRID_W
    row = jnp.repeat(jnp.arange(rows, dtype=jnp.float32), GRID_W)
    col = jnp.tile(jnp.arange(GRID_W, dtype=jnp.float32), rows)
    half = HEAD_DIM // 2
    inv = ROPE_THETA ** (-jnp.arange(0, half, 2, dtype=jnp.float32) / half)
    ang = jnp.concatenate([row[:, None] * inv, col[:, None] * inv], axis=-1)
    return jnp.cos(ang), jnp.sin(ang)


def apply_rope(x, cos, sin):
    xf = x.astype(jnp.float32).reshape(*x.shape[:-1], HEAD_DIM // 2, 2)
    x0, x1 = xf[..., 0], xf[..., 1]
    c = cos[None, :, None, :]
    sn = sin[None, :, None, :]
    out = jnp.stack([x0 * c - x1 * sn, x0 * sn + x1 * c], axis=-1)
    return out.reshape(x.shape).astype(x.dtype)


def gqa_block_attention(q, k, v):
    b, hq, s, hd = q.shape
    hkv = k.shape[1]
    g = hq // hkv
    scale = HEAD_DIM ** -0.5
    nb = s // Q_BLOCK
    qb = q.reshape(b, hkv, g, nb, Q_BLOCK, hd).transpose(3, 0, 1, 2, 4, 5)

    def block(qblk):
        sc = jnp.einsum('bkgqd,bksd->bkgqs', qblk, k).astype(jnp.float32) * scale
        p = jax.nn.softmax(sc, axis=-1)
        return jnp.einsum('bkgqs,bksd->bkgqd', p.astype(v.dtype), v)

    out = lax.map(block, qb)
    return out.transpose(1, 2, 3, 0, 4, 5).reshape(b, hq, s, hd)


def peer_ffn(x, w_query, sub_keys, expert_u, expert_v):
    b, s, d = x.shape
    n_tok = b * s
    xt = x.reshape(n_tok, d)
    q = (xt @ w_query).reshape(n_tok, PEER_HEADS, 2, PEER_QUERY_DIM // 2)
    scores = jnp.einsum('thcd,hcnd->thcn', q, sub_keys).astype(jnp.float32)
    s_top, i_top = lax.top_k(scores, PEER_TOPK)
    cand = s_top[:, :, 0, :, None] + s_top[:, :, 1, None, :]
    cand_idx = i_top[:, :, 0, :, None] * PEER_N_KEYS + i_top[:, :, 1, None, :]
    best, pos = lax.top_k(cand.reshape(n_tok, PEER_HEADS, PEER_TOPK * PEER_TOPK), PEER_TOPK)
    experts = jnp.take_along_axis(cand_idx.reshape(n_tok, PEER_HEADS, -1), pos, axis=-1)
    gates = jax.nn.softmax(best, axis=-1).astype(x.dtype)
    nb = n_tok // TOKEN_BLOCK

    def block(args):
        xb, eb, gb = args
        u = jnp.take(expert_u, eb, axis=0)
        vv = jnp.take(expert_v, eb, axis=0)
        act = jax.nn.gelu(jnp.einsum('td,thkd->thk', xb, u))
        return jnp.einsum('thk,thkd->td', act * gb, vv)

    out = lax.map(block, (xt.reshape(nb, TOKEN_BLOCK, d),
                          experts.reshape(nb, TOKEN_BLOCK, PEER_HEADS, PEER_TOPK),
                          gates.reshape(nb, TOKEN_BLOCK, PEER_HEADS, PEER_TOPK)))
    return out.reshape(b, s, d)


def setup_inputs(seed: int = 0) -> dict:
    key = jax.random.key(seed)
    ks = jax.random.split(key, 12)
    f32 = jnp.float32
    x = jax.random.normal(ks[0], (BATCH, SEQ, D_MODEL), f32)
    norm1_g = 1.0 + 0.02 * jax.random.normal(ks[1], (DEPTH, D_MODEL), f32)
    w_in = jax.random.normal(ks[2], (DEPTH, D_MODEL, IN_COLS), f32) * D_MODEL ** -0.5
    q_norm_g = 1.0 + 0.02 * jax.random.normal(ks[3], (DEPTH, HEAD_DIM), f32)
    k_norm_g = 1.0 + 0.02 * jax.random.normal(ks[4], (DEPTH, HEAD_DIM), f32)
    w_out = jax.random.normal(ks[5], (DEPTH, MIX_WIDTH, D_MODEL), f32) * MIX_WIDTH ** -0.5
    norm2_g = 1.0 + 0.02 * jax.random.normal(ks[6], (DEPTH, D_MODEL), f32)
    peer_w_query = jax.random.normal(ks[7], (DEPTH, D_MODEL, PEER_HEADS * PEER_QUERY_DIM), f32) * D_MODEL ** -0.5
    peer_sub_keys = jax.random.normal(ks[8], (DEPTH, PEER_HEADS, 2, PEER_N_KEYS, PEER_QUERY_DIM // 2), f32) * (PEER_QUERY_DIM // 2) ** -0.5
    peer_u = jax.random.normal(ks[9], (DEPTH, PEER_N_EXPERTS, D_MODEL), f32) * D_MODEL ** -0.5
    peer_v = jax.random.normal(ks[10], (DEPTH, PEER_N_EXPERTS, D_MODEL), f32) * PEER_HEADS ** -0.5
    final_norm_g = 1.0 + 0.02 * jax.random.normal(ks[11], (D_MODEL,), f32)
    return {"x": x, "norm1_g": norm1_g, "w_in": w_in, "q_norm_g": q_norm_g,
            "k_norm_g": k_norm_g, "w_out": w_out, "norm2_g": norm2_g,
            "peer_w_query": peer_w_query, "peer_sub_keys": peer_sub_keys,
            "peer_u": peer_u, "peer_v": peer_v, "final_norm_g": final_norm_g}


def reference(x, norm1_g, w_in, q_norm_g, k_norm_g, w_out, norm2_g,
              peer_w_query, peer_sub_keys, peer_u, peer_v, final_norm_g):
    b, s, _ = x.shape
    cos, sin = axial_rope(s)
    splits = [A_WIDTH, 2 * A_WIDTH, 3 * A_WIDTH, 3 * A_WIDTH + B_Q_WIDTH,
              3 * A_WIDTH + B_Q_WIDTH + B_KV_WIDTH]
    for l in range(DEPTH):
        h = rms_norm(x, norm1_g[l])
        proj = h @ w_in[l]
        qa, ka, va, qb, kb, vb = jnp.split(proj, splits, axis=-1)
        to_heads = lambda t, n: t.reshape(b, s, n, HEAD_DIM)
        qa_, ka_, va_ = [to_heads(t, A_HEADS).transpose(0, 2, 1, 3) for t in (qa, ka, va)]
        out_a = dilated_window_attention(qa_, ka_, va_)
        qb_ = apply_rope(rms_norm(to_heads(qb, B_Q_HEADS), q_norm_g[l]), cos, sin)
        kb_ = apply_rope(rms_norm(to_heads(kb, B_KV_HEADS), k_norm_g[l]), cos, sin)
        vb_ = to_heads(vb, B_KV_HEADS)
        out_b = gqa_block_attention(qb_.transpose(0, 2, 1, 3), kb_.transpose(0, 2, 1, 3),
                                    vb_.transpose(0, 2, 1, 3))
        mixed = jnp.concatenate([out_a.transpose(0, 2, 1, 3).reshape(b, s, A_WIDTH),
                                 out_b.transpose(0, 2, 1, 3).reshape(b, s, B_Q_WIDTH)], axis=-1)
        x = x + mixed @ w_out[l]
        x = x + peer_ffn(rms_norm(x, norm2_g[l]), peer_w_query[l], peer_sub_keys[l],
                         peer_u[l], peer_v[l])
    return rms_norm(x, final_norm_g)
```

```python
import math
from contextlib import ExitStack

import numpy as np
import ml_dtypes
import concourse.bass as bass
import concourse.mybir as mybir
from concourse.bass_utils import run_bass_kernel_spmd

F32 = mybir.dt.float32
BF16 = mybir.dt.bfloat16
U32 = mybir.dt.uint32
AF = mybir.ActivationFunctionType
ALU = mybir.AluOpType
AX = mybir.AxisListType

NCORES = 8
S = 8192
D = 2048
TOK = 1024
WIN = 3072
EPS = 1e-6
SCALE = 128.0 ** -0.5
ARENA = 51800
NEG = -1.0e30


class Fw:
    ENG = ("sp", "gp", "pe", "act", "dve")

    def __init__(self):
        self.ops = []

    def op(self, eng, fn, r=(), w=(), dma=False, key=None, extra=None):
        self.ops.append(dict(eng=eng, fn=fn, r=tuple(r), w=tuple(w), dma=dma, key=key,
                             extra=extra, deps=set(), signal=False))
        return len(self.ops) - 1

    def dma(self, eng, out, in_, r=(), w=(), key=None):
        k = key if key is not None else (w[0] if w else ("dma", len(self.ops)))
        return self.op(eng, lambda e, o=out, i=in_: e.dma_start(out=o, in_=i), r=r, w=w, dma=True, key=k)

    def barrier(self):
        last = {}
        for i, o in enumerate(self.ops):
            last[o["eng"]] = i
            if o["dma"]:
                last[("k", o["key"])] = i
        deps = set(last.values())
        for e in self.ENG:
            self.op(e, None, extra=set(deps))

    def analyze(self):
        lastw, readers = {}, {}
        for i, o in enumerate(self.ops):
            deps = set()
            if o["extra"]:
                deps |= o["extra"]
            for b in o["r"]:
                if b in lastw:
                    deps.add(lastw[b])
            for b in o["w"]:
                if b in lastw:
                    deps.add(lastw[b])
                deps.update(readers.get(b, ()))
            deps.discard(i)
            for b in o["r"]:
                readers.setdefault(b, []).append(i)
            for b in o["w"]:
                lastw[b] = i
                readers[b] = []
            real = set()
            for j in deps:
                p = self.ops[j]
                if p["fn"] is None:
                    continue
                if (not p["dma"]) and p["eng"] == "pe" and o["eng"] == "pe" and not o["dma"]:
                    continue
                real.add(j)
            o["deps"] = real
            for j in real:
                self.ops[j]["signal"] = True
        for o in self.ops:
            if o["dma"]:
                o["signal"] = True
        cnt = {}
        for o in self.ops:
            if o["fn"] is None or not o["signal"]:
                continue
            k = ("k", o["key"]) if o["dma"] else ("e", o["eng"])
            cnt[k] = cnt.get(k, 0) + (16 if o["dma"] else 1)
            o["sig"] = (k, cnt[k])
        self.final = dict(cnt)
        return sorted(cnt.keys(), key=str)

    def emit(self, nc, block, sems):
        engs = {"sp": block.sync, "gp": block.gpsimd, "pe": block.tensor, "act": block.scalar,
                "dve": block.vector}
        for en in self.ENG:
            mine = [o for o in self.ops if o["eng"] == en]
            final = self.final if en == "sp" else None

            def body(e, mine=mine, final=final):
                seen = {}
                for o in mine:
                    for j in sorted(o["deps"]):
                        k, v = self.ops[j]["sig"]
                        if seen.get(k, 0) >= v:
                            continue
                        seen[k] = v
                        e.wait_ge(sems[k], v)
                    if o["fn"] is None:
                        continue
                    ins = o["fn"](e)
                    if o["signal"]:
                        k, v = o["sig"]
                        ins.then_inc(sems[k], 16 if o["dma"] else 1)
                if final is not None:
                    for k, v in sorted(final.items(), key=str):
                        if k[0] == "k" and seen.get(k, 0) < v:
                            e.wait_ge(sems[k], v)

            engs[en](body)


def build_program():
    nc = bass.Bass("TRN2", target_bir_lowering=False)
    fw = Fw()

    def din(name, shape, dt=F32):
        return nc.dram_tensor(name, list(shape), dt, kind="ExternalInput").ap()

    xT = din("xT", [S // 256, 128, 16 * 256])
    x_own = din("x_own", [TOK, D])
    ropeT = din("ropeT", [128, 64, 128])
    valid_d = din("valid", [128, 24])
    maskA = din("maskA", [8, 128, 23 * 128], BF16)
    g1T_d = din("g1T", [128, 16])
    qg_d = din("qg_b", [128, 128])
    kg_d = din("kg_b", [128, 128])
    g2_d = din("g2_b", [128, D])
    gf_d = din("gf_b", [128, D])
    iota_d = din("iota", [128, 128])
    ident_d = din("ident", [128, 128])
    wA = din("wA", [8, D, 384])
    wqB = din("wqB", [D, 1024])
    wkvB = din("wkvB", [D, 512])
    w_out = din("w_out", [D, D])
    wq = din("wq", [D, D])
    subkT_d = din("subkT", [128, 16, 128])
    UT_l = din("UT_l", [128, 128, 16 * 128])
    V_l = din("V_l", [128, 128, D])
    out_d = nc.dram_tensor("out", [TOK, D], F32, kind="ExternalOutput").ap()
    Gd = nc.dram_tensor("Gd", [8, 128, 128, 128], BF16, kind="Internal").ap()
    x1_scr = nc.dram_tensor("x1_scr", [128, 8 * D], F32, kind="Internal").ap()

    arena = nc.alloc_sbuf_tensor("arena", [128, ARENA], F32)
    psA = nc.alloc_psum_tensor("psA", [128, 2048], F32)
    psB = nc.alloc_psum_tensor("psB", [128, 2048], F32)

    def bank(i):
        t = psA if i < 4 else psB
        j = i % 4
        return t[:, j * 512:(j + 1) * 512]

    def bank_bf(i):
        return bank(i).bitcast(BF16)

    class Ar:
        def __init__(self):
            self.p = 0

        def alloc(self, shape, dt=F32):
            n = int(np.prod(shape[1:]))
            slots = n if dt in (F32, U32) else (n + 1) // 2
            slots = (slots + 7) // 8 * 8
            off = self.p
            self.p += slots
            assert self.p <= ARENA, ("arena overflow", self.p)
            ap = arena[:, off:off + slots]
            if dt != F32:
                ap = ap.bitcast(dt)
            ap = ap[:, 0:n]
            if len(shape) == 3:
                ap = ap.rearrange("p (a b) -> p a b", a=shape[1])
            elif len(shape) == 4:
                ap = ap.rearrange("p (a b c) -> p a b c", a=shape[1], b=shape[2])
            return ap

    ar = Ar()

    ones_bf = ar.alloc([128, 128], BF16)
    ident_bf = ar.alloc([128, 128], BF16)
    g1T = ar.alloc([128, 16])
    validT = ar.alloc([128, 24])
    kg_b = ar.alloc([128, 128])
    qg_b = ar.alloc([128, 128])
    iota = ar.alloc([128, 128])
    fw.dma("gp", ident_bf, ident_d, w=["ident"])
    fw.dma("sp", g1T, g1T_d, w=["g1T"])
    fw.dma("sp", validT, valid_d, w=["valid"])
    fw.dma("sp", kg_b, kg_d, w=["kg"])
    fw.dma("sp", qg_b, qg_d, w=["qg"])
    fw.dma("sp", iota, iota_d, w=["iota"])
    fw.op("dve", lambda e: e.memset(ones_bf, 1.0), w=["ones"])
    MIX0 = ar.p
    mixA = ar.alloc([128, 8, TOK], BF16)
    QT_B = ar.alloc([128, 8, TOK], BF16)
    P0 = ar.p

    def rms_feature_major(xst, ntok, slot_id, sq, rt, rstd, dst_fn, psb, rid, wid):
        fw.op("act", lambda e: e.activation(out=sq, in_=xst, func=AF.Square), r=[slot_id], w=["sq"])

        def mm(e):
            ins = None
            for dk in range(16):
                ins = e.matmul(bank(psb)[:, 0:ntok], ones_bf, sq[:, dk, :], start=(dk == 0), stop=(dk == 15))
            return ins
        fw.op("pe", mm, r=["sq", "ones"], w=[("ps", psb)])
        fw.op("act", lambda e: e.activation(out=rt, in_=bank(psb)[:, 0:ntok], func=AF.Sqrt, bias=EPS,
                                            scale=1.0 / D), r=[("ps", psb)], w=["rt"])
        fw.op("dve", lambda e: e.reciprocal(rstd, rt), r=["rt"], w=["rstd"])

        def norm(e):
            ins = None
            for dk in range(16):
                ins = e.scalar_tensor_tensor(out=dst_fn(dk), in0=xst[:, dk, :], scalar=g1T[:, dk:dk + 1],
                                             in1=rstd, op0=ALU.mult, op1=ALU.mult)
            return ins
        fw.op("dve", norm, r=[slot_id, "rstd", "g1T"] + list(rid), w=list(wid))

    def qk_post(raw, nh, gain, gain_id, rope_ap, rope_id, tmp, ssq, rt2, rs2, kn, kr, tA, tB, psb, dst_fn, dst_ids, tag):
        n = nh * 128
        raw3 = raw.rearrange("p (h d) -> p h d", h=nh)
        tmp3 = tmp.rearrange("p (h d) -> p h d", h=nh)
        kn3 = kn.rearrange("p (h d) -> p h d", h=nh)
        fw.op("dve", lambda e: e.tensor_tensor(tmp, raw, raw, ALU.mult), r=[tag + "raw"], w=[tag + "tmp"])
        fw.op("dve", lambda e: e.tensor_reduce(ssq, tmp3, AX.X, ALU.add), r=[tag + "tmp"], w=[tag + "ssq"])
        fw.op("act", lambda e: e.activation(out=rt2, in_=ssq, func=AF.Sqrt, bias=EPS, scale=1.0 / 128),
              r=[tag + "ssq"], w=[tag + "rt2"])
        fw.op("dve", lambda e: e.reciprocal(rs2, rt2), r=[tag + "rt2"], w=[tag + "rs2"])
        fw.op("dve", lambda e: e.tensor_tensor(kn3, raw3, rs2.unsqueeze(2).to_broadcast([128, nh, 128]), ALU.mult),
              r=[tag + "raw", tag + "rs2"], w=[tag + "kn"])
        fw.op("dve", lambda e: e.tensor_tensor(kn3, kn3, gain.unsqueeze(1).to_broadcast([128, nh, 128]), ALU.mult),
              r=[tag + "kn", gain_id], w=[tag + "kn"])
        kn4 = kn.rearrange("p (h i two) -> p h i two", h=nh, two=2)
        kr4 = kr.rearrange("p (h i two) -> p h i two", h=nh, two=2)
        x0, x1 = kn4[:, :, :, 0], kn4[:, :, :, 1]
        cosb = rope_ap[:, 0:64].unsqueeze(1).to_broadcast([128, nh, 64])
        sinb = rope_ap[:, 64:128].unsqueeze(1).to_broadcast([128, nh, 64])
        tA3 = tA.rearrange("p (h i) -> p h i", h=nh)
        tB3 = tB.rearrange("p (h i) -> p h i", h=nh)
        fw.op("dve", lambda e: e.tensor_tensor(tA3, x0, cosb, ALU.mult), r=[tag + "kn", rope_id], w=[tag + "tA"])
        fw.op("dve", lambda e: e.tensor_tensor(tB3, x1, sinb, ALU.mult), r=[tag + "kn", rope_id], w=[tag + "tB"])
        fw.op("dve", lambda e: e.tensor_tensor(kr4[:, :, :, 0], tA3, tB3, ALU.subtract),
              r=[tag + "tA", tag + "tB"], w=[tag + "kr"])
        fw.op("dve", lambda e: e.tensor_tensor(tA3, x0, sinb, ALU.mult), r=[tag + "kn", rope_id], w=[tag + "tA"])
        fw.op("dve", lambda e: e.tensor_tensor(tB3, x1, cosb, ALU.mult), r=[tag + "kn", rope_id], w=[tag + "tB"])
        fw.op("dve", lambda e: e.tensor_tensor(kr4[:, :, :, 1], tA3, tB3, ALU.add),
              r=[tag + "tA", tag + "tB"], w=[tag + "kr"])
        for h0 in range(0, nh, 4):
            hn = min(4, nh - h0)

            def tr(e, h0=h0, hn=hn):
                ins = None
                for j in range(hn):
                    ins = e.transpose(bank_bf(psb)[:, j * 128:(j + 1) * 128], kr[:, (h0 + j) * 128:(h0 + j + 1) * 128],
                                      ident_bf)
                return ins
            fw.op("pe", tr, r=[tag + "kr", "ident"], w=[("ps", psb)])
            for j in range(hn):
                fw.op("act", lambda e, j=j, h0=h0: e.activation(out=dst_fn(h0 + j), in_=bank_bf(psb)[:, j * 128:(j + 1) * 128],
                                                                func=AF.Copy),
                      r=[("ps", psb)], w=[dst_ids[h0 + j]])

    def attention(KT_fn, V_fn, QT, kts, kid, vid, qid, mask_fn, mask_id, E2, P2, rec, dst, dst_id, pso, psd, tagc,
                  sbanks=(2, 3), hook=None):
        n = len(kts)
        nb = len(sbanks)
        L = nb - 1

        def qk(i):
            kt = kts[i]
            b = sbanks[i % nb]
            fw.op("pe", lambda e, kt=kt, b=b: e.matmul(bank(b), KT_fn(kt), QT, start=True, stop=True),
                  r=[kid, qid], w=[("ps", b)])

        for i in range(min(L, n)):
            qk(i)
        for i in range(n):
            kt = kts[i]
            b = sbanks[i % nb]
            if i + L < n:
                qk(i + L)
            Eb = E2[i % nb]
            fw.op("act", lambda e, b=b, Eb=Eb: e.activation(out=Eb, in_=bank(b), func=AF.Exp, scale=SCALE),
                  r=[("ps", b)], w=[("E", i % nb)])
            if mask_fn is not None:
                Pb = P2[i % nb]
                mk = mask_fn(i)
                fw.op("dve", lambda e, Pb=Pb, Eb=Eb, mk=mk, kt=kt: e.scalar_tensor_tensor(
                    out=Pb, in0=Eb, scalar=validT[:, kt:kt + 1], in1=mk, op0=ALU.mult, op1=ALU.mult),
                    r=[("E", i % nb), mask_id, "valid"], w=[("P", i % nb)])
                pid = ("P", i % nb)
            else:
                Pb = Eb
                pid = ("E", i % nb)

            def pv(e, kt=kt, Pb=Pb, i=i):
                e.matmul(bank(pso), V_fn(kt), Pb, start=(i == 0), stop=(i == n - 1))
                return e.matmul(bank(psd), ones_bf, Pb, start=(i == 0), stop=(i == n - 1))
            fw.op("pe", pv, r=[pid, vid, "ones"], w=[("ps", pso), ("ps", psd)])
            if hook is not None:
                hook(i)
        fw.op("dve", lambda e: e.reciprocal(rec, bank(psd)), r=[("ps", psd)], w=["rec" + tagc])
        fw.op("dve", lambda e: e.tensor_tensor(dst, bank(pso), rec, ALU.mult),
              r=[("ps", pso), "rec" + tagc], w=[dst_id])

    ar.p = P0
    hwin = ar.alloc([128, 16, WIN], BF16)
    PA = ar.p
    xst = [ar.alloc([128, 16, 256]) for _ in range(2)]
    sq = ar.alloc([128, 16, 256], BF16)
    rt = ar.alloc([128, 256])
    rstd = ar.alloc([128, 256])
    for ck in range(WIN // 256):
        s = ck % 2
        fw.dma("sp", xst[s].rearrange("p a b -> p (a b)"), xT[ck], w=[("xst", s)])
        rms_feature_major(xst[s], 256, ("xst", s), sq, rt, rstd,
                          lambda dk, ck=ck: hwin[:, dk, ck * 256:(ck + 1) * 256], 0, [], [("hwin", ck // 2)])
    fw.barrier()

    ar.p = PA
    WqB = ar.alloc([128, 16, 1024], BF16)
    ropeQ = ar.alloc([128, 8, 128])
    rawq = ar.alloc([128, 1024])
    tmpq = ar.alloc([128, 1024])
    knq = ar.alloc([128, 1024])
    krq = ar.alloc([128, 1024], BF16)
    tAq = ar.alloc([128, 512])
    tBq = ar.alloc([128, 512])
    ssq8 = ar.alloc([128, 8])
    rt8 = ar.alloc([128, 8])
    rs8 = ar.alloc([128, 8])
    fw.dma("gp", WqB, wqB.rearrange("(dk p) c -> p dk c", p=128), w=["WqB"])
    fw.dma("sp", ropeQ, ropeT[:, 8:16, :], w=["ropeQ"])
    win_ids = [("hwin", i) for i in range(6)]
    for tt in range(8):
        for half in range(2):
            def mm(e, tt=tt, half=half):
                ins = None
                for dk in range(16):
                    ins = e.matmul(bank(half), hwin[:, dk, 1024 + tt * 128:1024 + (tt + 1) * 128],
                                   WqB[:, dk, half * 512:(half + 1) * 512], start=(dk == 0), stop=(dk == 15))
                return ins
            fw.op("pe", mm, r=["WqB"] + win_ids, w=[("ps", half)])
            fw.op("act", lambda e, half=half: e.activation(out=rawq[:, half * 512:(half + 1) * 512], in_=bank(half),
                                                           func=AF.Copy), r=[("ps", half)], w=["qraw"])
        qk_post(rawq, 8, qg_b, "qg", ropeQ[:, tt, :], "ropeQ", tmpq, ssq8, rt8, rs8, knq, krq, tAq, tBq, 6,
                lambda h, tt=tt: QT_B[:, h, tt * 128:(tt + 1) * 128], [("QTB", h) for h in range(8)], "q")
    fw.barrier()

    ar.p = PA
    WA = [ar.alloc([128, 16, 384], BF16) for _ in range(2)]
    maskS = [ar.alloc([128, 23, 128], BF16) for _ in range(2)]
    KT_h = [ar.alloc([128, WIN], BF16) for _ in range(2)]
    V_h = [ar.alloc([128, 24, 128], BF16) for _ in range(2)]
    QT_h = [ar.alloc([128, TOK], BF16) for _ in range(2)]
    E2 = [ar.alloc([128, 512], BF16) for _ in range(2)]
    P2 = [ar.alloc([128, 512], BF16) for _ in range(2)]
    rec = ar.alloc([128, 512])

    def a2_loads(h):
        s = h % 2
        fw.dma("gp", WA[s], wA[h].rearrange("(dk p) c -> p dk c", p=128), w=[("WA", s)])
        fw.dma("sp", maskS[s].rearrange("p a b -> p (a b)"), maskA[h], w=[("mask", s)])

    def a2_proj_groups(h):
        s = h % 2
        pieces = []
        gcount = [0]

        def add_group(nmm, mk_mm, evac):
            per = 4 if nmm == 16 else 16
            pb = gcount[0] % 2
            gcount[0] += 1
            for p0 in range(0, nmm, per):
                last = (p0 + per >= nmm)

                def piece(p0=p0, pb=pb):
                    def mm(e):
                        ins = None
                        for q in range(p0, p0 + per):
                            ins = mk_mm(e, q, pb)
                        return ins
                    fw.op("pe", mm, r=[("WA", s)] + win_ids, w=[("ps", pb)])
                pieces.append((piece, (lambda pb=pb: evac(pb)) if last else None))

        for c6 in range(6):
            add_group(16,
                      lambda e, dk, pb, c6=c6: e.matmul(bank(pb), WA[s][:, dk, 128:256], hwin[:, dk, c6 * 512:(c6 + 1) * 512],
                                                        start=(dk == 0), stop=(dk == 15)),
                      lambda pb, c6=c6: fw.op("act", lambda e: e.activation(out=KT_h[s][:, c6 * 512:(c6 + 1) * 512], in_=bank(pb),
                                                                             func=AF.Copy), r=[("ps", pb)], w=[("KTh", s)]))
        for c2 in range(2):
            add_group(16,
                      lambda e, dk, pb, c2=c2: e.matmul(bank(pb), WA[s][:, dk, 0:128],
                                                        hwin[:, dk, 1024 + c2 * 512:1024 + (c2 + 1) * 512],
                                                        start=(dk == 0), stop=(dk == 15)),
                      lambda pb, c2=c2: fw.op("act", lambda e: e.activation(out=QT_h[s][:, c2 * 512:(c2 + 1) * 512], in_=bank(pb),
                                                                             func=AF.Copy), r=[("ps", pb)], w=[("QTh", s)]))
        for gg in range(6):
            add_group(64,
                      lambda e, q, pb, gg=gg: e.matmul(bank(pb)[:, (q // 16) * 128:(q // 16 + 1) * 128],
                                                       hwin[:, q % 16, (4 * gg + q // 16) * 128:(4 * gg + q // 16 + 1) * 128],
                                                       WA[s][:, q % 16, 256:384], start=(q % 16 == 0), stop=(q % 16 == 15)),
                      lambda pb, gg=gg: fw.op("dve", lambda e: e.tensor_copy(
                          V_h[s][:, 4 * gg:4 * gg + 4, :].rearrange("p a b -> p (a b)"), bank(pb)),
                          r=[("ps", pb)], w=[("Vh", s)]))
        return pieces

    a2_loads(0)
    for (pc, ev) in a2_proj_groups(0):
        pc()
        if ev is not None:
            ev()
    for h in range(8):
        s = h % 2
        if h + 1 < 8:
            a2_loads(h + 1)
            pend = a2_proj_groups(h + 1)
        else:
            pend = []
        cnt_t = [0]
        npend = len(pend)
        late = []

        def hook(i, pend=pend, cnt_t=cnt_t, npend=npend, late=late):
            cnt_t[0] += 1
            for (d, ev) in [x for x in late if x[0] <= cnt_t[0]]:
                ev()
            late[:] = [x for x in late if x[0] > cnt_t[0]]
            want = (npend * cnt_t[0] + 39) // 40
            while pend and (npend - len(pend)) < want:
                pc, ev = pend.pop(0)
                pc()
                if ev is not None:
                    late.append((cnt_t[0] + 2, ev))
        for qc in range(2):
            kts = list(range(4 * qc, 4 * qc + 20))
            qt0 = 8 + 4 * qc
            pso, psd = (4, 5) if qc == 0 else (6, 7)

            def mask_fn(i, kts=kts, qt0=qt0, s=s):
                m0 = 11 - (kts[i] - qt0)
                return maskS[s][:, m0:m0 + 4, :].rearrange("p a b -> p (a b)")
            attention(lambda kt, s=s: KT_h[s][:, kt * 128:(kt + 1) * 128], lambda kt, s=s: V_h[s][:, kt, :],
                      QT_h[s][:, qc * 512:(qc + 1) * 512], kts, ("KTh", s), ("Vh", s), ("QTh", s), mask_fn, ("mask", s),
                      E2, P2, rec, mixA[:, h, qc * 512:(qc + 1) * 512], ("mixT", h), pso, psd, "A",
                      sbanks=(2, 3), hook=hook)
        while pend:
            pc, ev = pend.pop(0)
            pc()
            if ev is not None:
                late.append((0, ev))
        for (d, ev) in late:
            ev()
        late[:] = []
    fw.barrier()

    ar.p = P0
    KT_B = ar.alloc([128, 2, S], BF16)
    V_B = ar.alloc([128, 64, 256], BF16)
    PB = ar.p
    Wst = ar.alloc([128, 16, 512])
    ar.p = PB
    xst = [ar.alloc([128, 16, 256]) for _ in range(2)]
    sq = ar.alloc([128, 16, 256], BF16)
    xb = [ar.alloc([128, 16, 256], BF16) for _ in range(2)]
    Wkv = ar.alloc([128, 16, 512], BF16)
    rope4 = ar.alloc([128, 4, 128])
    raw = [ar.alloc([128, 1024]) for _ in range(2)]
    kgt = ar.alloc([128, 1024])
    sqt = ar.alloc([128, 1024])
    tA = ar.alloc([128, 512])
    tB = ar.alloc([128, 512])
    ob = ar.alloc([128, 1024])
    kr = ar.alloc([128, 1024], BF16)
    rtk = [ar.alloc([128, 2]) for _ in range(2)]
    rsk = [ar.alloc([128, 2]) for _ in range(2)]
    ssq8 = ar.alloc([128, 8])
    rt8 = ar.alloc([128, 8])
    rs8 = ar.alloc([128, 8])
    tC = sqt[:, 0:512]
    tD = sqt[:, 512:1024]
    fw.dma("sp", Wst, wkvB.rearrange("(dk p) c -> p dk c", p=128), w=["Wst"])

    def foldw(e):
        ins = None
        for dk in range(16):
            ins = e.tensor_scalar(Wkv[:, dk, :], Wst[:, dk, :], g1T[:, dk:dk + 1], None, ALU.mult)
        return ins
    fw.op("dve", foldw, r=["Wst", "g1T"], w=["Wkv"])
    SSQB = (0, 6)
    sched = []
    step = [0]

    def run_due():
        due = [f for (d, f) in sched if d <= step[0]]
        rest = [(d, f) for (d, f) in sched if d > step[0]]
        sched[:] = rest
        for f in due:
            f()

    def b1_head(C, half):
        ck = 2 * C + half
        s = ck % 2
        fw.dma("sp", xst[s].rearrange("p a b -> p (a b)"), xT[ck], w=[("xst", s)] + (["Wst"] if ck < 2 else []))
        fw.op("act", lambda e, s=s: e.activation(out=sq, in_=xst[s], func=AF.Square), r=[("xst", s)], w=["sq"])
        fw.op("dve", lambda e, s=s: e.tensor_copy(xb[s], xst[s]), r=[("xst", s)], w=[("xb", s)])

        def mms(e, half=half):
            ins = None
            for sub in range(2):
                for dk in range(16):
                    ins = e.matmul(bank(SSQB[half])[:, sub:sub + 1], sq[:, dk, sub * 128:(sub + 1) * 128],
                                   ones_bf[:, 0:1], start=(dk == 0), stop=(dk == 15))
            return ins
        fw.op("pe", mms, r=["sq", "ones"], w=[("ps", SSQB[half])])
        for sub in range(2):
            bb = 1 + 2 * half + sub

            def mm(e, s=s, sub=sub, bb=bb):
                ins = None
                for dk in range(16):
                    ins = e.matmul(bank(bb), xb[s][:, dk, sub * 128:(sub + 1) * 128], Wkv[:, dk, :],
                                   start=(dk == 0), stop=(dk == 15))
                return ins
            fw.op("pe", mm, r=[("xb", s), "Wkv"], w=[("ps", bb)])

    def b1_tail(C, half):
        r_ = C % 2
        fw.op("act", lambda e, half=half: e.activation(out=rtk[half], in_=bank(SSQB[half])[:, 0:2], func=AF.Sqrt,
                                                       bias=EPS, scale=1.0 / D), r=[("ps", SSQB[half])], w=[("rtk", half)])
        fw.op("dve", lambda e, half=half: e.reciprocal(rsk[half], rtk[half]), r=[("rtk", half)], w=[("rsk", half)])
        for sub in range(2):
            j = 2 * half + sub
            bb = 1 + j
            fw.op("act", lambda e, bb=bb, C=C, j=j, half=half, sub=sub: e.activation(
                out=V_B[:, 4 * C + j, :], in_=bank(bb)[:, 256:512], func=AF.Copy, scale=rsk[half][:, sub:sub + 1]),
                r=[("ps", bb), ("rsk", half)], w=[("VB", C, j)])
            fw.op("act", lambda e, bb=bb, j=j, half=half, sub=sub, r_=r_: e.activation(
                out=raw[r_][:, j * 256:(j + 1) * 256], in_=bank(bb)[:, 0:256], func=AF.Copy,
                scale=rsk[half][:, sub:sub + 1]), r=[("ps", bb), ("rsk", half)], w=[("raw", r_, j)])

    def b1_postA(C):
        r_ = C % 2
        fw.dma("sp", rope4, ropeT[:, 4 * C:4 * C + 4, :], w=["rope4"])
        rw = raw[r_]
        raw_ids = [("raw", r_, j) for j in range(4)]
        rw3 = rw.rearrange("p (a d) -> p a d", a=8)
        kg3 = kgt.rearrange("p (a d) -> p a d", a=8)
        sq3 = sqt.rearrange("p (a d) -> p a d", a=8)
        kg5 = kgt.rearrange("p (j h i two) -> p j h i two", j=4, h=2, two=2)
        ob5 = ob.rearrange("p (j h i two) -> p j h i two", j=4, h=2, two=2)
        ob3 = ob.rearrange("p (a d) -> p a d", a=8)
        kr3 = kr.rearrange("p (a d) -> p a d", a=8)
        x0, x1 = kg5[:, :, :, :, 0], kg5[:, :, :, :, 1]
        cosb = rope4[:, :, 0:64].unsqueeze(2).to_broadcast([128, 4, 2, 64])
        sinb = rope4[:, :, 64:128].unsqueeze(2).to_broadcast([128, 4, 2, 64])
        v4 = lambda t: t.rearrange("p (j h i) -> p j h i", j=4, h=2)
        fw.op("dve", lambda e: e.tensor_tensor(kg3, rw3, kg_b.unsqueeze(1).to_broadcast([128, 8, 128]), ALU.mult),
              r=raw_ids + ["kg"], w=["kgt"])
        fw.op("dve", lambda e: e.tensor_tensor(sqt, rw, rw, ALU.mult), r=raw_ids, w=["sqt", "sqt2"])
        fw.op("dve", lambda e: e.tensor_tensor(v4(tA), x0, cosb, ALU.mult), r=["kgt", "rope4"], w=["tA"])
        fw.op("dve", lambda e: e.tensor_reduce(ssq8, sq3, AX.X, ALU.add), r=["sqt", "sqt2"], w=["ssq8"])
        fw.op("dve", lambda e: e.tensor_tensor(v4(tB), x1, sinb, ALU.mult), r=["kgt", "rope4"], w=["tB"])
        fw.op("dve", lambda e: e.tensor_tensor(v4(tC), x0, sinb, ALU.mult), r=["kgt", "rope4"], w=["sqt"])
        fw.op("dve", lambda e: e.tensor_tensor(v4(tD), x1, cosb, ALU.mult), r=["kgt", "rope4"], w=["sqt2"])
        fw.op("dve", lambda e: e.tensor_tensor(ob5[:, :, :, :, 0], v4(tA), v4(tB), ALU.subtract), r=["tA", "tB"], w=["ob0"])
        fw.op("dve", lambda e: e.tensor_tensor(ob5[:, :, :, :, 1], v4(tC), v4(tD), ALU.add), r=["sqt", "sqt2"], w=["ob1"])

    def b1_postB(C):
        ob3 = ob.rearrange("p (a d) -> p a d", a=8)
        kr3 = kr.rearrange("p (a d) -> p a d", a=8)
        fw.op("act", lambda e: e.activation(out=rt8, in_=ssq8, func=AF.Sqrt, bias=EPS, scale=1.0 / 128), r=["ssq8"], w=["rt8"])
        fw.op("dve", lambda e: e.reciprocal(rs8, rt8), r=["rt8"], w=["rs8"])
        fw.op("dve", lambda e: e.tensor_tensor(kr3, ob3, rs8.unsqueeze(2).to_broadcast([128, 8, 128]), ALU.mult),
              r=["ob0", "ob1", "rs8"], w=["kr"])
        def trk(e):
            ins = None
            for j in range(4):
                for hh in range(2):
                    ins = e.transpose(bank_bf(5)[:, (hh * 4 + j) * 128:(hh * 4 + j + 1) * 128],
                                      kr[:, (j * 2 + hh) * 128:(j * 2 + hh + 1) * 128], ident_bf)
            return ins
        fw.op("pe", trk, r=["kr", "ident"], w=[("ps", 5)])
        for hh in range(2):
            fw.op("dve", lambda e, hh=hh, C=C: e.tensor_copy(KT_B[:, hh, C * 512:(C + 1) * 512],
                                                             bank_bf(5)[:, hh * 512:(hh + 1) * 512]),
                  r=[("ps", 5)], w=[("KTB", C, hh)])

    for C in range(S // 512):
        for half in range(2):
            b1_head(C, half)
            run_due()
            sched.append((step[0] + 1, lambda C=C, half=half: b1_tail(C, half)))
            if half == 1:
                sched.append((step[0] + 1, lambda C=C: b1_postA(C)))
                sched.append((step[0] + 2, lambda C=C: b1_postB(C)))
            step[0] += 1
    step[0] += 10
    run_due()
    fw.barrier()

    ar.p = PB
    mixB = ar.alloc([128, 8, TOK], BF16)
    E2 = [ar.alloc([128, 512], BF16) for _ in range(4)]
    rec = ar.alloc([128, 512])
    cnt = 0
    for h in range(8):
        kv = h // 4
        for qc in range(2):
            pso, psd = (4, 5) if cnt % 2 == 0 else (6, 7)
            cnt += 1
            attention(lambda kt, kv=kv: KT_B[:, kv, kt * 128:(kt + 1) * 128],
                      lambda kt, kv=kv: V_B[:, kt, kv * 128:(kv + 1) * 128],
                      QT_B[:, h, qc * 512:(qc + 1) * 512], list(range(64)), "KTB", "VB", ("QTB", h), None, None,
                      E2, None, rec, mixB[:, h, qc * 512:(qc + 1) * 512], ("mixT", 8 + h), pso, psd, "B", sbanks=(0, 1, 2, 3))
    fw.barrier()

    ar.p = P0
    acc = ar.alloc([128, 8, D])
    assert ar.p <= PB
    PO = PB + 4096
    ar.p = PO
    Wo = [ar.alloc([128, 16, 512], BF16) for _ in range(2)]
    for tt in range(8):
        fw.dma("sp", acc[:, tt, :], x_own[tt * 128:(tt + 1) * 128, :], w=[("acc", tt)], key=("accld", tt))
    w_out3 = w_out.rearrange("(m p) c -> p m c", p=128)
    mix_ids = [("mixT", i) for i in range(16)]
    k = 0
    for oc in range(4):
        s = oc % 2
        fw.dma("gp", Wo[s], w_out3[:, :, oc * 512:(oc + 1) * 512], w=[("Wo", s)])
        for tt in range(8):
            b = k % 4
            k += 1

            def mm(e, tt=tt, s=s, b=b):
                ins = None
                for m in range(16):
                    ins = e.matmul(bank(b), (mixA[:, m, tt * 128:(tt + 1) * 128] if m < 8 else mixB[:, m - 8, tt * 128:(tt + 1) * 128]), Wo[s][:, m, :],
                                   start=(m == 0), stop=(m == 15))
                return ins
            fw.op("pe", mm, r=[("Wo", s)] + mix_ids, w=[("ps", b)])
            fw.op("dve", lambda e, tt=tt, oc=oc, b=b: e.tensor_tensor(
                acc[:, tt, oc * 512:(oc + 1) * 512], bank(b), acc[:, tt, oc * 512:(oc + 1) * 512], ALU.add),
                r=[("ps", b), ("acc", tt)], w=[("acc", tt)])
    fw.barrier()

    ar.p = MIX0
    h2T = ar.alloc([128, 16, TOK], BF16)
    PH = ar.p
    ar.p = PO
    g2_b = ar.alloc([128, D])
    junk = ar.alloc([128, D], BF16)
    h2 = [ar.alloc([128, D], BF16) for _ in range(2)]
    ssqn = ar.alloc([128, 8])
    rtn = ar.alloc([128, 8])
    rsn = ar.alloc([128, 8])
    fw.dma("sp", g2_b, g2_d, w=["g2"])
    acc_ids = [("acc", t_) for t_ in range(8)]
    fw.dma("sp", x1_scr, acc.rearrange("p a b -> p (a b)"), r=acc_ids, w=["x1scr"])
    fw.op("dve", lambda e: e.memset(ssqn, 0.0), w=["ssqn"])
    for tt in range(8):
        fw.op("act", lambda e, tt=tt: e.activation(out=junk, in_=acc[:, tt, :], func=AF.Square,
                                                   accum_out=ssqn[:, tt:tt + 1]), r=[("acc", tt), "ssqn"], w=["junk", ("ssqn", tt)])
        fw.op("act", lambda e, tt=tt: e.activation(out=rtn[:, tt:tt + 1], in_=ssqn[:, tt:tt + 1], func=AF.Sqrt,
                                                   bias=EPS, scale=1.0 / D), r=[("ssqn", tt)], w=[("rtn", tt)])
    for tt in range(8):
        fw.op("dve", lambda e, tt=tt: e.reciprocal(rsn[:, tt:tt + 1], rtn[:, tt:tt + 1]), r=[("rtn", tt)], w=[("rsn", tt)])
        s = tt % 2
        fw.op("dve", lambda e, tt=tt, s=s: e.scalar_tensor_tensor(out=h2[s], in0=acc[:, tt, :], scalar=rsn[:, tt:tt + 1],
                                                                  in1=g2_b, op0=ALU.mult, op1=ALU.mult),
              r=[("acc", tt), ("rsn", tt), "g2"], w=[("h2", s)])
        for g in range(4):
            b = g % 2

            def tr(e, g=g, s=s, b=b):
                ins = None
                for j in range(4):
                    dk = 4 * g + j
                    ins = e.transpose(bank_bf(b)[:, j * 128:(j + 1) * 128], h2[s][:, dk * 128:(dk + 1) * 128], ident_bf)
                return ins
            fw.op("pe", tr, r=[("h2", s), "ident"], w=[("ps", b)])
            fw.op("act", lambda e, g=g, tt=tt, b=b: e.activation(
                out=h2T[:, 4 * g:4 * g + 4, tt * 128:(tt + 1) * 128],
                in_=bank_bf(b)[:, 0:512].rearrange("p (a t) -> p a t", a=4), func=AF.Copy),
                r=[("ps", b)], w=["h2T"])
    fw.barrier()

    ar.p = PH
    qT = ar.alloc([128, 16, TOK], BF16)
    subkT = ar.alloc([128, 16, 128], BF16)
    PQ = ar.p
    Wq = [ar.alloc([128, 16, 512], BF16) for _ in range(2)]
    fw.dma("gp", subkT, subkT_d, w=["subk"])
    wq3 = wq.rearrange("(dk p) c -> p dk c", p=128)
    k = 0
    for piece in range(4):
        s = piece % 2
        fw.dma("gp", Wq[s], wq3[:, :, piece * 512:(piece + 1) * 512], w=[("Wq", s)])
        for bb in range(4):
            blk = piece * 4 + bb
            for half in range(2):
                b = k % 4
                k += 1

                def mm(e, bb=bb, half=half, s=s, b=b):
                    ins = None
                    for dk in range(16):
                        ins = e.matmul(bank(b), Wq[s][:, dk, bb * 128:(bb + 1) * 128], h2T[:, dk, half * 512:(half + 1) * 512],
                                       start=(dk == 0), stop=(dk == 15))
                    return ins
                fw.op("pe", mm, r=[("Wq", s), "h2T"], w=[("ps", b)])
                fw.op("act", lambda e, blk=blk, half=half, b=b: e.activation(
                    out=qT[:, blk, half * 512:(half + 1) * 512], in_=bank(b), func=AF.Copy), r=[("ps", b)], w=["qT"])
    fw.barrier()

    ar.p = PQ
    bufX = ar.alloc([128, 2048])
    bufY = ar.alloc([128, 2048])
    top = ar.alloc([128, 256])
    idx = ar.alloc([128, 256], U32)
    idxf = ar.alloc([128, 256])
    best = ar.alloc([128, 128])
    pos = ar.alloc([128, 128], U32)
    pa = ar.alloc([128, 128], U32)
    pb = ar.alloc([128, 128], U32)
    paf = ar.alloc([128, 128])
    pbf = ar.alloc([128, 128])
    bm = ar.alloc([128, 128])
    ex = ar.alloc([128, 128])
    Zs = ar.alloc([128, 8])
    rZ = ar.alloc([128, 8])
    R3 = ar.alloc([128, 3, 128], BF16)
    RT = ar.alloc([128, 3, 128])
    NT = 16
    A1t = [ar.alloc([128, NT, 128]) for _ in range(2)]
    A1 = [ar.alloc([128, NT, 128], BF16) for _ in range(2)]
    A2 = [ar.alloc([128, NT, 128], BF16) for _ in range(2)]
    Gs = [ar.alloc([128, 128, 128], BF16) for _ in range(2)]
    XS = [("X", i) for i in range(16)]
    YS = [("Y", i) for i in range(16)]
    sc3 = bufX.rearrange("p (s n) -> p s n", s=16)
    wk3 = bufY.rearrange("p (s n) -> p s n", s=16)
    top3 = top.rearrange("p (s k) -> p s k", s=16)
    idx3 = idx.rearrange("p (s k) -> p s k", s=16)
    top4 = top.rearrange("p (h c k) -> p h c k", h=8, c=2)
    idxf4 = idxf.rearrange("p (h c k) -> p h c k", h=8, c=2)
    cand3 = bufX.rearrange("p (h n) -> p h n", h=8)
    cand4 = bufX.rearrange("p (h a b) -> p h a b", h=8, a=16)
    cw3 = bufY.rearrange("p (h n) -> p h n", h=8)
    best3 = best.rearrange("p (h k) -> p h k", h=8)
    pos3 = pos.rearrange("p (h k) -> p h k", h=8)
    bm3 = bm.rearrange("p (h k) -> p h k", h=8)
    ex3 = ex.rearrange("p (h k) -> p h k", h=8)
    selY = bufY.rearrange("p (h k a) -> p h k a", h=8, k=16)
    selX = bufX.rearrange("p (h k a) -> p h k a", h=8, k=16)
    iota16b = iota[:, 0:16].unsqueeze(1).unsqueeze(1).to_broadcast([128, 8, 16, 16])
    all_top = [("top", sg, j) for sg in range(16) for j in range(2)]
    all_idx = [("idx", sg, j) for sg in range(16) for j in range(2)]
    all_best = [("best", hh, j) for hh in range(8) for j in range(2)]
    all_pos = [("pos", hh, j) for hh in range(8) for j in range(2)]

    def topk_stages(tt):
        def st0():
            def mm(e):
                ins = None
                for blk in range(16):
                    ins = e.matmul(psA[:, blk * 128:(blk + 1) * 128], qT[:, blk, tt * 128:(tt + 1) * 128], subkT[:, blk, :],
                                   start=True, stop=True)
                return ins
            fw.op("pe", mm, r=["qT", "subk"], w=[("ps", 0), ("ps", 1), ("ps", 2), ("ps", 3)])
            fw.op("act", lambda e: e.activation(out=bufX, in_=psA[:, :], func=AF.Copy),
                  r=[("ps", 0), ("ps", 1), ("ps", 2), ("ps", 3)], w=XS)
            for sg in range(16):
                fw.op("dve", lambda e, sg=sg: e.max(out=top3[:, sg, 0:8], in_=sc3[:, sg, :]), r=[("X", sg)], w=[("top", sg, 0)])

        def st1():
            for sg in range(16):
                fw.op("dve", lambda e, sg=sg: e.max_index(out=idx3[:, sg, 0:8], in_max=top3[:, sg, 0:8], in_values=sc3[:, sg, :]),
                      r=[("X", sg), ("top", sg, 0)], w=[("idx", sg, 0)])
                fw.op("dve", lambda e, sg=sg: e.match_replace(out=wk3[:, sg, :], in_to_replace=top3[:, sg, 0:8],
                                                              in_values=sc3[:, sg, :], imm_value=NEG),
                      r=[("X", sg), ("top", sg, 0)], w=[("Y", sg)])

        def st2():
            for sg in range(16):
                fw.op("dve", lambda e, sg=sg: e.max(out=top3[:, sg, 8:16], in_=wk3[:, sg, :]), r=[("Y", sg)], w=[("top", sg, 1)])

        def st3():
            for sg in range(16):
                fw.op("dve", lambda e, sg=sg: e.max_index(out=idx3[:, sg, 8:16], in_max=top3[:, sg, 8:16], in_values=wk3[:, sg, :]),
                      r=[("Y", sg), ("top", sg, 1)], w=[("idx", sg, 1)])
            fw.op("dve", lambda e: e.tensor_copy(idxf, idx), r=all_idx, w=["idxf"])

        def st4():
            fw.op("dve", lambda e: e.tensor_tensor(cand4, top4[:, :, 0, :].unsqueeze(3).to_broadcast([128, 8, 16, 16]),
                                                   top4[:, :, 1, :].unsqueeze(2).to_broadcast([128, 8, 16, 16]), ALU.add),
                  r=all_top, w=XS)
            for hh in range(8):
                fw.op("dve", lambda e, hh=hh: e.max(out=best3[:, hh, 0:8], in_=cand3[:, hh, :]),
                      r=[("X", 2 * hh), ("X", 2 * hh + 1)], w=[("best", hh, 0)])

        def st5():
            for hh in range(8):
                fw.op("dve", lambda e, hh=hh: e.max_index(out=pos3[:, hh, 0:8], in_max=best3[:, hh, 0:8], in_values=cand3[:, hh, :]),
                      r=[("X", 2 * hh), ("X", 2 * hh + 1), ("best", hh, 0)], w=[("pos", hh, 0)])
                fw.op("dve", lambda e, hh=hh: e.match_replace(out=cw3[:, hh, :], in_to_replace=best3[:, hh, 0:8],
                                                              in_values=cand3[:, hh, :], imm_value=NEG),
                      r=[("X", 2 * hh), ("X", 2 * hh + 1), ("best", hh, 0)], w=[("Y", 2 * hh), ("Y", 2 * hh + 1)])
            for hh in range(8):
                fw.op("dve", lambda e, hh=hh: e.max(out=best3[:, hh, 8:16], in_=cw3[:, hh, :]),
                      r=[("Y", 2 * hh), ("Y", 2 * hh + 1)], w=[("best", hh, 1)])

        def st6():
            for hh in range(8):
                fw.op("dve", lambda e, hh=hh: e.max_index(out=pos3[:, hh, 8:16], in_max=best3[:, hh, 8:16], in_values=cw3[:, hh, :]),
                      r=[("Y", 2 * hh), ("Y", 2 * hh + 1), ("best", hh, 1)], w=[("pos", hh, 1)])
            fw.op("dve", lambda e: e.tensor_tensor(bm3, best3, best3[:, :, 0:1].to_broadcast([128, 8, 16]), ALU.subtract),
                  r=all_best, w=["bm"])
            fw.op("act", lambda e: e.activation(out=ex, in_=bm, func=AF.Exp), r=["bm"], w=["ex"])
            fw.op("dve", lambda e: e.tensor_single_scalar(pa, pos, 4, ALU.logical_shift_right), r=all_pos, w=["pa"])
            fw.op("dve", lambda e: e.tensor_single_scalar(pb, pos, 15, ALU.bitwise_and), r=all_pos, w=["pb"])
            fw.op("dve", lambda e: e.tensor_copy(paf, pa), r=["pa"], w=["paf"])
            fw.op("dve", lambda e: e.tensor_copy(pbf, pb), r=["pb"], w=["pbf"])

        def st7():
            paf3 = paf.rearrange("p (h k) -> p h k", h=8)
            pbf3 = pbf.rearrange("p (h k) -> p h k", h=8)
            fw.op("dve", lambda e: e.tensor_tensor(selY, iota16b, paf3.unsqueeze(3).to_broadcast([128, 8, 16, 16]), ALU.is_equal),
                  r=["paf", "iota"], w=YS)
            fw.op("gp", lambda e: e.tensor_tensor(selY, selY, idxf4[:, :, 0, :].unsqueeze(2).to_broadcast([128, 8, 16, 16]), ALU.mult),
                  r=YS + ["idxf"], w=YS)
            fw.op("dve", lambda e: e.tensor_tensor(selX, iota16b, pbf3.unsqueeze(3).to_broadcast([128, 8, 16, 16]), ALU.is_equal),
                  r=["pbf", "iota"], w=XS)
            fw.op("gp", lambda e: e.tensor_tensor(selX, selX, idxf4[:, :, 1, :].unsqueeze(2).to_broadcast([128, 8, 16, 16]), ALU.mult),
                  r=XS + ["idxf"], w=XS)
            fw.op("dve", lambda e: e.tensor_reduce(Zs, ex3, AX.X, ALU.add), r=["ex"], w=["Zs"])
            fw.op("dve", lambda e: e.reciprocal(rZ, Zs), r=["Zs"], w=["rZ"])
            fw.op("dve", lambda e: e.tensor_tensor(R3[:, 2, :].rearrange("p (h k) -> p h k", h=8), ex3,
                                                   rZ.unsqueeze(2).to_broadcast([128, 8, 16]), ALU.mult),
                  r=["ex", "rZ"], w=["R3g"])
            fw.op("dve", lambda e: e.tensor_reduce(R3[:, 0, :].rearrange("p (h k) -> p h k", h=8), selY, AX.X, ALU.add),
                  r=YS, w=[("R3", 0)])
            fw.op("dve", lambda e: e.tensor_reduce(R3[:, 1, :].rearrange("p (h k) -> p h k", h=8), selX, AX.X, ALU.add),
                  r=XS, w=[("R3", 1)])
        return [st0, st1, st2, st3, st4, st5, st6, st7]

    def onehot_prelude(tt):
        def tr(e):
            ins = None
            for j in range(3):
                ins = e.transpose(bank_bf(4)[:, j * 128:(j + 1) * 128], R3[:, j, :], ident_bf)
            return ins
        fw.op("pe", tr, r=[("R3", 0), ("R3", 1), "R3g", "ident"], w=[("ps", 4)])
        fw.op("act", lambda e: e.activation(out=RT.rearrange("p a b -> p (a b)"), in_=bank_bf(4)[:, 0:384], func=AF.Copy),
              r=[("ps", 4)], w=["RT"])

    iotab = iota.unsqueeze(1).to_broadcast([128, NT, 128])

    def onehot_group(tt, g):
        gs = Gs[tt % 2]
        s = g % 2
        t0 = g * NT
        fw.op("dve", lambda e: e.tensor_tensor(A2[s], iotab, RT[:, 1, t0:t0 + NT].unsqueeze(2).to_broadcast([128, NT, 128]),
                                               ALU.is_equal), r=["RT", "iota"], w=[("A2", s)])
        fw.op("dve", lambda e: e.tensor_tensor(A1t[s], iotab, RT[:, 0, t0:t0 + NT].unsqueeze(2).to_broadcast([128, NT, 128]),
                                               ALU.is_equal), r=["RT", "iota"], w=[("A1t", s)])
        def gate(e):
            ins = None
            for j in range(NT):
                ins = e.activation(out=A1[s][:, j, :], in_=A1t[s][:, j, :], func=AF.Copy, scale=RT[:, 2, t0 + j:t0 + j + 1])
            return ins
        if g % 3 == 2:
            fw.op("gp", lambda e: e.tensor_tensor(A1[s], A1t[s], RT[:, 2, t0:t0 + NT].unsqueeze(2).to_broadcast([128, NT, 128]),
                                                  ALU.mult), r=["RT", ("A1t", s)], w=[("A1", s)])
        else:
            fw.op("act", gate, r=["RT", ("A1t", s)], w=[("A1", s)])
        for q4 in range(NT // 4):
            b = 5 + (q4 % 2)

            def gm(e, q4=q4, b=b):
                ins = None
                for j in range(4):
                    ins = e.matmul(bank(b)[:, j * 128:(j + 1) * 128], A1[s][:, 4 * q4 + j, :], A2[s][:, 4 * q4 + j, :],
                                   start=True, stop=True)
                return ins
            fw.op("pe", gm, r=[("A1", s), ("A2", s)], w=[("ps", b)])
            tq = t0 + 4 * q4
            fw.op("act", lambda e, b=b, tq=tq: e.activation(
                out=gs[:, :, tq:tq + 4], in_=bank(b).rearrange("p (t i) -> p i t", t=4), func=AF.Copy),
                r=[("ps", b)], w=[("Gs", tt % 2, tq)])

    for st in topk_stages(0):
        st()
    for tt in range(8):
        onehot_prelude(tt)
        nxt = topk_stages(tt + 1) if tt + 1 < 8 else []
        for g in range(128 // NT):
            if g < len(nxt):
                nxt[g]()
            onehot_group(tt, g)
        fw.dma("sp", Gd[tt].rearrange("p a b -> p (a b)"), Gs[tt % 2].rearrange("p a b -> p (a b)"),
               r=[("Gs", tt % 2, tq_) for tq_ in range(0, 128, 4)], w=[("Gd", tt)], key=("Gdk", tt % 2))
    fw.barrier()

    ar.p = PO
    GRP = 4
    UT = [ar.alloc([128, 16, 128], BF16) for _ in range(3)]
    Vc = [[ar.alloc([128, D], BF16) for _ in range(GRP)] for _ in range(2)]
    Gc = [ar.alloc([128, TOK], BF16) for _ in range(3)]
    AT = [ar.alloc([128, GRP, TOK], BF16) for _ in range(2)]
    ge = [ar.alloc([128, 512], BF16) for _ in range(2)]
    all_gd = [("Gd", t) for t in range(8)]
    kU = 0
    kV = 0
    for grp in range(128 // GRP):
        gsl = grp % 2
        for j in range(GRP):
            c = grp * GRP + j
            s3 = c % 3
            fw.dma("gp", UT[s3].rearrange("p a b -> p (a b)"), UT_l[c], w=[("UT", s3)])
            fw.dma("gp", Vc[gsl][j], V_l[c], w=[("Vc", gsl, j)])
            fw.dma("sp", Gc[s3].rearrange("p (a t) -> p a t", a=8), Gd[:, :, c, :].rearrange("a p t -> p a t"),
                   r=all_gd, w=[("Gc", s3)])
            for half in range(2):
                b = kU % 2
                kU += 1

                def mm(e, s3=s3, half=half, b=b):
                    ins = None
                    for dk in range(16):
                        ins = e.matmul(bank(b), UT[s3][:, dk, :], h2T[:, dk, half * 512:(half + 1) * 512],
                                       start=(dk == 0), stop=(dk == 15))
                    return ins
                fw.op("pe", mm, r=[("UT", s3), "h2T"], w=[("ps", b)])
                fw.op("act", lambda e, b=b: e.activation(out=ge[b], in_=bank(b), func=AF.Gelu_apprx_tanh), r=[("ps", b)], w=[("ge", b)])
                fw.op("dve", lambda e, b=b, gsl=gsl, j=j, half=half, s3=s3: e.tensor_tensor(
                    AT[gsl][:, j, half * 512:(half + 1) * 512], ge[b], Gc[s3][:, half * 512:(half + 1) * 512], ALU.mult),
                    r=[("ge", b), ("Gc", s3)], w=[("AT", gsl, j)])
        if grp == 0:
            fw.dma("sp", acc.rearrange("p a b -> p (a b)"), x1_scr, r=["x1scr"],
                   w=["acc"] + [("acc", t_, o_) for t_ in range(8) for o_ in range(4)], key="accld")
        for tt in range(8):
            for oc in range(4):
                b = 4 + (kV % 4)
                kV += 1

                def vm(e, gsl=gsl, tt=tt, oc=oc, b=b):
                    ins = None
                    for j in range(GRP):
                        ins = e.matmul(bank(b), AT[gsl][:, j, tt * 128:(tt + 1) * 128], Vc[gsl][j][:, oc * 512:(oc + 1) * 512],
                                       start=(j == 0), stop=(j == GRP - 1))
                    return ins
                fw.op("pe", vm, r=[("AT", gsl, j) for j in range(GRP)] + [("Vc", gsl, j) for j in range(GRP)], w=[("ps", b)])
                fw.op("dve", lambda e, tt=tt, oc=oc, b=b: e.tensor_tensor(
                    acc[:, tt, oc * 512:(oc + 1) * 512], bank(b), acc[:, tt, oc * 512:(oc + 1) * 512], ALU.add),
                    r=[("ps", b), ("acc", tt, oc)], w=[("acc", tt, oc)], extra=None)
    fw.barrier()

    ar.p = PO
    gf_b = ar.alloc([128, D])
    junk = ar.alloc([128, D], BF16)
    ost = [ar.alloc([128, D]) for _ in range(2)]
    ssqf = ar.alloc([128, 8])
    rtf = ar.alloc([128, 8])
    rsf = ar.alloc([128, 8])
    fw.dma("sp", gf_b, gf_d, w=["gf"])
    fw.op("dve", lambda e: e.memset(ssqf, 0.0), w=["ssqf"])
    for tt in range(8):
        s = tt % 2
        fw.op("act", lambda e, tt=tt: e.activation(out=junk, in_=acc[:, tt, :], func=AF.Square,
                                                   accum_out=ssqf[:, tt:tt + 1]), r=["ssqf"], w=["junkf", ("ssqf", tt)])
        fw.op("act", lambda e, tt=tt: e.activation(out=rtf[:, tt:tt + 1], in_=ssqf[:, tt:tt + 1], func=AF.Sqrt,
                                                   bias=EPS, scale=1.0 / D), r=[("ssqf", tt)], w=[("rtf", tt)])
        fw.op("dve", lambda e, tt=tt: e.reciprocal(rsf[:, tt:tt + 1], rtf[:, tt:tt + 1]), r=[("rtf", tt)], w=[("rsf", tt)])
        fw.op("dve", lambda e, tt=tt, s=s: e.scalar_tensor_tensor(out=ost[s], in0=acc[:, tt, :], scalar=rsf[:, tt:tt + 1],
                                                                  in1=gf_b, op0=ALU.mult, op1=ALU.mult),
              r=[("rsf", tt), "gf"], w=[("ost", s)])
        fw.dma("sp", out_d[tt * 128:(tt + 1) * 128, :], ost[s], r=[("ost", s)], w=[("out", tt)], key=("out", s))

    keys = fw.analyze()
    with ExitStack() as es:
        sems = {}
        for i, k in enumerate(keys):
            sems[k] = es.enter_context(nc.semaphore("s%d" % i))
        block = es.enter_context(nc.Block())
        with nc.allow_low_precision(reason="bf16 matmul operands, exact small ints"):
            fw.emit(nc, block, sems)
    return nc


def _constants():
    half = 64
    inv = (10000.0 ** (-np.arange(0, half, 2, dtype=np.float32) / half)).astype(np.float32)
    row = np.repeat(np.arange(S // 64, dtype=np.float32), 64)
    col = np.tile(np.arange(64, dtype=np.float32), S // 64)
    ang = np.concatenate([row[:, None] * inv, col[:, None] * inv], axis=-1).astype(np.float32)
    rope = np.concatenate([np.cos(ang), np.sin(ang)], axis=-1).astype(np.float32)
    slopes = 2.0 ** (-8.0 * (np.arange(8, dtype=np.float64) + 1.0) / 8)
    kk = np.arange(128)[:, None, None]
    mm_ = np.arange(23)[None, :, None]
    qq = np.arange(128)[None, None, :]
    delta = (11 - mm_) * 128 + kk - qq
    ad = np.abs(delta)
    cnt = (ad <= 64).astype(np.float64) + ((ad <= 256) & (delta % 4 == 0)) + ((ad <= 1024) & (delta % 16 == 0))
    mask = np.stack([cnt * np.exp(-slopes[h] * ad) for h in range(8)], axis=0)
    mask = mask.reshape(8, 128, 23 * 128).astype(np.float32).astype(ml_dtypes.bfloat16)
    iota = np.tile(np.arange(128, dtype=np.float32)[None, :], (128, 1))
    ident = np.eye(128, dtype=np.float32)
    return rope, mask, iota, ident


_NC_CACHE = {}


def kernel(x, norm1_g, w_in, q_norm_g, k_norm_g, w_out, norm2_g, peer_w_query, peer_sub_keys,
           peer_u, peer_v, final_norm_g):
    f32 = np.float32
    x = np.asarray(x, f32)
    w_in0 = np.asarray(w_in, f32)[0]
    rope, mask, iota, ident = _constants()
    xT_full = np.ascontiguousarray(x[0].T)
    wA = np.ascontiguousarray(np.stack([np.concatenate(
        [w_in0[:, h * 128:(h + 1) * 128], w_in0[:, 1024 + h * 128:1024 + (h + 1) * 128],
         w_in0[:, 2048 + h * 128:2048 + (h + 1) * 128]], axis=1) for h in range(8)], axis=0))
    wqB = np.ascontiguousarray(w_in0[:, 3072:4096])
    wkvB = np.ascontiguousarray(w_in0[:, 4096:4608])
    U = np.asarray(peer_u, f32)[0]
    V = np.asarray(peer_v, f32)[0]
    UT_l = np.ascontiguousarray(U.reshape(128, 128, 16, 128).transpose(1, 3, 2, 0)).reshape(128, 128, 16 * 128)
    V_l = np.ascontiguousarray(V.reshape(128, 128, D).transpose(1, 0, 2))
    subkT = np.ascontiguousarray(np.asarray(peer_sub_keys, f32)[0].reshape(16, 128, 128).transpose(2, 0, 1))
    common = {
        "maskA": mask, "g1T": np.ascontiguousarray(np.asarray(norm1_g, f32)[0].reshape(16, 128).T),
        "qg_b": np.ascontiguousarray(np.tile(np.asarray(q_norm_g, f32)[0][None, :], (128, 1))),
        "kg_b": np.ascontiguousarray(np.tile(np.asarray(k_norm_g, f32)[0][None, :], (128, 1))),
        "g2_b": np.ascontiguousarray(np.tile(np.asarray(norm2_g, f32)[0][None, :], (128, 1))),
        "gf_b": np.ascontiguousarray(np.tile(np.asarray(final_norm_g, f32)[None, :], (128, 1))),
        "iota": iota, "ident": ident, "wA": wA, "wqB": wqB, "wkvB": wkvB,
        "w_out": np.ascontiguousarray(np.asarray(w_out, f32)[0]),
        "wq": np.ascontiguousarray(np.asarray(peer_w_query, f32)[0]),
        "subkT": subkT, "UT_l": UT_l, "V_l": V_l,
    }
    in_maps = []
    for c in range(NCORES):
        shift = 1024 * c - 1024
        xr = np.roll(xT_full, -shift, axis=1).reshape(16, 128, S // 256, 256)
        xr = np.ascontiguousarray(xr.transpose(2, 1, 0, 3)).reshape(S // 256, 128, 16 * 256)
        rr = np.roll(rope, -shift, axis=0)
        ropeT = np.ascontiguousarray(rr.reshape(64, 128, 128).transpose(1, 0, 2))
        tokpos = shift + np.arange(WIN)
        valid = ((tokpos >= 0) & (tokpos < S)).astype(f32).reshape(24, 128).T
        m = dict(common)
        m.update({"xT": xr, "x_own": np.ascontiguousarray(x[0, 1024 * c:1024 * (c + 1), :]), "ropeT": ropeT,
                  "valid": np.ascontiguousarray(valid)})
        in_maps.append(m)
    if "nc" not in _NC_CACHE:
        _NC_CACHE["nc"] = build_program()
    res = run_bass_kernel_spmd(_NC_CACHE["nc"], in_maps, core_ids=list(range(NCORES)))
    out = np.concatenate([np.asarray(r["out"], f32) for r in res.results], axis=0)
    return out.reshape(1, S, D)
```

```python
import math
from contextlib import ExitStack

import numpy as np
import ml_dtypes
import concourse.bass as bass
import concourse.mybir as mybir
from concourse.bass_utils import run_bass_kernel_spmd

F32 = mybir.dt.float32
BF16 = mybir.dt.bfloat16
U32 = mybir.dt.uint32
AF = mybir.ActivationFunctionType
ALU = mybir.AluOpType
AX = mybir.AxisListType

NCORES = 8
S = 8192
D = 2048
TOK = 1024
WIN = 3072
EPS = 1e-6
SCALE = 128.0 ** -0.5
ARENA = 51800
NEG = -1.0e30


class Fw:
    ENG = ("sp", "gp", "pe", "act", "dve")

    def __init__(self):
        self.ops = []

    def op(self, eng, fn, r=(), w=(), dma=False, key=None, extra=None):
        self.ops.append(dict(eng=eng, fn=fn, r=tuple(r), w=tuple(w), dma=dma, key=key,
                             extra=extra, deps=set(), signal=False))
        return len(self.ops) - 1

    def dma(self, eng, out, in_, r=(), w=(), key=None):
        k = key if key is not None else (w[0] if w else ("dma", len(self.ops)))
        return self.op(eng, lambda e, o=out, i=in_: e.dma_start(out=o, in_=i), r=r, w=w, dma=True, key=k)

    def barrier(self):
        last = {}
        for i, o in enumerate(self.ops):
            last[o["eng"]] = i
            if o["dma"]:
                last[("k", o["key"])] = i
        deps = set(last.values())
        for e in self.ENG:
            self.op(e, None, extra=set(deps))

    def analyze(self):
        lastw, readers = {}, {}
        for i, o in enumerate(self.ops):
            deps = set()
            if o["extra"]:
                deps |= o["extra"]
            for b in o["r"]:
                if b in lastw:
                    deps.add(lastw[b])
            for b in o["w"]:
                if b in lastw:
                    deps.add(lastw[b])
                deps.update(readers.get(b, ()))
            deps.discard(i)
            for b in o["r"]:
                readers.setdefault(b, []).append(i)
            for b in o["w"]:
                lastw[b] = i
                readers[b] = []
            real = set()
            for j in deps:
                p = self.ops[j]
                if p["fn"] is None:
                    continue
                if (not p["dma"]) and p["eng"] == "pe" and o["eng"] == "pe" and not o["dma"]:
                    continue
                real.add(j)
            o["deps"] = real
            for j in real:
                self.ops[j]["signal"] = True
        for o in self.ops:
            if o["dma"]:
                o["signal"] = True
        cnt = {}
        for o in self.ops:
            if o["fn"] is None or not o["signal"]:
                continue
            k = ("k", o["key"]) if o["dma"] else ("e", o["eng"])
            cnt[k] = cnt.get(k, 0) + (16 if o["dma"] else 1)
            o["sig"] = (k, cnt[k])
        self.final = dict(cnt)
        return sorted(cnt.keys(), key=str)

    def emit(self, nc, block, sems):
        engs = {"sp": block.sync, "gp": block.gpsimd, "pe": block.tensor, "act": block.scalar,
                "dve": block.vector}
        for en in self.ENG:
            mine = [o for o in self.ops if o["eng"] == en]
            final = self.final if en == "sp" else None

            def body(e, mine=mine, final=final):
                seen = {}
                for o in mine:
                    for j in sorted(o["deps"]):
                        k, v = self.ops[j]["sig"]
                        if seen.get(k, 0) >= v:
                            continue
                        seen[k] = v
                        e.wait_ge(sems[k], v)
                    if o["fn"] is None:
                        continue
                    ins = o["fn"](e)
                    if o["signal"]:
                        k, v = o["sig"]
                        ins.then_inc(sems[k], 16 if o["dma"] else 1)
                if final is not None:
                    for k, v in sorted(final.items(), key=str):
                        if k[0] == "k" and seen.get(k, 0) < v:
                            e.wait_ge(sems[k], v)

            engs[en](body)


def build_program():
    nc = bass.Bass("TRN2", target_bir_lowering=False)
    fw = Fw()

    def din(name, shape, dt=F32):
        return nc.dram_tensor(name, list(shape), dt, kind="ExternalInput").ap()

    xT = din("xT", [S // 256, 128, 16 * 256])
    x_own = din("x_own", [TOK, D])
    ropeT = din("ropeT", [128, 64, 128])
    valid_d = din("valid", [128, 24])
    maskA = din("maskA", [8, 128, 23 * 128], BF16)
    g1T_d = din("g1T", [128, 16])
    qg_d = din("qg_b", [128, 128])
    kg_d = din("kg_b", [128, 128])
    g2_d = din("g2_b", [128, D])
    gf_d = din("gf_b", [128, D])
    iota_d = din("iota", [128, 128])
    ident_d = din("ident", [128, 128])
    wA = din("wA", [8, D, 384])
    wqB = din("wqB", [D, 1024])
    wkvB = din("wkvB", [D, 512])
    w_out = din("w_out", [D, D])
    wq = din("wq", [D, D])
    subkT_d = din("subkT", [128, 16, 128])
    UT_l = din("UT_l", [128, 128, 16 * 128])
    V_l = din("V_l", [128, 128, D])
    out_d = nc.dram_tensor("out", [TOK, D], F32, kind="ExternalOutput").ap()
    Gd = nc.dram_tensor("Gd", [8, 128, 128, 128], BF16, kind="Internal").ap()
    x1_scr = nc.dram_tensor("x1_scr", [128, 8 * D], F32, kind="Internal").ap()

    arena = nc.alloc_sbuf_tensor("arena", [128, ARENA], F32)
    psA = nc.alloc_psum_tensor("psA", [128, 2048], F32)
    psB = nc.alloc_psum_tensor("psB", [128, 2048], F32)

    def bank(i):
        t = psA if i < 4 else psB
        j = i % 4
        return t[:, j * 512:(j + 1) * 512]

    def bank_bf(i):
        return bank(i).bitcast(BF16)

    class Ar:
        def __init__(self):
            self.p = 0

        def alloc(self, shape, dt=F32):
            n = int(np.prod(shape[1:]))
            slots = n if dt in (F32, U32) else (n + 1) // 2
            slots = (slots + 7) // 8 * 8
            off = self.p
            self.p += slots
            assert self.p <= ARENA, ("arena overflow", self.p)
            ap = arena[:, off:off + slots]
            if dt != F32:
                ap = ap.bitcast(dt)
            ap = ap[:, 0:n]
            if len(shape) == 3:
                ap = ap.rearrange("p (a b) -> p a b", a=shape[1])
            elif len(shape) == 4:
                ap = ap.rearrange("p (a b c) -> p a b c", a=shape[1], b=shape[2])
            return ap

    ar = Ar()

    ones_bf = ar.alloc([128, 128], BF16)
    ident_bf = ar.alloc([128, 128], BF16)
    g1T = ar.alloc([128, 16])
    validT = ar.alloc([128, 24])
    kg_b = ar.alloc([128, 128])
    qg_b = ar.alloc([128, 128])
    iota = ar.alloc([128, 128])
    fw.dma("gp", ident_bf, ident_d, w=["ident"])
    fw.dma("sp", g1T, g1T_d, w=["g1T"])
    fw.dma("sp", validT, valid_d, w=["valid"])
    fw.dma("sp", kg_b, kg_d, w=["kg"])
    fw.dma("sp", qg_b, qg_d, w=["qg"])
    fw.dma("sp", iota, iota_d, w=["iota"])
    fw.op("dve", lambda e: e.memset(ones_bf, 1.0), w=["ones"])
    MIX0 = ar.p
    mixA = ar.alloc([128, 8, TOK], BF16)
    QT_B = ar.alloc([128, 8, TOK], BF16)
    P0 = ar.p

    def rms_feature_major(xst, ntok, slot_id, sq, rt, rstd, dst_fn, psb, rid, wid):
        fw.op("act", lambda e: e.activation(out=sq, in_=xst, func=AF.Square), r=[slot_id], w=["sq"])

        def mm(e):
            ins = None
            for dk in range(16):
                ins = e.matmul(bank(psb)[:, 0:ntok], ones_bf, sq[:, dk, :], start=(dk == 0), stop=(dk == 15))
            return ins
        fw.op("pe", mm, r=["sq", "ones"], w=[("ps", psb)])
        fw.op("act", lambda e: e.activation(out=rt, in_=bank(psb)[:, 0:ntok], func=AF.Sqrt, bias=EPS,
                                            scale=1.0 / D), r=[("ps", psb)], w=["rt"])
        fw.op("dve", lambda e: e.reciprocal(rstd, rt), r=["rt"], w=["rstd"])

        def norm(e):
            ins = None
            for dk in range(16):
                ins = e.scalar_tensor_tensor(out=dst_fn(dk), in0=xst[:, dk, :], scalar=g1T[:, dk:dk + 1],
                                             in1=rstd, op0=ALU.mult, op1=ALU.mult)
            return ins
        fw.op("dve", norm, r=[slot_id, "rstd", "g1T"] + list(rid), w=list(wid))

    def qk_post(raw, nh, gain, gain_id, rope_ap, rope_id, tmp, ssq, rt2, rs2, kn, kr, tA, tB, psb, dst_fn, dst_ids, tag):
        n = nh * 128
        raw3 = raw.rearrange("p (h d) -> p h d", h=nh)
        tmp3 = tmp.rearrange("p (h d) -> p h d", h=nh)
        kn3 = kn.rearrange("p (h d) -> p h d", h=nh)
        fw.op("dve", lambda e: e.tensor_tensor(tmp, raw, raw, ALU.mult), r=[tag + "raw"], w=[tag + "tmp"])
        fw.op("dve", lambda e: e.tensor_reduce(ssq, tmp3, AX.X, ALU.add), r=[tag + "tmp"], w=[tag + "ssq"])
        fw.op("act", lambda e: e.activation(out=rt2, in_=ssq, func=AF.Sqrt, bias=EPS, scale=1.0 / 128),
              r=[tag + "ssq"], w=[tag + "rt2"])
        fw.op("dve", lambda e: e.reciprocal(rs2, rt2), r=[tag + "rt2"], w=[tag + "rs2"])
        fw.op("dve", lambda e: e.tensor_tensor(kn3, raw3, rs2.unsqueeze(2).to_broadcast([128, nh, 128]), ALU.mult),
              r=[tag + "raw", tag + "rs2"], w=[tag + "kn"])
        fw.op("dve", lambda e: e.tensor_tensor(kn3, kn3, gain.unsqueeze(1).to_broadcast([128, nh, 128]), ALU.mult),
              r=[tag + "kn", gain_id], w=[tag + "kn"])
        kn4 = kn.rearrange("p (h i two) -> p h i two", h=nh, two=2)
        kr4 = kr.rearrange("p (h i two) -> p h i two", h=nh, two=2)
        x0, x1 = kn4[:, :, :, 0], kn4[:, :, :, 1]
        cosb = rope_ap[:, 0:64].unsqueeze(1).to_broadcast([128, nh, 64])
        sinb = rope_ap[:, 64:128].unsqueeze(1).to_broadcast([128, nh, 64])
        tA3 = tA.rearrange("p (h i) -> p h i", h=nh)
        tB3 = tB.rearrange("p (h i) -> p h i", h=nh)
        fw.op("dve", lambda e: e.tensor_tensor(tA3, x0, cosb, ALU.mult), r=[tag + "kn", rope_id], w=[tag + "tA"])
        fw.op("dve", lambda e: e.tensor_tensor(tB3, x1, sinb, ALU.mult), r=[tag + "kn", rope_id], w=[tag + "tB"])
        fw.op("dve", lambda e: e.tensor_tensor(kr4[:, :, :, 0], tA3, tB3, ALU.subtract),
              r=[tag + "tA", tag + "tB"], w=[tag + "kr"])
        fw.op("dve", lambda e: e.tensor_tensor(tA3, x0, sinb, ALU.mult), r=[tag + "kn", rope_id], w=[tag + "tA"])
        fw.op("dve", lambda e: e.tensor_tensor(tB3, x1, cosb, ALU.mult), r=[tag + "kn", rope_id], w=[tag + "tB"])
        fw.op("dve", lambda e: e.tensor_tensor(kr4[:, :, :, 1], tA3, tB3, ALU.add),
              r=[tag + "tA", tag + "tB"], w=[tag + "kr"])
        for h0 in range(0, nh, 4):
            hn = min(4, nh - h0)

            def tr(e, h0=h0, hn=hn):
                ins = None
                for j in range(hn):
                    ins = e.transpose(bank_bf(psb)[:, j * 128:(j + 1) * 128], kr[:, (h0 + j) * 128:(h0 + j + 1) * 128],
                                      ident_bf)
                return ins
            fw.op("pe", tr, r=[tag + "kr", "ident"], w=[("ps", psb)])
            for j in range(hn):
                fw.op("act", lambda e, j=j, h0=h0: e.activation(out=dst_fn(h0 + j), in_=bank_bf(psb)[:, j * 128:(j + 1) * 128],
                                                                func=AF.Copy),
                      r=[("ps", psb)], w=[dst_ids[h0 + j]])

    def attention(KT_fn, V_fn, QT, kts, kid, vid, qid, mask_fn, mask_id, E2, P2, rec, dst, dst_id, pso, psd, tagc,
                  sbanks=(2, 3), hook=None):
        n = len(kts)
        nb = len(sbanks)
        L = nb - 1

        def qk(i):
            kt = kts[i]
            b = sbanks[i % nb]
            fw.op("pe", lambda e, kt=kt, b=b: e.matmul(bank(b), KT_fn(kt), QT, start=True, stop=True),
                  r=[kid, qid], w=[("ps", b)])

        for i in range(min(L, n)):
            qk(i)
        for i in range(n):
            kt = kts[i]
            b = sbanks[i % nb]
            if i + L < n:
                qk(i + L)
            Eb = E2[i % nb]
            fw.op("act", lambda e, b=b, Eb=Eb: e.activation(out=Eb, in_=bank(b), func=AF.Exp, scale=SCALE),
                  r=[("ps", b)], w=[("E", i % nb)])
            if mask_fn is not None:
                Pb = P2[i % nb]
                mk = mask_fn(i)
                fw.op("dve", lambda e, Pb=Pb, Eb=Eb, mk=mk, kt=kt: e.scalar_tensor_tensor(
                    out=Pb, in0=Eb, scalar=validT[:, kt:kt + 1], in1=mk, op0=ALU.mult, op1=ALU.mult),
                    r=[("E", i % nb), mask_id, "valid"], w=[("P", i % nb)])
                pid = ("P", i % nb)
            else:
                Pb = Eb
                pid = ("E", i % nb)

            def pv(e, kt=kt, Pb=Pb, i=i):
                e.matmul(bank(pso), V_fn(kt), Pb, start=(i == 0), stop=(i == n - 1))
                return e.matmul(bank(psd), ones_bf, Pb, start=(i == 0), stop=(i == n - 1))
            fw.op("pe", pv, r=[pid, vid, "ones"], w=[("ps", pso), ("ps", psd)])
            if hook is not None:
                hook(i)
        fw.op("dve", lambda e: e.reciprocal(rec, bank(psd)), r=[("ps", psd)], w=["rec" + tagc])
        fw.op("dve", lambda e: e.tensor_tensor(dst, bank(pso), rec, ALU.mult),
              r=[("ps", pso), "rec" + tagc], w=[dst_id])

    ar.p = P0
    hwin = ar.alloc([128, 16, WIN], BF16)
    PA = ar.p
    xst = [ar.alloc([128, 16, 256]) for _ in range(2)]
    sq = ar.alloc([128, 16, 256], BF16)
    rt = ar.alloc([128, 256])
    rstd = ar.alloc([128, 256])
    for ck in range(WIN // 256):
        s = ck % 2
        fw.dma("sp", xst[s].rearrange("p a b -> p (a b)"), xT[ck], w=[("xst", s)])
        rms_feature_major(xst[s], 256, ("xst", s), sq, rt, rstd,
                          lambda dk, ck=ck: hwin[:, dk, ck * 256:(ck + 1) * 256], 0, [], [("hwin", ck // 2)])
    fw.barrier()

    ar.p = PA
    WqB = ar.alloc([128, 16, 1024], BF16)
    ropeQ = ar.alloc([128, 8, 128])
    rawq = ar.alloc([128, 1024])
    tmpq = ar.alloc([128, 1024])
    knq = ar.alloc([128, 1024])
    krq = ar.alloc([128, 1024], BF16)
    tAq = ar.alloc([128, 512])
    tBq = ar.alloc([128, 512])
    ssq8 = ar.alloc([128, 8])
    rt8 = ar.alloc([128, 8])
    rs8 = ar.alloc([128, 8])
    fw.dma("gp", WqB, wqB.rearrange("(dk p) c -> p dk c", p=128), w=["WqB"])
    fw.dma("sp", ropeQ, ropeT[:, 8:16, :], w=["ropeQ"])
    win_ids = [("hwin", i) for i in range(6)]
    for tt in range(8):
        for half in range(2):
            def mm(e, tt=tt, half=half):
                ins = None
                for dk in range(16):
                    ins = e.matmul(bank(half), hwin[:, dk, 1024 + tt * 128:1024 + (tt + 1) * 128],
                                   WqB[:, dk, half * 512:(half + 1) * 512], start=(dk == 0), stop=(dk == 15))
                return ins
            fw.op("pe", mm, r=["WqB"] + win_ids, w=[("ps", half)])
            fw.op("act", lambda e, half=half: e.activation(out=rawq[:, half * 512:(half + 1) * 512], in_=bank(half),
                                                           func=AF.Copy), r=[("ps", half)], w=["qraw"])
        qk_post(rawq, 8, qg_b, "qg", ropeQ[:, tt, :], "ropeQ", tmpq, ssq8, rt8, rs8, knq, krq, tAq, tBq, 6,
                lambda h, tt=tt: QT_B[:, h, tt * 128:(tt + 1) * 128], [("QTB", h) for h in range(8)], "q")
    fw.barrier()

    ar.p = PA
    WA = [ar.alloc([128, 16, 384], BF16) for _ in range(2)]
    maskS = [ar.alloc([128, 23, 128], BF16) for _ in range(2)]
    KT_h = [ar.alloc([128, WIN], BF16) for _ in range(2)]
    V_h = [ar.alloc([128, 24, 128], BF16) for _ in range(2)]
    QT_h = [ar.alloc([128, TOK], BF16) for _ in range(2)]
    E2 = [ar.alloc([128, 512], BF16) for _ in range(2)]
    P2 = [ar.alloc([128, 512], BF16) for _ in range(2)]
    rec = ar.alloc([128, 512])

    def a2_loads(h):
        s = h % 2
        fw.dma("gp", WA[s], wA[h].rearrange("(dk p) c -> p dk c", p=128), w=[("WA", s)])
        fw.dma("sp", maskS[s].rearrange("p a b -> p (a b)"), maskA[h], w=[("mask", s)])

    def a2_proj_groups(h):
        s = h % 2
        pieces = []
        gcount = [0]

        def add_group(nmm, mk_mm, evac):
            per = 4 if nmm == 16 else 16
            pb = gcount[0] % 2
            gcount[0] += 1
            for p0 in range(0, nmm, per):
                last = (p0 + per >= nmm)

                def piece(p0=p0, pb=pb):
                    def mm(e):
                        ins = None
                        for q in range(p0, p0 + per):
                            ins = mk_mm(e, q, pb)
                        return ins
                    fw.op("pe", mm, r=[("WA", s)] + win_ids, w=[("ps", pb)])
                pieces.append((piece, (lambda pb=pb: evac(pb)) if last else None))

        for c6 in range(6):
            add_group(16,
                      lambda e, dk, pb, c6=c6: e.matmul(bank(pb), WA[s][:, dk, 128:256], hwin[:, dk, c6 * 512:(c6 + 1) * 512],
                                                        start=(dk == 0), stop=(dk == 15)),
                      lambda pb, c6=c6: fw.op("act", lambda e: e.activation(out=KT_h[s][:, c6 * 512:(c6 + 1) * 512], in_=bank(pb),
                                                                             func=AF.Copy), r=[("ps", pb)], w=[("KTh", s)]))
        for c2 in range(2):
            add_group(16,
                      lambda e, dk, pb, c2=c2: e.matmul(bank(pb), WA[s][:, dk, 0:128],
                                                        hwin[:, dk, 1024 + c2 * 512:1024 + (c2 + 1) * 512],
                                                        start=(dk == 0), stop=(dk == 15)),
                      lambda pb, c2=c2: fw.op("act", lambda e: e.activation(out=QT_h[s][:, c2 * 512:(c2 + 1) * 512], in_=bank(pb),
                                                                             func=AF.Copy), r=[("ps", pb)], w=[("QTh", s)]))
        for gg in range(6):
            add_group(64,
                      lambda e, q, pb, gg=gg: e.matmul(bank(pb)[:, (q // 16) * 128:(q // 16 + 1) * 128],
                                                       hwin[:, q % 16, (4 * gg + q // 16) * 128:(4 * gg + q // 16 + 1) * 128],
                                                       WA[s][:, q % 16, 256:384], start=(q % 16 == 0), stop=(q % 16 == 15)),
                      lambda pb, gg=gg: fw.op("dve", lambda e: e.tensor_copy(
                          V_h[s][:, 4 * gg:4 * gg + 4, :].rearrange("p a b -> p (a b)"), bank(pb)),
                          r=[("ps", pb)], w=[("Vh", s)]))
        return pieces

    a2_loads(0)
    for (pc, ev) in a2_proj_groups(0):
        pc()
        if ev is not None:
            ev()
    for h in range(8):
        s = h % 2
        if h + 1 < 8:
            a2_loads(h + 1)
            pend = a2_proj_groups(h + 1)
        else:
            pend = []
        cnt_t = [0]
        npend = len(pend)
        late = []

        def hook(i, pend=pend, cnt_t=cnt_t, npend=npend, late=late):
            cnt_t[0] += 1
            for (d, ev) in [x for x in late if x[0] <= cnt_t[0]]:
                ev()
            late[:] = [x for x in late if x[0] > cnt_t[0]]
            want = (npend * cnt_t[0] + 39) // 40
            while pend and (npend - len(pend)) < want:
                pc, ev = pend.pop(0)
                pc()
                if ev is not None:
                    late.append((cnt_t[0] + 2, ev))
        for qc in range(2):
            kts = list(range(4 * qc, 4 * qc + 20))
            qt0 = 8 + 4 * qc
            pso, psd = (4, 5) if qc == 0 else (6, 7)

            def mask_fn(i, kts=kts, qt0=qt0, s=s):
                m0 = 11 - (kts[i] - qt0)
                return maskS[s][:, m0:m0 + 4, :].rearrange("p a b -> p (a b)")
            attention(lambda kt, s=s: KT_h[s][:, kt * 128:(kt + 1) * 128], lambda kt, s=s: V_h[s][:, kt, :],
                      QT_h[s][:, qc * 512:(qc + 1) * 512], kts, ("KTh", s), ("Vh", s), ("QTh", s), mask_fn, ("mask", s),
                      E2, P2, rec, mixA[:, h, qc * 512:(qc + 1) * 512], ("mixT", h), pso, psd, "A",
                      sbanks=(2, 3), hook=hook)
        while pend:
            pc, ev = pend.pop(0)
            pc()
            if ev is not None:
                late.append((0, ev))
        for (d, ev) in late:
            ev()
        late[:] = []
    fw.barrier()

    ar.p = P0
    KT_B = ar.alloc([128, 2, S], BF16)
    V_B = ar.alloc([128, 64, 256], BF16)
    PB = ar.p
    Wst = ar.alloc([128, 16, 512])
    ar.p = PB
    xst = [ar.alloc([128, 16, 256]) for _ in range(2)]
    sq = ar.alloc([128, 16, 256], BF16)
    xb = [ar.alloc([128, 16, 256], BF16) for _ in range(2)]
    Wkv = ar.alloc([128, 16, 512], BF16)
    rope4 = ar.alloc([128, 4, 128])
    raw = [ar.alloc([128, 1024]) for _ in range(2)]
    kgt = ar.alloc([128, 1024])
    sqt = ar.alloc([128, 1024])
    tA = ar.alloc([128, 512])
    tB = ar.alloc([128, 512])
    ob = ar.alloc([128, 1024])
    kr = ar.alloc([128, 1024], BF16)
    rtk = [ar.alloc([128, 2]) for _ in range(2)]
    rsk = [ar.alloc([128, 2]) for _ in range(2)]
    ssq8 = ar.alloc([128, 8])
    rt8 = ar.alloc([128, 8])
    rs8 = ar.alloc([128, 8])
    tC = sqt[:, 0:512]
    tD = sqt[:, 512:1024]
    fw.dma("sp", Wst, wkvB.rearrange("(dk p) c -> p dk c", p=128), w=["Wst"])

    def foldw(e):
        ins = None
        for dk in range(16):
            ins = e.tensor_scalar(Wkv[:, dk, :], Wst[:, dk, :], g1T[:, dk:dk + 1], None, ALU.mult)
        return ins
    fw.op("dve", foldw, r=["Wst", "g1T"], w=["Wkv"])
    SSQB = (0, 6)
    sched = []
    step = [0]

    def run_due():
        due = [f for (d, f) in sched if d <= step[0]]
        rest = [(d, f) for (d, f) in sched if d > step[0]]
        sched[:] = rest
        for f in due:
            f()

    def b1_head(C, half):
        ck = 2 * C + half
        s = ck % 2
        fw.dma("sp", xst[s].rearrange("p a b -> p (a b)"), xT[ck], w=[("xst", s)] + (["Wst"] if ck < 2 else []))
        fw.op("act", lambda e, s=s: e.activation(out=sq, in_=xst[s], func=AF.Square), r=[("xst", s)], w=["sq"])
        fw.op("dve", lambda e, s=s: e.tensor_copy(xb[s], xst[s]), r=[("xst", s)], w=[("xb", s)])

        def mms(e, half=half):
            ins = None
            for sub in range(2):
                for dk in range(16):
                    ins = e.matmul(bank(SSQB[half])[:, sub:sub + 1], sq[:, dk, sub * 128:(sub + 1) * 128],
                                   ones_bf[:, 0:1], start=(dk == 0), stop=(dk == 15))
            return ins
        fw.op("pe", mms, r=["sq", "ones"], w=[("ps", SSQB[half])])
        for sub in range(2):
            bb = 1 + 2 * half + sub

            def mm(e, s=s, sub=sub, bb=bb):
                ins = None
                for dk in range(16):
                    ins = e.matmul(bank(bb), xb[s][:, dk, sub * 128:(sub + 1) * 128], Wkv[:, dk, :],
                                   start=(dk == 0), stop=(dk == 15))
                return ins
            fw.op("pe", mm, r=[("xb", s), "Wkv"], w=[("ps", bb)])

    def b1_tail(C, half):
        r_ = C % 2
        fw.op("act", lambda e, half=half: e.activation(out=rtk[half], in_=bank(SSQB[half])[:, 0:2], func=AF.Sqrt,
                                                       bias=EPS, scale=1.0 / D), r=[("ps", SSQB[half])], w=[("rtk", half)])
        fw.op("dve", lambda e, half=half: e.reciprocal(rsk[half], rtk[half]), r=[("rtk", half)], w=[("rsk", half)])
        for sub in range(2):
            j = 2 * half + sub
            bb = 1 + j
            fw.op("act", lambda e, bb=bb, C=C, j=j, half=half, sub=sub: e.activation(
                out=V_B[:, 4 * C + j, :], in_=bank(bb)[:, 256:512], func=AF.Copy, scale=rsk[half][:, sub:sub + 1]),
                r=[("ps", bb), ("rsk", half)], w=[("VB", C, j)])
            fw.op("act", lambda e, bb=bb, j=j, half=half, sub=sub, r_=r_: e.activation(
                out=raw[r_][:, j * 256:(j + 1) * 256], in_=bank(bb)[:, 0:256], func=AF.Copy,
                scale=rsk[half][:, sub:sub + 1]), r=[("ps", bb), ("rsk", half)], w=[("raw", r_, j)])

    def b1_postA(C):
        r_ = C % 2
        fw.dma("sp", rope4, ropeT[:, 4 * C:4 * C + 4, :], w=["rope4"])
        rw = raw[r_]
        raw_ids = [("raw", r_, j) for j in range(4)]
        rw3 = rw.rearrange("p (a d) -> p a d", a=8)
        kg3 = kgt.rearrange("p (a d) -> p a d", a=8)
        sq3 = sqt.rearrange("p (a d) -> p a d", a=8)
        kg5 = kgt.rearrange("p (j h i two) -> p j h i two", j=4, h=2, two=2)
        ob5 = ob.rearrange("p (j h i two) -> p j h i two", j=4, h=2, two=2)
        ob3 = ob.rearrange("p (a d) -> p a d", a=8)
        kr3 = kr.rearrange("p (a d) -> p a d", a=8)
        x0, x1 = kg5[:, :, :, :, 0], kg5[:, :, :, :, 1]
        cosb = rope4[:, :, 0:64].unsqueeze(2).to_broadcast([128, 4, 2, 64])
        sinb = rope4[:, :, 64:128].unsqueeze(2).to_broadcast([128, 4, 2, 64])
        v4 = lambda t: t.rearrange("p (j h i) -> p j h i", j=4, h=2)
        fw.op("dve", lambda e: e.tensor_tensor(kg3, rw3, kg_b.unsqueeze(1).to_broadcast([128, 8, 128]), ALU.mult),
              r=raw_ids + ["kg"], w=["kgt"])
        fw.op("dve", lambda e: e.tensor_tensor(sqt, rw, rw, ALU.mult), r=raw_ids, w=["sqt", "sqt2"])
        fw.op("dve", lambda e: e.tensor_tensor(v4(tA), x0, cosb, ALU.mult), r=["kgt", "rope4"], w=["tA"])
        fw.op("dve", lambda e: e.tensor_reduce(ssq8, sq3, AX.X, ALU.add), r=["sqt", "sqt2"], w=["ssq8"])
        fw.op("dve", lambda e: e.tensor_tensor(v4(tB), x1, sinb, ALU.mult), r=["kgt", "rope4"], w=["tB"])
        fw.op("dve", lambda e: e.tensor_tensor(v4(tC), x0, sinb, ALU.mult), r=["kgt", "rope4"], w=["sqt"])
        fw.op("dve", lambda e: e.tensor_tensor(v4(tD), x1, cosb, ALU.mult), r=["kgt", "rope4"], w=["sqt2"])
        fw.op("dve", lambda e: e.tensor_tensor(ob5[:, :, :, :, 0], v4(tA), v4(tB), ALU.subtract), r=["tA", "tB"], w=["ob0"])
        fw.op("dve", lambda e: e.tensor_tensor(ob5[:, :, :, :, 1], v4(tC), v4(tD), ALU.add), r=["sqt", "sqt2"], w=["ob1"])

    def b1_postB(C):
        ob3 = ob.rearrange("p (a d) -> p a d", a=8)
        kr3 = kr.rearrange("p (a d) -> p a d", a=8)
        fw.op("act", lambda e: e.activation(out=rt8, in_=ssq8, func=AF.Sqrt, bias=EPS, scale=1.0 / 128), r=["ssq8"], w=["rt8"])
        fw.op("dve", lambda e: e.reciprocal(rs8, rt8), r=["rt8"], w=["rs8"])
        fw.op("dve", lambda e: e.tensor_tensor(kr3, ob3, rs8.unsqueeze(2).to_broadcast([128, 8, 128]), ALU.mult),
              r=["ob0", "ob1", "rs8"], w=["kr"])
        def trk(e):
            ins = None
            for j in range(4):
                for hh in range(2):
                    ins = e.transpose(bank_bf(5)[:, (hh * 4 + j) * 128:(hh * 4 + j + 1) * 128],
                                      kr[:, (j * 2 + hh) * 128:(j * 2 + hh + 1) * 128], ident_bf)
            return ins
        fw.op("pe", trk, r=["kr", "ident"], w=[("ps", 5)])
        for hh in range(2):
            fw.op("dve", lambda e, hh=hh, C=C: e.tensor_copy(KT_B[:, hh, C * 512:(C + 1) * 512],
                                                             bank_bf(5)[:, hh * 512:(hh + 1) * 512]),
                  r=[("ps", 5)], w=[("KTB", C, hh)])

    for C in range(S // 512):
        for half in range(2):
            b1_head(C, half)
            run_due()
            sched.append((step[0] + 1, lambda C=C, half=half: b1_tail(C, half)))
            if half == 1:
                sched.append((step[0] + 1, lambda C=C: b1_postA(C)))
                sched.append((step[0] + 2, lambda C=C: b1_postB(C)))
            step[0] += 1
    step[0] += 10
    run_due()
    fw.barrier()

    ar.p = PB
    mixB = ar.alloc([128, 8, TOK], BF16)
    E2 = [ar.alloc([128, 512], BF16) for _ in range(4)]
    rec = ar.alloc([128, 512])
    cnt = 0
    for h in range(8):
        kv = h // 4
        for qc in range(2):
            pso, psd = (4, 5) if cnt % 2 == 0 else (6, 7)
            cnt += 1
            attention(lambda kt, kv=kv: KT_B[:, kv, kt * 128:(kt + 1) * 128],
                      lambda kt, kv=kv: V_B[:, kt, kv * 128:(kv + 1) * 128],
                      QT_B[:, h, qc * 512:(qc + 1) * 512], list(range(64)), "KTB", "VB", ("QTB", h), None, None,
                      E2, None, rec, mixB[:, h, qc * 512:(qc + 1) * 512], ("mixT", 8 + h), pso, psd, "B", sbanks=(0, 1, 2, 3))
    fw.barrier()

    ar.p = P0
    acc = ar.alloc([128, 8, D])
    assert ar.p <= PB
    PO = PB + 4096
    ar.p = PO
    Wo = [ar.alloc([128, 16, 512], BF16) for _ in range(2)]
    for tt in range(8):
        fw.dma("sp", acc[:, tt, :], x_own[tt * 128:(tt + 1) * 128, :], w=[("acc", tt)], key=("accld", tt))
    w_out3 = w_out.rearrange("(m p) c -> p m c", p=128)
    mix_ids = [("mixT", i) for i in range(16)]
    k = 0
    for oc in range(4):
        s = oc % 2
        fw.dma("gp", Wo[s], w_out3[:, :, oc * 512:(oc + 1) * 512], w=[("Wo", s)])
        for tt in range(8):
            b = k % 4
            k += 1

            def mm(e, tt=tt, s=s, b=b):
                ins = None
                for m in range(16):
                    ins = e.matmul(bank(b), (mixA[:, m, tt * 128:(tt + 1) * 128] if m < 8 else mixB[:, m - 8, tt * 128:(tt + 1) * 128]), Wo[s][:, m, :],
                                   start=(m == 0), stop=(m == 15))
                return ins
            fw.op("pe", mm, r=[("Wo", s)] + mix_ids, w=[("ps", b)])
            fw.op("dve", lambda e, tt=tt, oc=oc, b=b: e.tensor_tensor(
                acc[:, tt, oc * 512:(oc + 1) * 512], bank(b), acc[:, tt, oc * 512:(oc + 1) * 512], ALU.add),
                r=[("ps", b), ("acc", tt)], w=[("acc", tt)])
    fw.barrier()

    ar.p = MIX0
    h2T = ar.alloc([128, 16, TOK], BF16)
    PH = ar.p
    ar.p = PO
    g2_b = ar.alloc([128, D])
    junk = ar.alloc([128, D], BF16)
    h2 = [ar.alloc([128, D], BF16) for _ in range(2)]
    ssqn = ar.alloc([128, 8])
    rtn = ar.alloc([128, 8])
    rsn = ar.alloc([128, 8])
    fw.dma("sp", g2_b, g2_d, w=["g2"])
    acc_ids = [("acc", t_) for t_ in range(8)]
    fw.dma("sp", x1_scr, acc.rearrange("p a b -> p (a b)"), r=acc_ids, w=["x1scr"])
    fw.op("dve", lambda e: e.memset(ssqn, 0.0), w=["ssqn"])
    for tt in range(8):
        fw.op("act", lambda e, tt=tt: e.activation(out=junk, in_=acc[:, tt, :], func=AF.Square,
                                                   accum_out=ssqn[:, tt:tt + 1]), r=[("acc", tt), "ssqn"], w=["junk", ("ssqn", tt)])
        fw.op("act", lambda e, tt=tt: e.activation(out=rtn[:, tt:tt + 1], in_=ssqn[:, tt:tt + 1], func=AF.Sqrt,
                                                   bias=EPS, scale=1.0 / D), r=[("ssqn", tt)], w=[("rtn", tt)])
    for tt in range(8):
        fw.op("dve", lambda e, tt=tt: e.reciprocal(rsn[:, tt:tt + 1], rtn[:, tt:tt + 1]), r=[("rtn", tt)], w=[("rsn", tt)])
        s = tt % 2
        fw.op("dve", lambda e, tt=tt, s=s: e.scalar_tensor_tensor(out=h2[s], in0=acc[:, tt, :], scalar=rsn[:, tt:tt + 1],
                                                                  in1=g2_b, op0=ALU.mult, op1=ALU.mult),
              r=[("acc", tt), ("rsn", tt), "g2"], w=[("h2", s)])
        for g in range(4):
            b = g % 2

            def tr(e, g=g, s=s, b=b):
                ins = None
                for j in range(4):
                    dk = 4 * g + j
                    ins = e.transpose(bank_bf(b)[:, j * 128:(j + 1) * 128], h2[s][:, dk * 128:(dk + 1) * 128], ident_bf)
                return ins
            fw.op("pe", tr, r=[("h2", s), "ident"], w=[("ps", b)])
            fw.op("act", lambda e, g=g, tt=tt, b=b: e.activation(
                out=h2T[:, 4 * g:4 * g + 4, tt * 128:(tt + 1) * 128],
                in_=bank_bf(b)[:, 0:512].rearrange("p (a t) -> p a t", a=4), func=AF.Copy),
                r=[("ps", b)], w=["h2T"])
    fw.barrier()

    ar.p = PH
    qT = ar.alloc([128, 16, TOK], BF16)
    subkT = ar.alloc([128, 16, 128], BF16)
    PQ = ar.p
    Wq = [ar.alloc([128, 16, 512], BF16) for _ in range(2)]
    fw.dma("gp", subkT, subkT_d, w=["subk"])
    wq3 = wq.rearrange("(dk p) c -> p dk c", p=128)
    k = 0
    for piece in range(4):
        s = piece % 2
        fw.dma("gp", Wq[s], wq3[:, :, piece * 512:(piece + 1) * 512], w=[("Wq", s)])
        for bb in range(4):
            blk = piece * 4 + bb
            for half in range(2):
                b = k % 4
                k += 1

                def mm(e, bb=bb, half=half, s=s, b=b):
                    ins = None
                    for dk in range(16):
                        ins = e.matmul(bank(b), Wq[s][:, dk, bb * 128:(bb + 1) * 128], h2T[:, dk, half * 512:(half + 1) * 512],
                                       start=(dk == 0), stop=(dk == 15))
                    return ins
                fw.op("pe", mm, r=[("Wq", s), "h2T"], w=[("ps", b)])
                fw.op("act", lambda e, blk=blk, half=half, b=b: e.activation(
                    out=qT[:, blk, half * 512:(half + 1) * 512], in_=bank(b), func=AF.Copy), r=[("ps", b)], w=["qT"])
    fw.barrier()

    ar.p = PQ
    bufX = ar.alloc([128, 2048])
    bufY = ar.alloc([128, 2048])
    top = ar.alloc([128, 256])
    idx = ar.alloc([128, 256], U32)
    idxf = ar.alloc([128, 256])
    best = ar.alloc([128, 128])
    pos = ar.alloc([128, 128], U32)
    pa = ar.alloc([128, 128], U32)
    pb = ar.alloc([128, 128], U32)
    paf = ar.alloc([128, 128])
    pbf = ar.alloc([128, 128])
    bm = ar.alloc([128, 128])
    ex = ar.alloc([128, 128])
    Zs = ar.alloc([128, 8])
    rZ = ar.alloc([128, 8])
    R3 = ar.alloc([128, 3, 128], BF16)
    RT = ar.alloc([128, 3, 128])
    NT = 16
    A1t = [ar.alloc([128, NT, 128]) for _ in range(2)]
    A1 = [ar.alloc([128, NT, 128], BF16) for _ in range(2)]
    A2 = [ar.alloc([128, NT, 128], BF16) for _ in range(2)]
    Gs = [ar.alloc([128, 128, 128], BF16) for _ in range(2)]
    XS = [("X", i) for i in range(16)]
    YS = [("Y", i) for i in range(16)]
    sc3 = bufX.rearrange("p (s n) -> p s n", s=16)
    wk3 = bufY.rearrange("p (s n) -> p s n", s=16)
    top3 = top.rearrange("p (s k) -> p s k", s=16)
    idx3 = idx.rearrange("p (s k) -> p s k", s=16)
    top4 = top.rearrange("p (h c k) -> p h c k", h=8, c=2)
    idxf4 = idxf.rearrange("p (h c k) -> p h c k", h=8, c=2)
    cand3 = bufX.rearrange("p (h n) -> p h n", h=8)
    cand4 = bufX.rearrange("p (h a b) -> p h a b", h=8, a=16)
    cw3 = bufY.rearrange("p (h n) -> p h n", h=8)
    best3 = best.rearrange("p (h k) -> p h k", h=8)
    pos3 = pos.rearrange("p (h k) -> p h k", h=8)
    bm3 = bm.rearrange("p (h k) -> p h k", h=8)
    ex3 = ex.rearrange("p (h k) -> p h k", h=8)
    selY = bufY.rearrange("p (h k a) -> p h k a", h=8, k=16)
    selX = bufX.rearrange("p (h k a) -> p h k a", h=8, k=16)
    iota16b = iota[:, 0:16].unsqueeze(1).unsqueeze(1).to_broadcast([128, 8, 16, 16])
    all_top = [("top", sg, j) for sg in range(16) for j in range(2)]
    all_idx = [("idx", sg, j) for sg in range(16) for j in range(2)]
    all_best = [("best", hh, j) for hh in range(8) for j in range(2)]
    all_pos = [("pos", hh, j) for hh in range(8) for j in range(2)]

    def topk_stages(tt):
        def st0():
            def mm(e):
                ins = None
                for blk in range(16):
                    ins = e.matmul(psA[:, blk * 128:(blk + 1) * 128], qT[:, blk, tt * 128:(tt + 1) * 128], subkT[:, blk, :],
                                   start=True, stop=True)
                return ins
            fw.op("pe", mm, r=["qT", "subk"], w=[("ps", 0), ("ps", 1), ("ps", 2), ("ps", 3)])
            fw.op("act", lambda e: e.activation(out=bufX, in_=psA[:, :], func=AF.Copy),
                  r=[("ps", 0), ("ps", 1), ("ps", 2), ("ps", 3)], w=XS)
            for sg in range(16):
                fw.op("dve", lambda e, sg=sg: e.max(out=top3[:, sg, 0:8], in_=sc3[:, sg, :]), r=[("X", sg)], w=[("top", sg, 0)])

        def st1():
            for sg in range(16):
                fw.op("dve", lambda e, sg=sg: e.max_index(out=idx3[:, sg, 0:8], in_max=top3[:, sg, 0:8], in_values=sc3[:, sg, :]),
                      r=[("X", sg), ("top", sg, 0)], w=[("idx", sg, 0)])
                fw.op("dve", lambda e, sg=sg: e.match_replace(out=wk3[:, sg, :], in_to_replace=top3[:, sg, 0:8],
                                                              in_values=sc3[:, sg, :], imm_value=NEG),
                      r=[("X", sg), ("top", sg, 0)], w=[("Y", sg)])

        def st2():
            for sg in range(16):
                fw.op("dve", lambda e, sg=sg: e.max(out=top3[:, sg, 8:16], in_=wk3[:, sg, :]), r=[("Y", sg)], w=[("top", sg, 1)])

        def st3():
            for sg in range(16):
                fw.op("dve", lambda e, sg=sg: e.max_index(out=idx3[:, sg, 8:16], in_max=top3[:, sg, 8:16], in_values=wk3[:, sg, :]),
                      r=[("Y", sg), ("top", sg, 1)], w=[("idx", sg, 1)])
            fw.op("dve", lambda e: e.tensor_copy(idxf, idx), r=all_idx, w=["idxf"])

        def st4():
            fw.op("dve", lambda e: e.tensor_tensor(cand4, top4[:, :, 0, :].unsqueeze(3).to_broadcast([128, 8, 16, 16]),
                                                   top4[:, :, 1, :].unsqueeze(2).to_broadcast([128, 8, 16, 16]), ALU.add),
                  r=all_top, w=XS)
            for hh in range(8):
                fw.op("dve", lambda e, hh=hh: e.max(out=best3[:, hh, 0:8], in_=cand3[:, hh, :]),
                      r=[("X", 2 * hh), ("X", 2 * hh + 1)], w=[("best", hh, 0)])

        def st5():
            for hh in range(8):
                fw.op("dve", lambda e, hh=hh: e.max_index(out=pos3[:, hh, 0:8], in_max=best3[:, hh, 0:8], in_values=cand3[:, hh, :]),
                      r=[("X", 2 * hh), ("X", 2 * hh + 1), ("best", hh, 0)], w=[("pos", hh, 0)])
                fw.op("dve", lambda e, hh=hh: e.match_replace(out=cw3[:, hh, :], in_to_replace=best3[:, hh, 0:8],
                                                              in_values=cand3[:, hh, :], imm_value=NEG),
                      r=[("X", 2 * hh), ("X", 2 * hh + 1), ("best", hh, 0)], w=[("Y", 2 * hh), ("Y", 2 * hh + 1)])
            for hh in range(8):
                fw.op("dve", lambda e, hh=hh: e.max(out=best3[:, hh, 8:16], in_=cw3[:, hh, :]),
                      r=[("Y", 2 * hh), ("Y", 2 * hh + 1)], w=[("best", hh, 1)])

        def st6():
            for hh in range(8):
                fw.op("dve", lambda e, hh=hh: e.max_index(out=pos3[:, hh, 8:16], in_max=best3[:, hh, 8:16], in_values=cw3[:, hh, :]),
                      r=[("Y", 2 * hh), ("Y", 2 * hh + 1), ("best", hh, 1)], w=[("pos", hh, 1)])
            fw.op("dve", lambda e: e.tensor_tensor(bm3, best3, best3[:, :, 0:1].to_broadcast([128, 8, 16]), ALU.subtract),
                  r=all_best, w=["bm"])
            fw.op("act", lambda e: e.activation(out=ex, in_=bm, func=AF.Exp), r=["bm"], w=["ex"])
            fw.op("dve", lambda e: e.tensor_single_scalar(pa, pos, 4, ALU.logical_shift_right), r=all_pos, w=["pa"])
            fw.op("dve", lambda e: e.tensor_single_scalar(pb, pos, 15, ALU.bitwise_and), r=all_pos, w=["pb"])
            fw.op("dve", lambda e: e.tensor_copy(paf, pa), r=["pa"], w=["paf"])
            fw.op("dve", lambda e: e.tensor_copy(pbf, pb), r=["pb"], w=["pbf"])

        def st7():
            paf3 = paf.rearrange("p (h k) -> p h k", h=8)
            pbf3 = pbf.rearrange("p (h k) -> p h k", h=8)
            fw.op("dve", lambda e: e.tensor_tensor(selY, iota16b, paf3.unsqueeze(3).to_broadcast([128, 8, 16, 16]), ALU.is_equal),
                  r=["paf", "iota"], w=YS)
            fw.op("gp", lambda e: e.tensor_tensor(selY, selY, idxf4[:, :, 0, :].unsqueeze(2).to_broadcast([128, 8, 16, 16]), ALU.mult),
                  r=YS + ["idxf"], w=YS)
            fw.op("dve", lambda e: e.tensor_tensor(selX, iota16b, pbf3.unsqueeze(3).to_broadcast([128, 8, 16, 16]), ALU.is_equal),
                  r=["pbf", "iota"], w=XS)
            fw.op("gp", lambda e: e.tensor_tensor(selX, selX, idxf4[:, :, 1, :].unsqueeze(2).to_broadcast([128, 8, 16, 16]), ALU.mult),
                  r=XS + ["idxf"], w=XS)
            fw.op("dve", lambda e: e.tensor_reduce(Zs, ex3, AX.X, ALU.add), r=["ex"], w=["Zs"])
            fw.op("dve", lambda e: e.reciprocal(rZ, Zs), r=["Zs"], w=["rZ"])
            fw.op("dve", lambda e: e.tensor_tensor(R3[:, 2, :].rearrange("p (h k) -> p h k", h=8), ex3,
                                                   rZ.unsqueeze(2).to_broadcast([128, 8, 16]), ALU.mult),
                  r=["ex", "rZ"], w=["R3g"])
            fw.op("dve", lambda e: e.tensor_reduce(R3[:, 0, :].rearrange("p (h k) -> p h k", h=8), selY, AX.X, ALU.add),
                  r=YS, w=[("R3", 0)])
            fw.op("dve", lambda e: e.tensor_reduce(R3[:, 1, :].rearrange("p (h k) -> p h k", h=8), selX, AX.X, ALU.add),
                  r=XS, w=[("R3", 1)])
        return [st0, st1, st2, st3, st4, st5, st6, st7]

    def onehot_prelude(tt):
        def tr(e):
            ins = None
            for j in range(3):
                ins = e.transpose(bank_bf(4)[:, j * 128:(j + 1) * 128], R3[:, j, :], ident_bf)
            return ins
        fw.op("pe", tr, r=[("R3", 0), ("R3", 1), "R3g", "ident"], w=[("ps", 4)])
        fw.op("act", lambda e: e.activation(out=RT.rearrange("p a b -> p (a b)"), in_=bank_bf(4)[:, 0:384], func=AF.Copy),
              r=[("ps", 4)], w=["RT"])

    iotab = iota.unsqueeze(1).to_broadcast([128, NT, 128])

    def onehot_group(tt, g):
        gs = Gs[tt % 2]
        s = g % 2
        t0 = g * NT
        fw.op("dve", lambda e: e.tensor_tensor(A2[s], iotab, RT[:, 1, t0:t0 + NT].unsqueeze(2).to_broadcast([128, NT, 128]),
                                               ALU.is_equal), r=["RT", "iota"], w=[("A2", s)])
        fw.op("dve", lambda e: e.tensor_tensor(A1t[s], iotab, RT[:, 0, t0:t0 + NT].unsqueeze(2).to_broadcast([128, NT, 128]),
                                               ALU.is_equal), r=["RT", "iota"], w=[("A1t", s)])
        def gate(e):
            ins = None
            for j in range(NT):
                ins = e.activation(out=A1[s][:, j, :], in_=A1t[s][:, j, :], func=AF.Copy, scale=RT[:, 2, t0 + j:t0 + j + 1])
            return ins
        if g % 3 == 2:
            fw.op("gp", lambda e: e.tensor_tensor(A1[s], A1t[s], RT[:, 2, t0:t0 + NT].unsqueeze(2).to_broadcast([128, NT, 128]),
                                                  ALU.mult), r=["RT", ("A1t", s)], w=[("A1", s)])
        else:
            fw.op("act", gate, r=["RT", ("A1t", s)], w=[("A1", s)])
        for q4 in range(NT // 4):
            b = 5 + (q4 % 2)

            def gm(e, q4=q4, b=b):
                ins = None
                for j in range(4):
                    ins = e.matmul(bank(b)[:, j * 128:(j + 1) * 128], A1[s][:, 4 * q4 + j, :], A2[s][:, 4 * q4 + j, :],
                                   start=True, stop=True)
                return ins
            fw.op("pe", gm, r=[("A1", s), ("A2", s)], w=[("ps", b)])
            tq = t0 + 4 * q4
            fw.op("act", lambda e, b=b, tq=tq: e.activation(
                out=gs[:, :, tq:tq + 4], in_=bank(b).rearrange("p (t i) -> p i t", t=4), func=AF.Copy),
                r=[("ps", b)], w=[("Gs", tt % 2, tq)])

    for st in topk_stages(0):
        st()
    for tt in range(8):
        onehot_prelude(tt)
        nxt = topk_stages(tt + 1) if tt + 1 < 8 else []
        for g in range(128 // NT):
            if g < len(nxt):
                nxt[g]()
            onehot_group(tt, g)
        fw.dma("sp", Gd[tt].rearrange("p a b -> p (a b)"), Gs[tt % 2].rearrange("p a b -> p (a b)"),
               r=[("Gs", tt % 2, tq_) for tq_ in range(0, 128, 4)], w=[("Gd", tt)], key=("Gdk", tt % 2))
    fw.barrier()

    ar.p = PO
    GRP = 4
    UT = [ar.alloc([128, 16, 128], BF16) for _ in range(3)]
    Vc = [[ar.alloc([128, D], BF16) for _ in range(GRP)] for _ in range(2)]
    Gc = [ar.alloc([128, TOK], BF16) for _ in range(3)]
    AT = [ar.alloc([128, GRP, TOK], BF16) for _ in range(2)]
    ge = [ar.alloc([128, 512], BF16) for _ in range(2)]
    gf_b = ar.alloc([128, D])
    ssqf = ar.alloc([128, 8])
    rtf = ar.alloc([128, 8])
    rsf = ar.alloc([128, 8])
    junkf = ar.alloc([128, D], BF16)
    fw.dma("sp", gf_b, gf_d, w=["gf"])
    fw.op("dve", lambda e: e.memset(ssqf, 0.0), w=["ssqf"])
    all_gd = [("Gd", t) for t in range(8)]
    kU = 0
    kV = 0
    for grp in range(128 // GRP):
        gsl = grp % 2
        for j in range(GRP):
            c = grp * GRP + j
            s3 = c % 3
            fw.dma("gp", UT[s3].rearrange("p a b -> p (a b)"), UT_l[c], w=[("UT", s3)])
            fw.dma("gp", Vc[gsl][j], V_l[c], w=[("Vc", gsl, j)])
            fw.dma("sp", Gc[s3].rearrange("p (a t) -> p a t", a=8), Gd[:, :, c, :].rearrange("a p t -> p a t"),
                   r=all_gd, w=[("Gc", s3)])
            for half in range(2):
                b = kU % 2
                kU += 1

                def mm(e, s3=s3, half=half, b=b):
                    ins = None
                    for dk in range(16):
                        ins = e.matmul(bank(b), UT[s3][:, dk, :], h2T[:, dk, half * 512:(half + 1) * 512],
                                       start=(dk == 0), stop=(dk == 15))
                    return ins
                fw.op("pe", mm, r=[("UT", s3), "h2T"], w=[("ps", b)])
                fw.op("act", lambda e, b=b: e.activation(out=ge[b], in_=bank(b), func=AF.Gelu_apprx_tanh), r=[("ps", b)], w=[("ge", b)])
                fw.op("dve", lambda e, b=b, gsl=gsl, j=j, half=half, s3=s3: e.tensor_tensor(
                    AT[gsl][:, j, half * 512:(half + 1) * 512], ge[b], Gc[s3][:, half * 512:(half + 1) * 512], ALU.mult),
                    r=[("ge", b), ("Gc", s3)], w=[("AT", gsl, j)])
        if grp == 0:
            fw.dma("sp", acc.rearrange("p a b -> p (a b)"), x1_scr, r=["x1scr"],
                   w=["acc"] + [("acc", t_, o_) for t_ in range(8) for o_ in range(4)], key="accld")
        for tt in range(8):
            for oc in range(4):
                b = 4 + (kV % 4)
                kV += 1

                def vm(e, gsl=gsl, tt=tt, oc=oc, b=b):
                    ins = None
                    for j in range(GRP):
                        ins = e.matmul(bank(b), AT[gsl][:, j, tt * 128:(tt + 1) * 128], Vc[gsl][j][:, oc * 512:(oc + 1) * 512],
                                       start=(j == 0), stop=(j == GRP - 1))
                    return ins
                fw.op("pe", vm, r=[("AT", gsl, j) for j in range(GRP)] + [("Vc", gsl, j) for j in range(GRP)], w=[("ps", b)])
                fw.op("dve", lambda e, tt=tt, oc=oc, b=b: e.tensor_tensor(
                    acc[:, tt, oc * 512:(oc + 1) * 512], bank(b), acc[:, tt, oc * 512:(oc + 1) * 512], ALU.add),
                    r=[("ps", b), ("acc", tt, oc)], w=[("acc", tt, oc)], extra=None)
    for tt in range(8):
        a_ids = [("acc", tt, o_) for o_ in range(4)]
        fw.op("act", lambda e, tt=tt: e.activation(out=junkf, in_=acc[:, tt, :], func=AF.Square,
                                                   accum_out=ssqf[:, tt:tt + 1]),
              r=a_ids + ["ssqf"], w=["junkf", ("ssqf", tt)])
        fw.op("act", lambda e, tt=tt: e.activation(out=rtf[:, tt:tt + 1], in_=ssqf[:, tt:tt + 1], func=AF.Sqrt,
                                                   bias=EPS, scale=1.0 / D), r=[("ssqf", tt)], w=[("rtf", tt)])
        fw.op("dve", lambda e, tt=tt: e.reciprocal(rsf[:, tt:tt + 1], rtf[:, tt:tt + 1]), r=[("rtf", tt)], w=[("rsf", tt)])
        fw.op("dve", lambda e, tt=tt: e.scalar_tensor_tensor(out=acc[:, tt, :], in0=acc[:, tt, :], scalar=rsf[:, tt:tt + 1],
                                                             in1=gf_b, op0=ALU.mult, op1=ALU.mult),
              r=a_ids + [("rsf", tt), "gf"], w=a_ids)
        fw.dma("sp", out_d[tt * 128:(tt + 1) * 128, :], acc[:, tt, :], r=a_ids, w=[("out", tt)], key=("out", tt))

    keys = fw.analyze()
    with ExitStack() as es:
        sems = {}
        for i, k in enumerate(keys):
            sems[k] = es.enter_context(nc.semaphore("s%d" % i))
        block = es.enter_context(nc.Block())
        with nc.allow_low_precision(reason="bf16 matmul operands, exact small ints"):
            fw.emit(nc, block, sems)
    return nc


def _constants():
    half = 64
    inv = (10000.0 ** (-np.arange(0, half, 2, dtype=np.float32) / half)).astype(np.float32)
    row = np.repeat(np.arange(S // 64, dtype=np.float32), 64)
    col = np.tile(np.arange(64, dtype=np.float32), S // 64)
    ang = np.concatenate([row[:, None] * inv, col[:, None] * inv], axis=-1).astype(np.float32)
    rope = np.concatenate([np.cos(ang), np.sin(ang)], axis=-1).astype(np.float32)
    slopes = 2.0 ** (-8.0 * (np.arange(8, dtype=np.float64) + 1.0) / 8)
    kk = np.arange(128)[:, None, None]
    mm_ = np.arange(23)[None, :, None]
    qq = np.arange(128)[None, None, :]
    delta = (11 - mm_) * 128 + kk - qq
    ad = np.abs(delta)
    cnt = (ad <= 64).astype(np.float64) + ((ad <= 256) & (delta % 4 == 0)) + ((ad <= 1024) & (delta % 16 == 0))
    mask = np.stack([cnt * np.exp(-slopes[h] * ad) for h in range(8)], axis=0)
    mask = mask.reshape(8, 128, 23 * 128).astype(np.float32).astype(ml_dtypes.bfloat16)
    iota = np.tile(np.arange(128, dtype=np.float32)[None, :], (128, 1))
    ident = np.eye(128, dtype=np.float32)
    return rope, mask, iota, ident


_NC_CACHE = {}


def kernel(x, norm1_g, w_in, q_norm_g, k_norm_g, w_out, norm2_g, peer_w_query, peer_sub_keys,
           peer_u, peer_v, final_norm_g):
    f32 = np.float32
    x = np.asarray(x, f32)
    w_in0 = np.asarray(w_in, f32)[0]
    rope, mask, iota, ident = _constants()
    xT_full = np.ascontiguousarray(x[0].T)
    wA = np.ascontiguousarray(np.stack([np.concatenate(
        [w_in0[:, h * 128:(h + 1) * 128], w_in0[:, 1024 + h * 128:1024 + (h + 1) * 128],
         w_in0[:, 2048 + h * 128:2048 + (h + 1) * 128]], axis=1) for h in range(8)], axis=0))
    wqB = np.ascontiguousarray(w_in0[:, 3072:4096])
    wkvB = np.ascontiguousarray(w_in0[:, 4096:4608])
    U = np.asarray(peer_u, f32)[0]
    V = np.asarray(peer_v, f32)[0]
    UT_l = np.ascontiguousarray(U.reshape(128, 128, 16, 128).transpose(1, 3, 2, 0)).reshape(128, 128, 16 * 128)
    V_l = np.ascontiguousarray(V.reshape(128, 128, D).transpose(1, 0, 2))
    subkT = np.ascontiguousarray(np.asarray(peer_sub_keys, f32)[0].reshape(16, 128, 128).transpose(2, 0, 1))
    common = {
        "maskA": mask, "g1T": np.ascontiguousarray(np.asarray(norm1_g, f32)[0].reshape(16, 128).T),
        "qg_b": np.ascontiguousarray(np.tile(np.asarray(q_norm_g, f32)[0][None, :], (128, 1))),
        "kg_b": np.ascontiguousarray(np.tile(np.asarray(k_norm_g, f32)[0][None, :], (128, 1))),
        "g2_b": np.ascontiguousarray(np.tile(np.asarray(norm2_g, f32)[0][None, :], (128, 1))),
        "gf_b": np.ascontiguousarray(np.tile(np.asarray(final_norm_g, f32)[None, :], (128, 1))),
        "iota": iota, "ident": ident, "wA": wA, "wqB": wqB, "wkvB": wkvB,
        "w_out": np.ascontiguousarray(np.asarray(w_out, f32)[0]),
        "wq": np.ascontiguousarray(np.asarray(peer_w_query, f32)[0]),
        "subkT": subkT, "UT_l": UT_l, "V_l": V_l,
    }
    in_maps = []
    for c in range(NCORES):
        shift = 1024 * c - 1024
        xr = np.roll(xT_full, -shift, axis=1).reshape(16, 128, S // 256, 256)
        xr = np.ascontiguousarray(xr.transpose(2, 1, 0, 3)).reshape(S // 256, 128, 16 * 256)
        rr = np.roll(rope, -shift, axis=0)
        ropeT = np.ascontiguousarray(rr.reshape(64, 128, 128).transpose(1, 0, 2))
        tokpos = shift + np.arange(WIN)
        valid = ((tokpos >= 0) & (tokpos < S)).astype(f32).reshape(24, 128).T
        m = dict(common)
        m.update({"xT": xr, "x_own": np.ascontiguousarray(x[0, 1024 * c:1024 * (c + 1), :]), "ropeT": ropeT,
                  "valid": np.ascontiguousarray(valid)})
        in_maps.append(m)
    if "nc" not in _NC_CACHE:
        _NC_CACHE["nc"] = build_program()
    res = run_bass_kernel_spmd(_NC_CACHE["nc"], in_maps, core_ids=list(range(NCORES)))
    out = np.concatenate([np.asarray(r["out"], f32) for r in res.results], axis=0)
    return out.reshape(1, S, D)
```

```python
import math
from contextlib import ExitStack

import numpy as np
import ml_dtypes
import concourse.bass as bass
import concourse.mybir as mybir
from concourse.bass_utils import run_bass_kernel_spmd

F32 = mybir.dt.float32
BF16 = mybir.dt.bfloat16
U32 = mybir.dt.uint32
AF = mybir.ActivationFunctionType
ALU = mybir.AluOpType
AX = mybir.AxisListType

NCORES = 8
S = 8192
D = 2048
TOK = 1024
WIN = 3072
EPS = 1e-6
SCALE = 128.0 ** -0.5
ARENA = 51800
NEG = -1.0e30


class Fw:
    ENG = ("sp", "gp", "pe", "act", "dve")

    def __init__(self):
        self.ops = []

    def op(self, eng, fn, r=(), w=(), dma=False, key=None, extra=None):
        self.ops.append(dict(eng=eng, fn=fn, r=tuple(r), w=tuple(w), dma=dma, key=key,
                             extra=extra, deps=set(), signal=False))
        return len(self.ops) - 1

    def dma(self, eng, out, in_, r=(), w=(), key=None):
        k = key if key is not None else (w[0] if w else ("dma", len(self.ops)))
        return self.op(eng, lambda e, o=out, i=in_: e.dma_start(out=o, in_=i), r=r, w=w, dma=True, key=k)

    def barrier(self):
        last = {}
        for i, o in enumerate(self.ops):
            last[o["eng"]] = i
            if o["dma"]:
                last[("k", o["key"])] = i
        deps = set(last.values())
        for e in self.ENG:
            self.op(e, None, extra=set(deps))

    def analyze(self):
        lastw, readers = {}, {}
        for i, o in enumerate(self.ops):
            deps = set()
            if o["extra"]:
                deps |= o["extra"]
            for b in o["r"]:
                if b in lastw:
                    deps.add(lastw[b])
            for b in o["w"]:
                if b in lastw:
                    deps.add(lastw[b])
                deps.update(readers.get(b, ()))
            deps.discard(i)
            for b in o["r"]:
                readers.setdefault(b, []).append(i)
            for b in o["w"]:
                lastw[b] = i
                readers[b] = []
            real = set()
            for j in deps:
                p = self.ops[j]
                if p["fn"] is None:
                    continue
                if (not p["dma"]) and p["eng"] == "pe" and o["eng"] == "pe" and not o["dma"]:
                    continue
                real.add(j)
            o["deps"] = real
            for j in real:
                self.ops[j]["signal"] = True
        for o in self.ops:
            if o["dma"]:
                o["signal"] = True
        cnt = {}
        for o in self.ops:
            if o["fn"] is None or not o["signal"]:
                continue
            k = ("k", o["key"]) if o["dma"] else ("e", o["eng"])
            cnt[k] = cnt.get(k, 0) + (16 if o["dma"] else 1)
            o["sig"] = (k, cnt[k])
        self.final = dict(cnt)
        return sorted(cnt.keys(), key=str)

    def emit(self, nc, block, sems):
        engs = {"sp": block.sync, "gp": block.gpsimd, "pe": block.tensor, "act": block.scalar,
                "dve": block.vector}
        for en in self.ENG:
            mine = [o for o in self.ops if o["eng"] == en]
            final = self.final if en == "sp" else None

            def body(e, mine=mine, final=final):
                seen = {}
                for o in mine:
                    for j in sorted(o["deps"]):
                        k, v = self.ops[j]["sig"]
                        if seen.get(k, 0) >= v:
                            continue
                        seen[k] = v
                        e.wait_ge(sems[k], v)
                    if o["fn"] is None:
                        continue
                    ins = o["fn"](e)
                    if o["signal"]:
                        k, v = o["sig"]
                        ins.then_inc(sems[k], 16 if o["dma"] else 1)
                if final is not None:
                    for k, v in sorted(final.items(), key=str):
                        if k[0] == "k" and seen.get(k, 0) < v:
                            e.wait_ge(sems[k], v)

            engs[en](body)


def build_program():
    nc = bass.Bass("TRN2", target_bir_lowering=False)
    fw = Fw()

    def din(name, shape, dt=F32):
        return nc.dram_tensor(name, list(shape), dt, kind="ExternalInput").ap()

    xT = din("xT", [S // 256, 128, 16 * 256])
    x_own = din("x_own", [TOK, D])
    ropeT = din("ropeT", [128, 64, 128])
    valid_d = din("valid", [128, 24])
    maskA = din("maskA", [8, 128, 23 * 128], BF16)
    g1T_d = din("g1T", [128, 16])
    qg_d = din("qg_b", [128, 128])
    kg_d = din("kg_b", [128, 128])
    g2_d = din("g2_b", [128, D])
    gf_d = din("gf_b", [128, D])
    iota_d = din("iota", [128, 128])
    ident_d = din("ident", [128, 128])
    wA = din("wA", [8, D, 384])
    wqB = din("wqB", [D, 1024])
    wkvB = din("wkvB", [D, 512])
    w_out = din("w_out", [D, D])
    wq = din("wq", [D, D])
    subkT_d = din("subkT", [128, 16, 128])
    UT_l = din("UT_l", [128, 128, 16 * 128])
    V_l = din("V_l", [128, 128, D])
    out_d = nc.dram_tensor("out", [TOK, D], F32, kind="ExternalOutput").ap()
    Gd = nc.dram_tensor("Gd", [8, 128, 128, 128], BF16, kind="Internal").ap()
    x1_scr = nc.dram_tensor("x1_scr", [128, 8 * D], F32, kind="Internal").ap()

    arena = nc.alloc_sbuf_tensor("arena", [128, ARENA], F32)
    psA = nc.alloc_psum_tensor("psA", [128, 2048], F32)
    psB = nc.alloc_psum_tensor("psB", [128, 2048], F32)

    def bank(i):
        t = psA if i < 4 else psB
        j = i % 4
        return t[:, j * 512:(j + 1) * 512]

    def bank_bf(i):
        return bank(i).bitcast(BF16)

    class Ar:
        def __init__(self):
            self.p = 0

        def alloc(self, shape, dt=F32):
            n = int(np.prod(shape[1:]))
            slots = n if dt in (F32, U32) else (n + 1) // 2
            slots = (slots + 7) // 8 * 8
            off = self.p
            self.p += slots
            assert self.p <= ARENA, ("arena overflow", self.p)
            ap = arena[:, off:off + slots]
            if dt != F32:
                ap = ap.bitcast(dt)
            ap = ap[:, 0:n]
            if len(shape) == 3:
                ap = ap.rearrange("p (a b) -> p a b", a=shape[1])
            elif len(shape) == 4:
                ap = ap.rearrange("p (a b c) -> p a b c", a=shape[1], b=shape[2])
            return ap

    ar = Ar()

    ones_bf = ar.alloc([128, 128], BF16)
    ident_bf = ar.alloc([128, 128], BF16)
    g1T = ar.alloc([128, 16])
    validT = ar.alloc([128, 24])
    kg_b = ar.alloc([128, 128])
    qg_b = ar.alloc([128, 128])
    iota = ar.alloc([128, 128])
    fw.dma("gp", ident_bf, ident_d, w=["ident"])
    fw.dma("sp", g1T, g1T_d, w=["g1T"])
    fw.dma("sp", validT, valid_d, w=["valid"])
    fw.dma("sp", kg_b, kg_d, w=["kg"])
    fw.dma("sp", qg_b, qg_d, w=["qg"])
    fw.dma("sp", iota, iota_d, w=["iota"])
    fw.op("dve", lambda e: e.memset(ones_bf, 1.0), w=["ones"])
    MIX0 = ar.p
    mixA = ar.alloc([128, 8, TOK], BF16)
    QT_B = ar.alloc([128, 8, TOK], BF16)
    P0 = ar.p

    def rms_feature_major(xst, ntok, slot_id, sq, rt, rstd, dst_fn, psb, rid, wid):
        fw.op("act", lambda e: e.activation(out=sq, in_=xst, func=AF.Square), r=[slot_id], w=["sq"])

        def mm(e):
            ins = None
            for dk in range(16):
                ins = e.matmul(bank(psb)[:, 0:ntok], ones_bf, sq[:, dk, :], start=(dk == 0), stop=(dk == 15))
            return ins
        fw.op("pe", mm, r=["sq", "ones"], w=[("ps", psb)])
        fw.op("act", lambda e: e.activation(out=rt, in_=bank(psb)[:, 0:ntok], func=AF.Sqrt, bias=EPS,
                                            scale=1.0 / D), r=[("ps", psb)], w=["rt"])
        fw.op("dve", lambda e: e.reciprocal(rstd, rt), r=["rt"], w=["rstd"])

        def norm(e):
            ins = None
            for dk in range(16):
                ins = e.scalar_tensor_tensor(out=dst_fn(dk), in0=xst[:, dk, :], scalar=g1T[:, dk:dk + 1],
                                             in1=rstd, op0=ALU.mult, op1=ALU.mult)
            return ins
        fw.op("dve", norm, r=[slot_id, "rstd", "g1T"] + list(rid), w=list(wid))

    def qk_post(raw, nh, gain, gain_id, rope_ap, rope_id, tmp, ssq, rt2, rs2, kn, kr, tA, tB, psb, dst_fn, dst_ids, tag):
        n = nh * 128
        raw3 = raw.rearrange("p (h d) -> p h d", h=nh)
        tmp3 = tmp.rearrange("p (h d) -> p h d", h=nh)
        kn3 = kn.rearrange("p (h d) -> p h d", h=nh)
        fw.op("dve", lambda e: e.tensor_tensor(tmp, raw, raw, ALU.mult), r=[tag + "raw"], w=[tag + "tmp"])
        fw.op("dve", lambda e: e.tensor_reduce(ssq, tmp3, AX.X, ALU.add), r=[tag + "tmp"], w=[tag + "ssq"])
        fw.op("act", lambda e: e.activation(out=rt2, in_=ssq, func=AF.Sqrt, bias=EPS, scale=1.0 / 128),
              r=[tag + "ssq"], w=[tag + "rt2"])
        fw.op("dve", lambda e: e.reciprocal(rs2, rt2), r=[tag + "rt2"], w=[tag + "rs2"])
        fw.op("dve", lambda e: e.tensor_tensor(kn3, raw3, rs2.unsqueeze(2).to_broadcast([128, nh, 128]), ALU.mult),
              r=[tag + "raw", tag + "rs2"], w=[tag + "kn"])
        fw.op("dve", lambda e: e.tensor_tensor(kn3, kn3, gain.unsqueeze(1).to_broadcast([128, nh, 128]), ALU.mult),
              r=[tag + "kn", gain_id], w=[tag + "kn"])
        kn4 = kn.rearrange("p (h i two) -> p h i two", h=nh, two=2)
        kr4 = kr.rearrange("p (h i two) -> p h i two", h=nh, two=2)
        x0, x1 = kn4[:, :, :, 0], kn4[:, :, :, 1]
        cosb = rope_ap[:, 0:64].unsqueeze(1).to_broadcast([128, nh, 64])
        sinb = rope_ap[:, 64:128].unsqueeze(1).to_broadcast([128, nh, 64])
        tA3 = tA.rearrange("p (h i) -> p h i", h=nh)
        tB3 = tB.rearrange("p (h i) -> p h i", h=nh)
        fw.op("dve", lambda e: e.tensor_tensor(tA3, x0, cosb, ALU.mult), r=[tag + "kn", rope_id], w=[tag + "tA"])
        fw.op("dve", lambda e: e.tensor_tensor(tB3, x1, sinb, ALU.mult), r=[tag + "kn", rope_id], w=[tag + "tB"])
        fw.op("dve", lambda e: e.tensor_tensor(kr4[:, :, :, 0], tA3, tB3, ALU.subtract),
              r=[tag + "tA", tag + "tB"], w=[tag + "kr"])
        fw.op("dve", lambda e: e.tensor_tensor(tA3, x0, sinb, ALU.mult), r=[tag + "kn", rope_id], w=[tag + "tA"])
        fw.op("dve", lambda e: e.tensor_tensor(tB3, x1, cosb, ALU.mult), r=[tag + "kn", rope_id], w=[tag + "tB"])
        fw.op("dve", lambda e: e.tensor_tensor(kr4[:, :, :, 1], tA3, tB3, ALU.add),
              r=[tag + "tA", tag + "tB"], w=[tag + "kr"])
        for h0 in range(0, nh, 4):
            hn = min(4, nh - h0)

            def tr(e, h0=h0, hn=hn):
                ins = None
                for j in range(hn):
                    ins = e.transpose(bank_bf(psb)[:, j * 128:(j + 1) * 128], kr[:, (h0 + j) * 128:(h0 + j + 1) * 128],
                                      ident_bf)
                return ins
            fw.op("pe", tr, r=[tag + "kr", "ident"], w=[("ps", psb)])
            for j in range(hn):
                fw.op("act", lambda e, j=j, h0=h0: e.activation(out=dst_fn(h0 + j), in_=bank_bf(psb)[:, j * 128:(j + 1) * 128],
                                                                func=AF.Copy),
                      r=[("ps", psb)], w=[dst_ids[h0 + j]])

    def attention(KT_fn, V_fn, QT, kts, kid, vid, qid, mask_fn, mask_id, E2, P2, rec, dst, dst_id, pso, psd, tagc,
                  sbanks=(2, 3), hook=None):
        n = len(kts)
        nb = len(sbanks)
        L = nb - 1

        def qk(i):
            kt = kts[i]
            b = sbanks[i % nb]
            fw.op("pe", lambda e, kt=kt, b=b: e.matmul(bank(b), KT_fn(kt), QT, start=True, stop=True),
                  r=[kid, qid], w=[("ps", b)])

        for i in range(min(L, n)):
            qk(i)
        for i in range(n):
            kt = kts[i]
            b = sbanks[i % nb]
            if i + L < n:
                qk(i + L)
            Eb = E2[i % nb]
            fw.op("act", lambda e, b=b, Eb=Eb: e.activation(out=Eb, in_=bank(b), func=AF.Exp, scale=SCALE),
                  r=[("ps", b)], w=[("E", i % nb)])
            if mask_fn is not None:
                Pb = P2[i % nb]
                mk = mask_fn(i)
                fw.op("dve", lambda e, Pb=Pb, Eb=Eb, mk=mk, kt=kt: e.scalar_tensor_tensor(
                    out=Pb, in0=Eb, scalar=validT[:, kt:kt + 1], in1=mk, op0=ALU.mult, op1=ALU.mult),
                    r=[("E", i % nb), mask_id, "valid"], w=[("P", i % nb)])
                pid = ("P", i % nb)
            else:
                Pb = Eb
                pid = ("E", i % nb)

            def pv(e, kt=kt, Pb=Pb, i=i):
                e.matmul(bank(pso), V_fn(kt), Pb, start=(i == 0), stop=(i == n - 1))
                return e.matmul(bank(psd), ones_bf, Pb, start=(i == 0), stop=(i == n - 1))
            fw.op("pe", pv, r=[pid, vid, "ones"], w=[("ps", pso), ("ps", psd)])
            if hook is not None:
                hook(i)
        fw.op("dve", lambda e: e.reciprocal(rec, bank(psd)), r=[("ps", psd)], w=["rec" + tagc])
        fw.op("dve", lambda e: e.tensor_tensor(dst, bank(pso), rec, ALU.mult),
              r=[("ps", pso), "rec" + tagc], w=[dst_id])

    ar.p = P0
    hwin = ar.alloc([128, 16, WIN], BF16)
    PA = ar.p
    xst = [ar.alloc([128, 16, 256]) for _ in range(2)]
    sq = ar.alloc([128, 16, 256], BF16)
    rt = ar.alloc([128, 256])
    rstd = ar.alloc([128, 256])
    for ck in range(WIN // 256):
        s = ck % 2
        fw.dma("sp", xst[s].rearrange("p a b -> p (a b)"), xT[ck], w=[("xst", s)])
        rms_feature_major(xst[s], 256, ("xst", s), sq, rt, rstd,
                          lambda dk, ck=ck: hwin[:, dk, ck * 256:(ck + 1) * 256], 0, [], [("hwin", ck // 2)])
    fw.barrier()

    ar.p = PA
    WqB = ar.alloc([128, 16, 1024], BF16)
    ropeQ = ar.alloc([128, 8, 128])
    rawq = ar.alloc([128, 1024])
    tmpq = ar.alloc([128, 1024])
    knq = ar.alloc([128, 1024])
    krq = ar.alloc([128, 1024], BF16)
    tAq = ar.alloc([128, 512])
    tBq = ar.alloc([128, 512])
    ssq8 = ar.alloc([128, 8])
    rt8 = ar.alloc([128, 8])
    rs8 = ar.alloc([128, 8])
    fw.dma("gp", WqB, wqB.rearrange("(dk p) c -> p dk c", p=128), w=["WqB"])
    fw.dma("sp", ropeQ, ropeT[:, 8:16, :], w=["ropeQ"])
    win_ids = [("hwin", i) for i in range(6)]
    for tt in range(8):
        for half in range(2):
            def mm(e, tt=tt, half=half):
                ins = None
                for dk in range(16):
                    ins = e.matmul(bank(half), hwin[:, dk, 1024 + tt * 128:1024 + (tt + 1) * 128],
                                   WqB[:, dk, half * 512:(half + 1) * 512], start=(dk == 0), stop=(dk == 15))
                return ins
            fw.op("pe", mm, r=["WqB"] + win_ids, w=[("ps", half)])
            fw.op("act", lambda e, half=half: e.activation(out=rawq[:, half * 512:(half + 1) * 512], in_=bank(half),
                                                           func=AF.Copy), r=[("ps", half)], w=["qraw"])
        qk_post(rawq, 8, qg_b, "qg", ropeQ[:, tt, :], "ropeQ", tmpq, ssq8, rt8, rs8, knq, krq, tAq, tBq, 6,
                lambda h, tt=tt: QT_B[:, h, tt * 128:(tt + 1) * 128], [("QTB", h) for h in range(8)], "q")
    fw.barrier()

    ar.p = PA
    WA = [ar.alloc([128, 16, 384], BF16) for _ in range(2)]
    maskS = [ar.alloc([128, 23, 128], BF16) for _ in range(2)]
    KT_h = [ar.alloc([128, WIN], BF16) for _ in range(2)]
    V_h = [ar.alloc([128, 24, 128], BF16) for _ in range(2)]
    QT_h = [ar.alloc([128, TOK], BF16) for _ in range(2)]
    E2 = [ar.alloc([128, 512], BF16) for _ in range(2)]
    P2 = [ar.alloc([128, 512], BF16) for _ in range(2)]
    rec = ar.alloc([128, 512])

    def a2_loads(h):
        s = h % 2
        fw.dma("gp", WA[s], wA[h].rearrange("(dk p) c -> p dk c", p=128), w=[("WA", s)])
        fw.dma("sp", maskS[s].rearrange("p a b -> p (a b)"), maskA[h], w=[("mask", s)])

    def a2_proj_groups(h):
        s = h % 2
        pieces = []
        gcount = [0]

        def add_group(nmm, mk_mm, evac):
            per = 4 if nmm == 16 else 16
            pb = gcount[0] % 2
            gcount[0] += 1
            for p0 in range(0, nmm, per):
                last = (p0 + per >= nmm)

                def piece(p0=p0, pb=pb):
                    def mm(e):
                        ins = None
                        for q in range(p0, p0 + per):
                            ins = mk_mm(e, q, pb)
                        return ins
                    fw.op("pe", mm, r=[("WA", s)] + win_ids, w=[("ps", pb)])
                pieces.append((piece, (lambda pb=pb: evac(pb)) if last else None))

        for c6 in range(6):
            add_group(16,
                      lambda e, dk, pb, c6=c6: e.matmul(bank(pb), WA[s][:, dk, 128:256], hwin[:, dk, c6 * 512:(c6 + 1) * 512],
                                                        start=(dk == 0), stop=(dk == 15)),
                      lambda pb, c6=c6: fw.op("act", lambda e: e.activation(out=KT_h[s][:, c6 * 512:(c6 + 1) * 512], in_=bank(pb),
                                                                             func=AF.Copy), r=[("ps", pb)], w=[("KTh", s)]))
        for c2 in range(2):
            add_group(16,
                      lambda e, dk, pb, c2=c2: e.matmul(bank(pb), WA[s][:, dk, 0:128],
                                                        hwin[:, dk, 1024 + c2 * 512:1024 + (c2 + 1) * 512],
                                                        start=(dk == 0), stop=(dk == 15)),
                      lambda pb, c2=c2: fw.op("act", lambda e: e.activation(out=QT_h[s][:, c2 * 512:(c2 + 1) * 512], in_=bank(pb),
                                                                             func=AF.Copy), r=[("ps", pb)], w=[("QTh", s)]))
        for gg in range(6):
            add_group(64,
                      lambda e, q, pb, gg=gg: e.matmul(bank(pb)[:, (q // 16) * 128:(q // 16 + 1) * 128],
                                                       hwin[:, q % 16, (4 * gg + q // 16) * 128:(4 * gg + q // 16 + 1) * 128],
                                                       WA[s][:, q % 16, 256:384], start=(q % 16 == 0), stop=(q % 16 == 15)),
                      lambda pb, gg=gg: fw.op("dve", lambda e: e.tensor_copy(
                          V_h[s][:, 4 * gg:4 * gg + 4, :].rearrange("p a b -> p (a b)"), bank(pb)),
                          r=[("ps", pb)], w=[("Vh", s)]))
        return pieces

    a2_loads(0)
    for (pc, ev) in a2_proj_groups(0):
        pc()
        if ev is not None:
            ev()
    for h in range(8):
        s = h % 2
        if h + 1 < 8:
            a2_loads(h + 1)
            pend = a2_proj_groups(h + 1)
        else:
            pend = []
        cnt_t = [0]
        npend = len(pend)
        late = []

        def hook(i, pend=pend, cnt_t=cnt_t, npend=npend, late=late):
            cnt_t[0] += 1
            for (d, ev) in [x for x in late if x[0] <= cnt_t[0]]:
                ev()
            late[:] = [x for x in late if x[0] > cnt_t[0]]
            want = (npend * cnt_t[0] + 39) // 40
            while pend and (npend - len(pend)) < want:
                pc, ev = pend.pop(0)
                pc()
                if ev is not None:
                    late.append((cnt_t[0] + 2, ev))
        for qc in range(2):
            kts = list(range(4 * qc, 4 * qc + 20))
            qt0 = 8 + 4 * qc
            pso, psd = (4, 5) if qc == 0 else (6, 7)

            def mask_fn(i, kts=kts, qt0=qt0, s=s):
                m0 = 11 - (kts[i] - qt0)
                return maskS[s][:, m0:m0 + 4, :].rearrange("p a b -> p (a b)")
            attention(lambda kt, s=s: KT_h[s][:, kt * 128:(kt + 1) * 128], lambda kt, s=s: V_h[s][:, kt, :],
                      QT_h[s][:, qc * 512:(qc + 1) * 512], kts, ("KTh", s), ("Vh", s), ("QTh", s), mask_fn, ("mask", s),
                      E2, P2, rec, mixA[:, h, qc * 512:(qc + 1) * 512], ("mixT", h), pso, psd, "A",
                      sbanks=(2, 3), hook=hook)
        while pend:
            pc, ev = pend.pop(0)
            pc()
            if ev is not None:
                late.append((0, ev))
        for (d, ev) in late:
            ev()
        late[:] = []
    fw.barrier()

    ar.p = P0
    KT_B = ar.alloc([128, 2, S], BF16)
    V_B = ar.alloc([128, 64, 256], BF16)
    PB = ar.p
    Wst = ar.alloc([128, 16, 512])
    ar.p = PB
    xst = [ar.alloc([128, 16, 256]) for _ in range(2)]
    sq = ar.alloc([128, 16, 256], BF16)
    xb = [ar.alloc([128, 16, 256], BF16) for _ in range(2)]
    Wkv = ar.alloc([128, 16, 512], BF16)
    rope4 = ar.alloc([128, 4, 128])
    raw = [ar.alloc([128, 1024]) for _ in range(2)]
    kgt = ar.alloc([128, 1024])
    sqt = ar.alloc([128, 1024])
    tA = ar.alloc([128, 512])
    tB = ar.alloc([128, 512])
    ob = ar.alloc([128, 1024])
    kr = ar.alloc([128, 1024], BF16)
    rtk = [ar.alloc([128, 2]) for _ in range(2)]
    rsk = [ar.alloc([128, 2]) for _ in range(2)]
    ssq8 = ar.alloc([128, 8])
    rt8 = ar.alloc([128, 8])
    rs8 = ar.alloc([128, 8])
    tC = sqt[:, 0:512]
    tD = sqt[:, 512:1024]
    fw.dma("sp", Wst, wkvB.rearrange("(dk p) c -> p dk c", p=128), w=["Wst"])

    def foldw(e):
        ins = None
        for dk in range(16):
            ins = e.tensor_scalar(Wkv[:, dk, :], Wst[:, dk, :], g1T[:, dk:dk + 1], None, ALU.mult)
        return ins
    fw.op("dve", foldw, r=["Wst", "g1T"], w=["Wkv"])
    SSQB = (0, 6)
    sched = []
    step = [0]

    def run_due():
        due = [f for (d, f) in sched if d <= step[0]]
        rest = [(d, f) for (d, f) in sched if d > step[0]]
        sched[:] = rest
        for f in due:
            f()

    def b1_head(C, half):
        ck = 2 * C + half
        s = ck % 2
        fw.dma("sp", xst[s].rearrange("p a b -> p (a b)"), xT[ck], w=[("xst", s)] + (["Wst"] if ck < 2 else []))
        fw.op("act", lambda e, s=s: e.activation(out=sq, in_=xst[s], func=AF.Square), r=[("xst", s)], w=["sq"])
        fw.op("dve", lambda e, s=s: e.tensor_copy(xb[s], xst[s]), r=[("xst", s)], w=[("xb", s)])

        def mms(e, half=half):
            ins = None
            for sub in range(2):
                for dk in range(16):
                    ins = e.matmul(bank(SSQB[half])[:, sub:sub + 1], sq[:, dk, sub * 128:(sub + 1) * 128],
                                   ones_bf[:, 0:1], start=(dk == 0), stop=(dk == 15))
            return ins
        fw.op("pe", mms, r=["sq", "ones"], w=[("ps", SSQB[half])])
        for sub in range(2):
            bb = 1 + 2 * half + sub

            def mm(e, s=s, sub=sub, bb=bb):
                ins = None
                for dk in range(16):
                    ins = e.matmul(bank(bb), xb[s][:, dk, sub * 128:(sub + 1) * 128], Wkv[:, dk, :],
                                   start=(dk == 0), stop=(dk == 15))
                return ins
            fw.op("pe", mm, r=[("xb", s), "Wkv"], w=[("ps", bb)])

    def b1_tail(C, half):
        r_ = C % 2
        fw.op("act", lambda e, half=half: e.activation(out=rtk[half], in_=bank(SSQB[half])[:, 0:2], func=AF.Sqrt,
                                                       bias=EPS, scale=1.0 / D), r=[("ps", SSQB[half])], w=[("rtk", half)])
        fw.op("dve", lambda e, half=half: e.reciprocal(rsk[half], rtk[half]), r=[("rtk", half)], w=[("rsk", half)])
        for sub in range(2):
            j = 2 * half + sub
            bb = 1 + j
            fw.op("act", lambda e, bb=bb, C=C, j=j, half=half, sub=sub: e.activation(
                out=V_B[:, 4 * C + j, :], in_=bank(bb)[:, 256:512], func=AF.Copy, scale=rsk[half][:, sub:sub + 1]),
                r=[("ps", bb), ("rsk", half)], w=[("VB", C, j)])
            fw.op("act", lambda e, bb=bb, j=j, half=half, sub=sub, r_=r_: e.activation(
                out=raw[r_][:, j * 256:(j + 1) * 256], in_=bank(bb)[:, 0:256], func=AF.Copy,
                scale=rsk[half][:, sub:sub + 1]), r=[("ps", bb), ("rsk", half)], w=[("raw", r_, j)])

    def b1_postA(C):
        r_ = C % 2
        fw.dma("sp", rope4, ropeT[:, 4 * C:4 * C + 4, :], w=["rope4"])
        rw = raw[r_]
        raw_ids = [("raw", r_, j) for j in range(4)]
        rw3 = rw.rearrange("p (a d) -> p a d", a=8)
        kg3 = kgt.rearrange("p (a d) -> p a d", a=8)
        sq3 = sqt.rearrange("p (a d) -> p a d", a=8)
        kg5 = kgt.rearrange("p (j h i two) -> p j h i two", j=4, h=2, two=2)
        ob5 = ob.rearrange("p (j h i two) -> p j h i two", j=4, h=2, two=2)
        ob3 = ob.rearrange("p (a d) -> p a d", a=8)
        kr3 = kr.rearrange("p (a d) -> p a d", a=8)
        x0, x1 = kg5[:, :, :, :, 0], kg5[:, :, :, :, 1]
        cosb = rope4[:, :, 0:64].unsqueeze(2).to_broadcast([128, 4, 2, 64])
        sinb = rope4[:, :, 64:128].unsqueeze(2).to_broadcast([128, 4, 2, 64])
        v4 = lambda t: t.rearrange("p (j h i) -> p j h i", j=4, h=2)
        fw.op("dve", lambda e: e.tensor_tensor(kg3, rw3, kg_b.unsqueeze(1).to_broadcast([128, 8, 128]), ALU.mult),
              r=raw_ids + ["kg"], w=["kgt"])
        fw.op("dve", lambda e: e.tensor_tensor(sqt, rw, rw, ALU.mult), r=raw_ids, w=["sqt", "sqt2"])
        fw.op("dve", lambda e: e.tensor_tensor(v4(tA), x0, cosb, ALU.mult), r=["kgt", "rope4"], w=["tA"])
        fw.op("dve", lambda e: e.tensor_reduce(ssq8, sq3, AX.X, ALU.add), r=["sqt", "sqt2"], w=["ssq8"])
        fw.op("dve", lambda e: e.tensor_tensor(v4(tB), x1, sinb, ALU.mult), r=["kgt", "rope4"], w=["tB"])
        fw.op("dve", lambda e: e.tensor_tensor(v4(tC), x0, sinb, ALU.mult), r=["kgt", "rope4"], w=["sqt"])
        fw.op("dve", lambda e: e.tensor_tensor(v4(tD), x1, cosb, ALU.mult), r=["kgt", "rope4"], w=["sqt2"])
        fw.op("dve", lambda e: e.tensor_tensor(ob5[:, :, :, :, 0], v4(tA), v4(tB), ALU.subtract), r=["tA", "tB"], w=["ob0"])
        fw.op("dve", lambda e: e.tensor_tensor(ob5[:, :, :, :, 1], v4(tC), v4(tD), ALU.add), r=["sqt", "sqt2"], w=["ob1"])

    def b1_postB(C):
        ob3 = ob.rearrange("p (a d) -> p a d", a=8)
        kr3 = kr.rearrange("p (a d) -> p a d", a=8)
        fw.op("act", lambda e: e.activation(out=rt8, in_=ssq8, func=AF.Sqrt, bias=EPS, scale=1.0 / 128), r=["ssq8"], w=["rt8"])
        fw.op("dve", lambda e: e.reciprocal(rs8, rt8), r=["rt8"], w=["rs8"])
        fw.op("dve", lambda e: e.tensor_tensor(kr3, ob3, rs8.unsqueeze(2).to_broadcast([128, 8, 128]), ALU.mult),
              r=["ob0", "ob1", "rs8"], w=["kr"])
        def trk(e):
            ins = None
            for j in range(4):
                for hh in range(2):
                    ins = e.transpose(bank_bf(5)[:, (hh * 4 + j) * 128:(hh * 4 + j + 1) * 128],
                                      kr[:, (j * 2 + hh) * 128:(j * 2 + hh + 1) * 128], ident_bf)
            return ins
        fw.op("pe", trk, r=["kr", "ident"], w=[("ps", 5)])
        for hh in range(2):
            fw.op("dve", lambda e, hh=hh, C=C: e.tensor_copy(KT_B[:, hh, C * 512:(C + 1) * 512],
                                                             bank_bf(5)[:, hh * 512:(hh + 1) * 512]),
                  r=[("ps", 5)], w=[("KTB", C, hh)])

    for C in range(S // 512):
        for half in range(2):
            b1_head(C, half)
            run_due()
            sched.append((step[0] + 1, lambda C=C, half=half: b1_tail(C, half)))
            if half == 1:
                sched.append((step[0] + 1, lambda C=C: b1_postA(C)))
                sched.append((step[0] + 2, lambda C=C: b1_postB(C)))
            step[0] += 1
    step[0] += 10
    run_due()
    fw.barrier()

    ar.p = PB
    mixB = ar.alloc([128, 8, TOK], BF16)
    E2 = [ar.alloc([128, 512], BF16) for _ in range(4)]
    rec = ar.alloc([128, 512])
    Wo = [ar.alloc([128, 16, 512], BF16) for _ in range(2)]
    w_out3 = w_out.rearrange("(m p) c -> p m c", p=128)
    for oc_ in range(2):
        fw.dma("gp", Wo[oc_], w_out3[:, :, oc_ * 512:(oc_ + 1) * 512], w=[("Wo", oc_)])
    cnt = 0
    for h in range(8):
        kv = h // 4
        for qc in range(2):
            pso, psd = (4, 5) if cnt % 2 == 0 else (6, 7)
            cnt += 1
            attention(lambda kt, kv=kv: KT_B[:, kv, kt * 128:(kt + 1) * 128],
                      lambda kt, kv=kv: V_B[:, kt, kv * 128:(kv + 1) * 128],
                      QT_B[:, h, qc * 512:(qc + 1) * 512], list(range(64)), "KTB", "VB", ("QTB", h), None, None,
                      E2, None, rec, mixB[:, h, qc * 512:(qc + 1) * 512], ("mixT", 8 + h), pso, psd, "B", sbanks=(0, 1, 2, 3))
    fw.barrier()

    ar.p = P0
    acc = ar.alloc([128, 8, D])
    assert ar.p <= PB
    PO = PB + 4096
    ar.p = PO
    for tt in range(8):
        fw.dma("sp", acc[:, tt, :], x_own[tt * 128:(tt + 1) * 128, :], w=[("acc", tt)], key=("accld", tt))
    mix_ids = [("mixT", i) for i in range(16)]
    k = 0
    for oc in range(4):
        s = oc % 2
        if oc >= 2:
            fw.dma("gp", Wo[s], w_out3[:, :, oc * 512:(oc + 1) * 512], w=[("Wo", s)])
        for tt in range(8):
            b = k % 4
            k += 1

            def mm(e, tt=tt, s=s, b=b):
                ins = None
                for m in range(16):
                    ins = e.matmul(bank(b), (mixA[:, m, tt * 128:(tt + 1) * 128] if m < 8 else mixB[:, m - 8, tt * 128:(tt + 1) * 128]), Wo[s][:, m, :],
                                   start=(m == 0), stop=(m == 15))
                return ins
            fw.op("pe", mm, r=[("Wo", s)] + mix_ids, w=[("ps", b)])
            fw.op("dve", lambda e, tt=tt, oc=oc, b=b: e.tensor_tensor(
                acc[:, tt, oc * 512:(oc + 1) * 512], bank(b), acc[:, tt, oc * 512:(oc + 1) * 512], ALU.add),
                r=[("ps", b), ("acc", tt)], w=[("acc", tt)])
    fw.barrier()

    ar.p = MIX0
    h2T = ar.alloc([128, 16, TOK], BF16)
    PH = ar.p
    ar.p = PO
    g2_b = ar.alloc([128, D])
    junk = ar.alloc([128, D], BF16)
    h2 = [ar.alloc([128, D], BF16) for _ in range(2)]
    ssqn = ar.alloc([128, 8])
    rtn = ar.alloc([128, 8])
    rsn = ar.alloc([128, 8])
    fw.dma("sp", g2_b, g2_d, w=["g2"])
    acc_ids = [("acc", t_) for t_ in range(8)]
    fw.dma("sp", x1_scr, acc.rearrange("p a b -> p (a b)"), r=acc_ids, w=["x1scr"])
    fw.op("dve", lambda e: e.memset(ssqn, 0.0), w=["ssqn"])
    for tt in range(8):
        fw.op("act", lambda e, tt=tt: e.activation(out=junk, in_=acc[:, tt, :], func=AF.Square,
                                                   accum_out=ssqn[:, tt:tt + 1]), r=[("acc", tt), "ssqn"], w=["junk", ("ssqn", tt)])
        fw.op("act", lambda e, tt=tt: e.activation(out=rtn[:, tt:tt + 1], in_=ssqn[:, tt:tt + 1], func=AF.Sqrt,
                                                   bias=EPS, scale=1.0 / D), r=[("ssqn", tt)], w=[("rtn", tt)])
    for tt in range(8):
        fw.op("dve", lambda e, tt=tt: e.reciprocal(rsn[:, tt:tt + 1], rtn[:, tt:tt + 1]), r=[("rtn", tt)], w=[("rsn", tt)])
        s = tt % 2
        fw.op("dve", lambda e, tt=tt, s=s: e.scalar_tensor_tensor(out=h2[s], in0=acc[:, tt, :], scalar=rsn[:, tt:tt + 1],
                                                                  in1=g2_b, op0=ALU.mult, op1=ALU.mult),
              r=[("acc", tt), ("rsn", tt), "g2"], w=[("h2", s)])
        for g in range(4):
            b = g % 2

            def tr(e, g=g, s=s, b=b):
                ins = None
                for j in range(4):
                    dk = 4 * g + j
                    ins = e.transpose(bank_bf(b)[:, j * 128:(j + 1) * 128], h2[s][:, dk * 128:(dk + 1) * 128], ident_bf)
                return ins
            fw.op("pe", tr, r=[("h2", s), "ident"], w=[("ps", b)])
            fw.op("act", lambda e, g=g, tt=tt, b=b: e.activation(
                out=h2T[:, 4 * g:4 * g + 4, tt * 128:(tt + 1) * 128],
                in_=bank_bf(b)[:, 0:512].rearrange("p (a t) -> p a t", a=4), func=AF.Copy),
                r=[("ps", b)], w=["h2T"])
    fw.barrier()

    ar.p = PH
    qT = ar.alloc([128, 16, TOK], BF16)
    subkT = ar.alloc([128, 16, 128], BF16)
    PQ = ar.p
    Wq = [ar.alloc([128, 16, 512], BF16) for _ in range(2)]
    fw.dma("gp", subkT, subkT_d, w=["subk"])
    wq3 = wq.rearrange("(dk p) c -> p dk c", p=128)
    k = 0
    for piece in range(4):
        s = piece % 2
        fw.dma("gp", Wq[s], wq3[:, :, piece * 512:(piece + 1) * 512], w=[("Wq", s)])
        for bb in range(4):
            blk = piece * 4 + bb
            for half in range(2):
                b = k % 4
                k += 1

                def mm(e, bb=bb, half=half, s=s, b=b):
                    ins = None
                    for dk in range(16):
                        ins = e.matmul(bank(b), Wq[s][:, dk, bb * 128:(bb + 1) * 128], h2T[:, dk, half * 512:(half + 1) * 512],
                                       start=(dk == 0), stop=(dk == 15))
                    return ins
                fw.op("pe", mm, r=[("Wq", s), "h2T"], w=[("ps", b)])
                fw.op("act", lambda e, blk=blk, half=half, b=b: e.activation(
                    out=qT[:, blk, half * 512:(half + 1) * 512], in_=bank(b), func=AF.Copy), r=[("ps", b)], w=["qT"])
    fw.barrier()

    ar.p = PQ
    bufX = ar.alloc([128, 2048])
    bufY = ar.alloc([128, 2048])
    top = ar.alloc([128, 256])
    idx = ar.alloc([128, 256], U32)
    idxf = ar.alloc([128, 256])
    best = ar.alloc([128, 128])
    pos = ar.alloc([128, 128], U32)
    pa = ar.alloc([128, 128], U32)
    pb = ar.alloc([128, 128], U32)
    paf = ar.alloc([128, 128])
    pbf = ar.alloc([128, 128])
    bm = ar.alloc([128, 128])
    ex = ar.alloc([128, 128])
    Zs = ar.alloc([128, 8])
    rZ = ar.alloc([128, 8])
    R3 = ar.alloc([128, 3, 128], BF16)
    RT = ar.alloc([128, 3, 128])
    NT = 16
    A1t = [ar.alloc([128, NT, 128]) for _ in range(2)]
    A1 = [ar.alloc([128, NT, 128], BF16) for _ in range(2)]
    A2 = [ar.alloc([128, NT, 128], BF16) for _ in range(2)]
    Gs = [ar.alloc([128, 128, 128], BF16) for _ in range(2)]
    XS = [("X", i) for i in range(16)]
    YS = [("Y", i) for i in range(16)]
    sc3 = bufX.rearrange("p (s n) -> p s n", s=16)
    wk3 = bufY.rearrange("p (s n) -> p s n", s=16)
    top3 = top.rearrange("p (s k) -> p s k", s=16)
    idx3 = idx.rearrange("p (s k) -> p s k", s=16)
    top4 = top.rearrange("p (h c k) -> p h c k", h=8, c=2)
    idxf4 = idxf.rearrange("p (h c k) -> p h c k", h=8, c=2)
    cand3 = bufX.rearrange("p (h n) -> p h n", h=8)
    cand4 = bufX.rearrange("p (h a b) -> p h a b", h=8, a=16)
    cw3 = bufY.rearrange("p (h n) -> p h n", h=8)
    best3 = best.rearrange("p (h k) -> p h k", h=8)
    pos3 = pos.rearrange("p (h k) -> p h k", h=8)
    bm3 = bm.rearrange("p (h k) -> p h k", h=8)
    ex3 = ex.rearrange("p (h k) -> p h k", h=8)
    selY = bufY.rearrange("p (h k a) -> p h k a", h=8, k=16)
    selX = bufX.rearrange("p (h k a) -> p h k a", h=8, k=16)
    iota16b = iota[:, 0:16].unsqueeze(1).unsqueeze(1).to_broadcast([128, 8, 16, 16])
    all_top = [("top", sg, j) for sg in range(16) for j in range(2)]
    all_idx = [("idx", sg, j) for sg in range(16) for j in range(2)]
    all_best = [("best", hh, j) for hh in range(8) for j in range(2)]
    all_pos = [("pos", hh, j) for hh in range(8) for j in range(2)]

    def topk_stages(tt):
        def st0():
            def mm(e):
                ins = None
                for blk in range(16):
                    ins = e.matmul(psA[:, blk * 128:(blk + 1) * 128], qT[:, blk, tt * 128:(tt + 1) * 128], subkT[:, blk, :],
                                   start=True, stop=True)
                return ins
            fw.op("pe", mm, r=["qT", "subk"], w=[("ps", 0), ("ps", 1), ("ps", 2), ("ps", 3)])
            fw.op("act", lambda e: e.activation(out=bufX, in_=psA[:, :], func=AF.Copy),
                  r=[("ps", 0), ("ps", 1), ("ps", 2), ("ps", 3)], w=XS)
            for sg in range(16):
                fw.op("dve", lambda e, sg=sg: e.max(out=top3[:, sg, 0:8], in_=sc3[:, sg, :]), r=[("X", sg)], w=[("top", sg, 0)])

        def st1():
            for sg in range(16):
                fw.op("dve", lambda e, sg=sg: e.max_index(out=idx3[:, sg, 0:8], in_max=top3[:, sg, 0:8], in_values=sc3[:, sg, :]),
                      r=[("X", sg), ("top", sg, 0)], w=[("idx", sg, 0)])
                fw.op("dve", lambda e, sg=sg: e.match_replace(out=wk3[:, sg, :], in_to_replace=top3[:, sg, 0:8],
                                                              in_values=sc3[:, sg, :], imm_value=NEG),
                      r=[("X", sg), ("top", sg, 0)], w=[("Y", sg)])

        def st2():
            for sg in range(16):
                fw.op("dve", lambda e, sg=sg: e.max(out=top3[:, sg, 8:16], in_=wk3[:, sg, :]), r=[("Y", sg)], w=[("top", sg, 1)])

        def st3():
            for sg in range(16):
                fw.op("dve", lambda e, sg=sg: e.max_index(out=idx3[:, sg, 8:16], in_max=top3[:, sg, 8:16], in_values=wk3[:, sg, :]),
                      r=[("Y", sg), ("top", sg, 1)], w=[("idx", sg, 1)])
            fw.op("dve", lambda e: e.tensor_copy(idxf, idx), r=all_idx, w=["idxf"])

        def st4():
            fw.op("dve", lambda e: e.tensor_tensor(cand4, top4[:, :, 0, :].unsqueeze(3).to_broadcast([128, 8, 16, 16]),
                                                   top4[:, :, 1, :].unsqueeze(2).to_broadcast([128, 8, 16, 16]), ALU.add),
                  r=all_top, w=XS)
            for hh in range(8):
                fw.op("dve", lambda e, hh=hh: e.max(out=best3[:, hh, 0:8], in_=cand3[:, hh, :]),
                      r=[("X", 2 * hh), ("X", 2 * hh + 1)], w=[("best", hh, 0)])

        def st5():
            for hh in range(8):
                fw.op("dve", lambda e, hh=hh: e.max_index(out=pos3[:, hh, 0:8], in_max=best3[:, hh, 0:8], in_values=cand3[:, hh, :]),
                      r=[("X", 2 * hh), ("X", 2 * hh + 1), ("best", hh, 0)], w=[("pos", hh, 0)])
                fw.op("dve", lambda e, hh=hh: e.match_replace(out=cw3[:, hh, :], in_to_replace=best3[:, hh, 0:8],
                                                              in_values=cand3[:, hh, :], imm_value=NEG),
                      r=[("X", 2 * hh), ("X", 2 * hh + 1), ("best", hh, 0)], w=[("Y", 2 * hh), ("Y", 2 * hh + 1)])
            for hh in range(8):
                fw.op("dve", lambda e, hh=hh: e.max(out=best3[:, hh, 8:16], in_=cw3[:, hh, :]),
                      r=[("Y", 2 * hh), ("Y", 2 * hh + 1)], w=[("best", hh, 1)])

        def st6():
            for hh in range(8):
                fw.op("dve", lambda e, hh=hh: e.max_index(out=pos3[:, hh, 8:16], in_max=best3[:, hh, 8:16], in_values=cw3[:, hh, :]),
                      r=[("Y", 2 * hh), ("Y", 2 * hh + 1), ("best", hh, 1)], w=[("pos", hh, 1)])
            fw.op("dve", lambda e: e.tensor_tensor(bm3, best3, best3[:, :, 0:1].to_broadcast([128, 8, 16]), ALU.subtract),
                  r=all_best, w=["bm"])
            fw.op("act", lambda e: e.activation(out=ex, in_=bm, func=AF.Exp), r=["bm"], w=["ex"])
            fw.op("dve", lambda e: e.tensor_single_scalar(pa, pos, 4, ALU.logical_shift_right), r=all_pos, w=["pa"])
            fw.op("dve", lambda e: e.tensor_single_scalar(pb, pos, 15, ALU.bitwise_and), r=all_pos, w=["pb"])
            fw.op("dve", lambda e: e.tensor_copy(paf, pa), r=["pa"], w=["paf"])
            fw.op("dve", lambda e: e.tensor_copy(pbf, pb), r=["pb"], w=["pbf"])

        def st7():
            paf3 = paf.rearrange("p (h k) -> p h k", h=8)
            pbf3 = pbf.rearrange("p (h k) -> p h k", h=8)
            fw.op("dve", lambda e: e.tensor_tensor(selY, iota16b, paf3.unsqueeze(3).to_broadcast([128, 8, 16, 16]), ALU.is_equal),
                  r=["paf", "iota"], w=YS)
            fw.op("gp", lambda e: e.tensor_tensor(selY, selY, idxf4[:, :, 0, :].unsqueeze(2).to_broadcast([128, 8, 16, 16]), ALU.mult),
                  r=YS + ["idxf"], w=YS)
            fw.op("dve", lambda e: e.tensor_tensor(selX, iota16b, pbf3.unsqueeze(3).to_broadcast([128, 8, 16, 16]), ALU.is_equal),
                  r=["pbf", "iota"], w=XS)
            fw.op("gp", lambda e: e.tensor_tensor(selX, selX, idxf4[:, :, 1, :].unsqueeze(2).to_broadcast([128, 8, 16, 16]), ALU.mult),
                  r=XS + ["idxf"], w=XS)
            fw.op("dve", lambda e: e.tensor_reduce(Zs, ex3, AX.X, ALU.add), r=["ex"], w=["Zs"])
            fw.op("dve", lambda e: e.reciprocal(rZ, Zs), r=["Zs"], w=["rZ"])
            fw.op("dve", lambda e: e.tensor_tensor(R3[:, 2, :].rearrange("p (h k) -> p h k", h=8), ex3,
                                                   rZ.unsqueeze(2).to_broadcast([128, 8, 16]), ALU.mult),
                  r=["ex", "rZ"], w=["R3g"])
            fw.op("dve", lambda e: e.tensor_reduce(R3[:, 0, :].rearrange("p (h k) -> p h k", h=8), selY, AX.X, ALU.add),
                  r=YS, w=[("R3", 0)])
            fw.op("dve", lambda e: e.tensor_reduce(R3[:, 1, :].rearrange("p (h k) -> p h k", h=8), selX, AX.X, ALU.add),
                  r=XS, w=[("R3", 1)])
        return [st0, st1, st2, st3, st4, st5, st6, st7]

    def onehot_prelude(tt):
        def tr(e):
            ins = None
            for j in range(3):
                ins = e.transpose(bank_bf(4)[:, j * 128:(j + 1) * 128], R3[:, j, :], ident_bf)
            return ins
        fw.op("pe", tr, r=[("R3", 0), ("R3", 1), "R3g", "ident"], w=[("ps", 4)])
        fw.op("act", lambda e: e.activation(out=RT.rearrange("p a b -> p (a b)"), in_=bank_bf(4)[:, 0:384], func=AF.Copy),
              r=[("ps", 4)], w=["RT"])

    iotab = iota.unsqueeze(1).to_broadcast([128, NT, 128])

    def onehot_group(tt, g):
        gs = Gs[tt % 2]
        s = g % 2
        t0 = g * NT
        fw.op("dve", lambda e: e.tensor_tensor(A2[s], iotab, RT[:, 1, t0:t0 + NT].unsqueeze(2).to_broadcast([128, NT, 128]),
                                               ALU.is_equal), r=["RT", "iota"], w=[("A2", s)])
        fw.op("dve", lambda e: e.tensor_tensor(A1t[s], iotab, RT[:, 0, t0:t0 + NT].unsqueeze(2).to_broadcast([128, NT, 128]),
                                               ALU.is_equal), r=["RT", "iota"], w=[("A1t", s)])
        def gate(e):
            ins = None
            for j in range(NT):
                ins = e.activation(out=A1[s][:, j, :], in_=A1t[s][:, j, :], func=AF.Copy, scale=RT[:, 2, t0 + j:t0 + j + 1])
            return ins
        if g % 3 == 2:
            fw.op("gp", lambda e: e.tensor_tensor(A1[s], A1t[s], RT[:, 2, t0:t0 + NT].unsqueeze(2).to_broadcast([128, NT, 128]),
                                                  ALU.mult), r=["RT", ("A1t", s)], w=[("A1", s)])
        else:
            fw.op("act", gate, r=["RT", ("A1t", s)], w=[("A1", s)])
        for q4 in range(NT // 4):
            b = 5 + (q4 % 2)

            def gm(e, q4=q4, b=b):
                ins = None
                for j in range(4):
                    ins = e.matmul(bank(b)[:, j * 128:(j + 1) * 128], A1[s][:, 4 * q4 + j, :], A2[s][:, 4 * q4 + j, :],
                                   start=True, stop=True)
                return ins
            fw.op("pe", gm, r=[("A1", s), ("A2", s)], w=[("ps", b)])
            tq = t0 + 4 * q4
            fw.op("act", lambda e, b=b, tq=tq: e.activation(
                out=gs[:, :, tq:tq + 4], in_=bank(b).rearrange("p (t i) -> p i t", t=4), func=AF.Copy),
                r=[("ps", b)], w=[("Gs", tt % 2, tq)])

    for st in topk_stages(0):
        st()
    for tt in range(8):
        onehot_prelude(tt)
        nxt = topk_stages(tt + 1) if tt + 1 < 8 else []
        for g in range(128 // NT):
            if g < len(nxt):
                nxt[g]()
            onehot_group(tt, g)
        fw.dma("sp", Gd[tt].rearrange("p a b -> p (a b)"), Gs[tt % 2].rearrange("p a b -> p (a b)"),
               r=[("Gs", tt % 2, tq_) for tq_ in range(0, 128, 4)], w=[("Gd", tt)], key=("Gdk", tt % 2))
    fw.barrier()

    ar.p = PO
    GRP = 4
    UT = [ar.alloc([128, 16, 128], BF16) for _ in range(3)]
    Vc = [[ar.alloc([128, D], BF16) for _ in range(GRP)] for _ in range(2)]
    Gc = [ar.alloc([128, TOK], BF16) for _ in range(3)]
    AT = [ar.alloc([128, GRP, TOK], BF16) for _ in range(2)]
    ge = [ar.alloc([128, 512], BF16) for _ in range(2)]
    all_gd = [("Gd", t) for t in range(8)]
    kU = 0
    kV = 0
    for grp in range(128 // GRP):
        gsl = grp % 2
        for j in range(GRP):
            c = grp * GRP + j
            s3 = c % 3
            fw.dma("gp", UT[s3].rearrange("p a b -> p (a b)"), UT_l[c], w=[("UT", s3)])
            fw.dma("gp", Vc[gsl][j], V_l[c], w=[("Vc", gsl, j)])
            fw.dma("sp", Gc[s3].rearrange("p (a t) -> p a t", a=8), Gd[:, :, c, :].rearrange("a p t -> p a t"),
                   r=all_gd, w=[("Gc", s3)])
            for half in range(2):
                b = kU % 2
                kU += 1

                def mm(e, s3=s3, half=half, b=b):
                    ins = None
                    for dk in range(16):
                        ins = e.matmul(bank(b), UT[s3][:, dk, :], h2T[:, dk, half * 512:(half + 1) * 512],
                                       start=(dk == 0), stop=(dk == 15))
                    return ins
                fw.op("pe", mm, r=[("UT", s3), "h2T"], w=[("ps", b)])
                fw.op("act", lambda e, b=b: e.activation(out=ge[b], in_=bank(b), func=AF.Gelu_apprx_tanh), r=[("ps", b)], w=[("ge", b)])
                fw.op("dve", lambda e, b=b, gsl=gsl, j=j, half=half, s3=s3: e.tensor_tensor(
                    AT[gsl][:, j, half * 512:(half + 1) * 512], ge[b], Gc[s3][:, half * 512:(half + 1) * 512], ALU.mult),
                    r=[("ge", b), ("Gc", s3)], w=[("AT", gsl, j)])
        if grp == 0:
            fw.dma("sp", acc.rearrange("p a b -> p (a b)"), x1_scr, r=["x1scr"],
                   w=["acc"] + [("acc", t_, o_) for t_ in range(8) for o_ in range(4)], key="accld")
        for tt in range(8):
            for oc in range(4):
                b = 4 + (kV % 4)
                kV += 1

                def vm(e, gsl=gsl, tt=tt, oc=oc, b=b):
                    ins = None
                    for j in range(GRP):
                        ins = e.matmul(bank(b), AT[gsl][:, j, tt * 128:(tt + 1) * 128], Vc[gsl][j][:, oc * 512:(oc + 1) * 512],
                                       start=(j == 0), stop=(j == GRP - 1))
                    return ins
                fw.op("pe", vm, r=[("AT", gsl, j) for j in range(GRP)] + [("Vc", gsl, j) for j in range(GRP)], w=[("ps", b)])
                fw.op("dve", lambda e, tt=tt, oc=oc, b=b: e.tensor_tensor(
                    acc[:, tt, oc * 512:(oc + 1) * 512], bank(b), acc[:, tt, oc * 512:(oc + 1) * 512], ALU.add),
                    r=[("ps", b), ("acc", tt, oc)], w=[("acc", tt, oc)], extra=None)
    fw.barrier()

    ar.p = PO
    gf_b = ar.alloc([128, D])
    junk = ar.alloc([128, D], BF16)
    ost = [ar.alloc([128, D]) for _ in range(2)]
    ssqf = ar.alloc([128, 8])
    rtf = ar.alloc([128, 8])
    rsf = ar.alloc([128, 8])
    fw.dma("sp", gf_b, gf_d, w=["gf"])
    fw.op("dve", lambda e: e.memset(ssqf, 0.0), w=["ssqf"])
    for tt in range(8):
        s = tt % 2
        fw.op("act", lambda e, tt=tt: e.activation(out=junk, in_=acc[:, tt, :], func=AF.Square,
                                                   accum_out=ssqf[:, tt:tt + 1]), r=["ssqf"], w=["junkf", ("ssqf", tt)])
        fw.op("act", lambda e, tt=tt: e.activation(out=rtf[:, tt:tt + 1], in_=ssqf[:, tt:tt + 1], func=AF.Sqrt,
                                                   bias=EPS, scale=1.0 / D), r=[("ssqf", tt)], w=[("rtf", tt)])
        fw.op("dve", lambda e, tt=tt: e.reciprocal(rsf[:, tt:tt + 1], rtf[:, tt:tt + 1]), r=[("rtf", tt)], w=[("rsf", tt)])
        fw.op("dve", lambda e, tt=tt, s=s: e.scalar_tensor_tensor(out=ost[s], in0=acc[:, tt, :], scalar=rsf[:, tt:tt + 1],
                                                                  in1=gf_b, op0=ALU.mult, op1=ALU.mult),
              r=[("rsf", tt), "gf"], w=[("ost", s)])
        fw.dma("sp", out_d[tt * 128:(tt + 1) * 128, :], ost[s], r=[("ost", s)], w=[("out", tt)], key=("out", s))

    keys = fw.analyze()
    with ExitStack() as es:
        sems = {}
        for i, k in enumerate(keys):
            sems[k] = es.enter_context(nc.semaphore("s%d" % i))
        block = es.enter_context(nc.Block())
        with nc.allow_low_precision(reason="bf16 matmul operands, exact small ints"):
            fw.emit(nc, block, sems)
    return nc


def _constants():
    half = 64
    inv = (10000.0 ** (-np.arange(0, half, 2, dtype=np.float32) / half)).astype(np.float32)
    row = np.repeat(np.arange(S // 64, dtype=np.float32), 64)
    col = np.tile(np.arange(64, dtype=np.float32), S // 64)
    ang = np.concatenate([row[:, None] * inv, col[:, None] * inv], axis=-1).astype(np.float32)
    rope = np.concatenate([np.cos(ang), np.sin(ang)], axis=-1).astype(np.float32)
    slopes = 2.0 ** (-8.0 * (np.arange(8, dtype=np.float64) + 1.0) / 8)
    kk = np.arange(128)[:, None, None]
    mm_ = np.arange(23)[None, :, None]
    qq = np.arange(128)[None, None, :]
    delta = (11 - mm_) * 128 + kk - qq
    ad = np.abs(delta)
    cnt = (ad <= 64).astype(np.float64) + ((ad <= 256) & (delta % 4 == 0)) + ((ad <= 1024) & (delta % 16 == 0))
    mask = np.stack([cnt * np.exp(-slopes[h] * ad) for h in range(8)], axis=0)
    mask = mask.reshape(8, 128, 23 * 128).astype(np.float32).astype(ml_dtypes.bfloat16)
    iota = np.tile(np.arange(128, dtype=np.float32)[None, :], (128, 1))
    ident = np.eye(128, dtype=np.float32)
    return rope, mask, iota, ident


_NC_CACHE = {}


def kernel(x, norm1_g, w_in, q_norm_g, k_norm_g, w_out, norm2_g, peer_w_query, peer_sub_keys,
           peer_u, peer_v, final_norm_g):
    f32 = np.float32
    x = np.asarray(x, f32)
    w_in0 = np.asarray(w_in, f32)[0]
    rope, mask, iota, ident = _constants()
    xT_full = np.ascontiguousarray(x[0].T)
    wA = np.ascontiguousarray(np.stack([np.concatenate(
        [w_in0[:, h * 128:(h + 1) * 128], w_in0[:, 1024 + h * 128:1024 + (h + 1) * 128],
         w_in0[:, 2048 + h * 128:2048 + (h + 1) * 128]], axis=1) for h in range(8)], axis=0))
    wqB = np.ascontiguousarray(w_in0[:, 3072:4096])
    wkvB = np.ascontiguousarray(w_in0[:, 4096:4608])
    U = np.asarray(peer_u, f32)[0]
    V = np.asarray(peer_v, f32)[0]
    UT_l = np.ascontiguousarray(U.reshape(128, 128, 16, 128).transpose(1, 3, 2, 0)).reshape(128, 128, 16 * 128)
    V_l = np.ascontiguousarray(V.reshape(128, 128, D).transpose(1, 0, 2))
    subkT = np.ascontiguousarray(np.asarray(peer_sub_keys, f32)[0].reshape(16, 128, 128).transpose(2, 0, 1))
    common = {
        "maskA": mask, "g1T": np.ascontiguousarray(np.asarray(norm1_g, f32)[0].reshape(16, 128).T),
        "qg_b": np.ascontiguousarray(np.tile(np.asarray(q_norm_g, f32)[0][None, :], (128, 1))),
        "kg_b": np.ascontiguousarray(np.tile(np.asarray(k_norm_g, f32)[0][None, :], (128, 1))),
        "g2_b": np.ascontiguousarray(np.tile(np.asarray(norm2_g, f32)[0][None, :], (128, 1))),
        "gf_b": np.ascontiguousarray(np.tile(np.asarray(final_norm_g, f32)[None, :], (128, 1))),
        "iota": iota, "ident": ident, "wA": wA, "wqB": wqB, "wkvB": wkvB,
        "w_out": np.ascontiguousarray(np.asarray(w_out, f32)[0]),
        "wq": np.ascontiguousarray(np.asarray(peer_w_query, f32)[0]),
        "subkT": subkT, "UT_l": UT_l, "V_l": V_l,
    }
    in_maps = []
    for c in range(NCORES):
        shift = 1024 * c - 1024
        xr = np.roll(xT_full, -shift, axis=1).reshape(16, 128, S // 256, 256)
        xr = np.ascontiguousarray(xr.transpose(2, 1, 0, 3)).reshape(S // 256, 128, 16 * 256)
        rr = np.roll(rope, -shift, axis=0)
        ropeT = np.ascontiguousarray(rr.reshape(64, 128, 128).transpose(1, 0, 2))
        tokpos = shift + np.arange(WIN)
        valid = ((tokpos >= 0) & (tokpos < S)).astype(f32).reshape(24, 128).T
        m = dict(common)
        m.update({"xT": xr, "x_own": np.ascontiguousarray(x[0, 1024 * c:1024 * (c + 1), :]), "ropeT": ropeT,
                  "valid": np.ascontiguousarray(valid)})
        in_maps.append(m)
    if "nc" not in _NC_CACHE:
        _NC_CACHE["nc"] = build_program()
    res = run_bass_kernel_spmd(_NC_CACHE["nc"], in_maps, core_ids=list(range(NCORES)))
    out = np.concatenate([np.asarray(r["out"], f32) for r in res.results], axis=0)
    return out.reshape(1, S, D)
```

```python
import math
from contextlib import ExitStack

import numpy as np
import ml_dtypes
import concourse.bass as bass
import concourse.mybir as mybir
from concourse.bass_utils import run_bass_kernel_spmd

F32 = mybir.dt.float32
BF16 = mybir.dt.bfloat16
U32 = mybir.dt.uint32
AF = mybir.ActivationFunctionType
ALU = mybir.AluOpType
AX = mybir.AxisListType

NCORES = 8
S = 8192
D = 2048
TOK = 1024
WIN = 3072
EPS = 1e-6
SCALE = 128.0 ** -0.5
ARENA = 51800
NEG = -1.0e30


class Fw:
    ENG = ("sp", "gp", "pe", "act", "dve")

    def __init__(self):
        self.ops = []

    def op(self, eng, fn, r=(), w=(), dma=False, key=None, extra=None):
        self.ops.append(dict(eng=eng, fn=fn, r=tuple(r), w=tuple(w), dma=dma, key=key,
                             extra=extra, deps=set(), signal=False))
        return len(self.ops) - 1

    def dma(self, eng, out, in_, r=(), w=(), key=None):
        k = key if key is not None else (w[0] if w else ("dma", len(self.ops)))
        return self.op(eng, lambda e, o=out, i=in_: e.dma_start(out=o, in_=i), r=r, w=w, dma=True, key=k)

    def barrier(self):
        last = {}
        for i, o in enumerate(self.ops):
            last[o["eng"]] = i
            if o["dma"]:
                last[("k", o["key"])] = i
        deps = set(last.values())
        for e in self.ENG:
            self.op(e, None, extra=set(deps))

    def analyze(self):
        lastw, readers = {}, {}
        for i, o in enumerate(self.ops):
            deps = set()
            if o["extra"]:
                deps |= o["extra"]
            for b in o["r"]:
                if b in lastw:
                    deps.add(lastw[b])
            for b in o["w"]:
                if b in lastw:
                    deps.add(lastw[b])
                deps.update(readers.get(b, ()))
            deps.discard(i)
            for b in o["r"]:
                readers.setdefault(b, []).append(i)
            for b in o["w"]:
                lastw[b] = i
                readers[b] = []
            real = set()
            for j in deps:
                p = self.ops[j]
                if p["fn"] is None:
                    continue
                if (not p["dma"]) and p["eng"] == "pe" and o["eng"] == "pe" and not o["dma"]:
                    continue
                real.add(j)
            o["deps"] = real
            for j in real:
                self.ops[j]["signal"] = True
        for o in self.ops:
            if o["dma"]:
                o["signal"] = True
        cnt = {}
        for o in self.ops:
            if o["fn"] is None or not o["signal"]:
                continue
            k = ("k", o["key"]) if o["dma"] else ("e", o["eng"])
            cnt[k] = cnt.get(k, 0) + (16 if o["dma"] else 1)
            o["sig"] = (k, cnt[k])
        self.final = dict(cnt)
        return sorted(cnt.keys(), key=str)

    def emit(self, nc, block, sems):
        engs = {"sp": block.sync, "gp": block.gpsimd, "pe": block.tensor, "act": block.scalar,
                "dve": block.vector}
        for en in self.ENG:
            mine = [o for o in self.ops if o["eng"] == en]
            final = self.final if en == "sp" else None

            def body(e, mine=mine, final=final):
                seen = {}
                for o in mine:
                    for j in sorted(o["deps"]):
                        k, v = self.ops[j]["sig"]
                        if seen.get(k, 0) >= v:
                            continue
                        seen[k] = v
                        e.wait_ge(sems[k], v)
                    if o["fn"] is None:
                        continue
                    ins = o["fn"](e)
                    if o["signal"]:
                        k, v = o["sig"]
                        ins.then_inc(sems[k], 16 if o["dma"] else 1)
                if final is not None:
                    for k, v in sorted(final.items(), key=str):
                        if k[0] == "k" and seen.get(k, 0) < v:
                            e.wait_ge(sems[k], v)

            engs[en](body)


def build_program():
    nc = bass.Bass("TRN2", target_bir_lowering=False)
    fw = Fw()

    def din(name, shape, dt=F32):
        return nc.dram_tensor(name, list(shape), dt, kind="ExternalInput").ap()

    xT = din("xT", [S // 256, 128, 16 * 256])
    x_own = din("x_own", [TOK, D])
    ropeT = din("ropeT", [128, 64, 128])
    valid_d = din("valid", [128, 24])
    maskA = din("maskA", [8, 128, 23 * 128], BF16)
    g1T_d = din("g1T", [128, 16])
    qg_d = din("qg_b", [128, 128])
    kg_d = din("kg_b", [128, 128])
    g2_d = din("g2_b", [128, D])
    gf_d = din("gf_b", [128, D])
    iota_d = din("iota", [128, 128])
    ident_d = din("ident", [128, 128])
    wA = din("wA", [8, D, 384])
    wqB = din("wqB", [D, 1024])
    wkvB = din("wkvB", [D, 512])
    w_out = din("w_out", [D, D])
    wq = din("wq", [D, D])
    subkT_d = din("subkT", [128, 16, 128])
    UT_l = din("UT_l", [128, 128, 16 * 128])
    V_l = din("V_l", [128, 128, D])
    out_d = nc.dram_tensor("out", [TOK, D], F32, kind="ExternalOutput").ap()
    Gd = nc.dram_tensor("Gd", [8, 128, 128, 128], BF16, kind="Internal").ap()
    x1_scr = nc.dram_tensor("x1_scr", [128, 8 * D], F32, kind="Internal").ap()

    arena = nc.alloc_sbuf_tensor("arena", [128, ARENA], F32)
    psA = nc.alloc_psum_tensor("psA", [128, 2048], F32)
    psB = nc.alloc_psum_tensor("psB", [128, 2048], F32)

    def bank(i):
        t = psA if i < 4 else psB
        j = i % 4
        return t[:, j * 512:(j + 1) * 512]

    def bank_bf(i):
        return bank(i).bitcast(BF16)

    class Ar:
        def __init__(self):
            self.p = 0

        def alloc(self, shape, dt=F32):
            n = int(np.prod(shape[1:]))
            slots = n if dt in (F32, U32) else (n + 1) // 2
            slots = (slots + 7) // 8 * 8
            off = self.p
            self.p += slots
            assert self.p <= ARENA, ("arena overflow", self.p)
            ap = arena[:, off:off + slots]
            if dt != F32:
                ap = ap.bitcast(dt)
            ap = ap[:, 0:n]
            if len(shape) == 3:
                ap = ap.rearrange("p (a b) -> p a b", a=shape[1])
            elif len(shape) == 4:
                ap = ap.rearrange("p (a b c) -> p a b c", a=shape[1], b=shape[2])
            return ap

    ar = Ar()

    ones_bf = ar.alloc([128, 128], BF16)
    ident_bf = ar.alloc([128, 128], BF16)
    g1T = ar.alloc([128, 16])
    validT = ar.alloc([128, 24])
    kg_b = ar.alloc([128, 128])
    qg_b = ar.alloc([128, 128])
    iota = ar.alloc([128, 128])
    fw.dma("gp", ident_bf, ident_d, w=["ident"])
    fw.dma("sp", g1T, g1T_d, w=["g1T"])
    fw.dma("sp", validT, valid_d, w=["valid"])
    fw.dma("sp", kg_b, kg_d, w=["kg"])
    fw.dma("sp", qg_b, qg_d, w=["qg"])
    fw.dma("sp", iota, iota_d, w=["iota"])
    fw.op("dve", lambda e: e.memset(ones_bf, 1.0), w=["ones"])
    MIX0 = ar.p
    mixA = ar.alloc([128, 8, TOK], BF16)
    QT_B = ar.alloc([128, 8, TOK], BF16)
    P0 = ar.p

    def rms_feature_major(xst, ntok, slot_id, sq, rt, rstd, dst_fn, psb, rid, wid):
        fw.op("act", lambda e: e.activation(out=sq, in_=xst, func=AF.Square), r=[slot_id], w=["sq"])

        def mm(e):
            ins = None
            for dk in range(16):
                ins = e.matmul(bank(psb)[:, 0:ntok], ones_bf, sq[:, dk, :], start=(dk == 0), stop=(dk == 15))
            return ins
        fw.op("pe", mm, r=["sq", "ones"], w=[("ps", psb)])
        fw.op("act", lambda e: e.activation(out=rt, in_=bank(psb)[:, 0:ntok], func=AF.Sqrt, bias=EPS,
                                            scale=1.0 / D), r=[("ps", psb)], w=["rt"])
        fw.op("dve", lambda e: e.reciprocal(rstd, rt), r=["rt"], w=["rstd"])

        def norm(e):
            ins = None
            for dk in range(16):
                ins = e.scalar_tensor_tensor(out=dst_fn(dk), in0=xst[:, dk, :], scalar=g1T[:, dk:dk + 1],
                                             in1=rstd, op0=ALU.mult, op1=ALU.mult)
            return ins
        fw.op("dve", norm, r=[slot_id, "rstd", "g1T"] + list(rid), w=list(wid))

    def qk_post(raw, nh, gain, gain_id, rope_ap, rope_id, tmp, ssq, rt2, rs2, kn, kr, tA, tB, psb, dst_fn, dst_ids, tag):
        n = nh * 128
        raw3 = raw.rearrange("p (h d) -> p h d", h=nh)
        tmp3 = tmp.rearrange("p (h d) -> p h d", h=nh)
        kn3 = kn.rearrange("p (h d) -> p h d", h=nh)
        fw.op("dve", lambda e: e.tensor_tensor(tmp, raw, raw, ALU.mult), r=[tag + "raw"], w=[tag + "tmp"])
        fw.op("dve", lambda e: e.tensor_reduce(ssq, tmp3, AX.X, ALU.add), r=[tag + "tmp"], w=[tag + "ssq"])
        fw.op("act", lambda e: e.activation(out=rt2, in_=ssq, func=AF.Sqrt, bias=EPS, scale=1.0 / 128),
              r=[tag + "ssq"], w=[tag + "rt2"])
        fw.op("dve", lambda e: e.reciprocal(rs2, rt2), r=[tag + "rt2"], w=[tag + "rs2"])
        fw.op("dve", lambda e: e.tensor_tensor(kn3, raw3, rs2.unsqueeze(2).to_broadcast([128, nh, 128]), ALU.mult),
              r=[tag + "raw", tag + "rs2"], w=[tag + "kn"])
        fw.op("dve", lambda e: e.tensor_tensor(kn3, kn3, gain.unsqueeze(1).to_broadcast([128, nh, 128]), ALU.mult),
              r=[tag + "kn", gain_id], w=[tag + "kn"])
        kn4 = kn.rearrange("p (h i two) -> p h i two", h=nh, two=2)
        kr4 = kr.rearrange("p (h i two) -> p h i two", h=nh, two=2)
        x0, x1 = kn4[:, :, :, 0], kn4[:, :, :, 1]
        cosb = rope_ap[:, 0:64].unsqueeze(1).to_broadcast([128, nh, 64])
        sinb = rope_ap[:, 64:128].unsqueeze(1).to_broadcast([128, nh, 64])
        tA3 = tA.rearrange("p (h i) -> p h i", h=nh)
        tB3 = tB.rearrange("p (h i) -> p h i", h=nh)
        fw.op("dve", lambda e: e.tensor_tensor(tA3, x0, cosb, ALU.mult), r=[tag + "kn", rope_id], w=[tag + "tA"])
        fw.op("dve", lambda e: e.tensor_tensor(tB3, x1, sinb, ALU.mult), r=[tag + "kn", rope_id], w=[tag + "tB"])
        fw.op("dve", lambda e: e.tensor_tensor(kr4[:, :, :, 0], tA3, tB3, ALU.subtract),
              r=[tag + "tA", tag + "tB"], w=[tag + "kr"])
        fw.op("dve", lambda e: e.tensor_tensor(tA3, x0, sinb, ALU.mult), r=[tag + "kn", rope_id], w=[tag + "tA"])
        fw.op("dve", lambda e: e.tensor_tensor(tB3, x1, cosb, ALU.mult), r=[tag + "kn", rope_id], w=[tag + "tB"])
        fw.op("dve", lambda e: e.tensor_tensor(kr4[:, :, :, 1], tA3, tB3, ALU.add),
              r=[tag + "tA", tag + "tB"], w=[tag + "kr"])
        for h0 in range(0, nh, 4):
            hn = min(4, nh - h0)

            def tr(e, h0=h0, hn=hn):
                ins = None
                for j in range(hn):
                    ins = e.transpose(bank_bf(psb)[:, j * 128:(j + 1) * 128], kr[:, (h0 + j) * 128:(h0 + j + 1) * 128],
                                      ident_bf)
                return ins
            fw.op("pe", tr, r=[tag + "kr", "ident"], w=[("ps", psb)])
            for j in range(hn):
                fw.op("act", lambda e, j=j, h0=h0: e.activation(out=dst_fn(h0 + j), in_=bank_bf(psb)[:, j * 128:(j + 1) * 128],
                                                                func=AF.Copy),
                      r=[("ps", psb)], w=[dst_ids[h0 + j]])

    def attention(KT_fn, V_fn, QT, kts, kid, vid, qid, mask_fn, mask_id, E2, P2, rec, dst, dst_id, pso, psd, tagc,
                  sbanks=(2, 3), hook=None):
        n = len(kts)
        nb = len(sbanks)
        L = nb - 1

        def qk(i):
            kt = kts[i]
            b = sbanks[i % nb]
            fw.op("pe", lambda e, kt=kt, b=b: e.matmul(bank(b), KT_fn(kt), QT, start=True, stop=True),
                  r=[kid, qid], w=[("ps", b)])

        for i in range(min(L, n)):
            qk(i)
        for i in range(n):
            kt = kts[i]
            b = sbanks[i % nb]
            if i + L < n:
                qk(i + L)
            Eb = E2[i % nb]
            fw.op("act", lambda e, b=b, Eb=Eb: e.activation(out=Eb, in_=bank(b), func=AF.Exp, scale=SCALE),
                  r=[("ps", b)], w=[("E", i % nb)])
            if mask_fn is not None:
                Pb = P2[i % nb]
                mk = mask_fn(i)
                fw.op("dve", lambda e, Pb=Pb, Eb=Eb, mk=mk, kt=kt: e.scalar_tensor_tensor(
                    out=Pb, in0=Eb, scalar=validT[:, kt:kt + 1], in1=mk, op0=ALU.mult, op1=ALU.mult),
                    r=[("E", i % nb), mask_id, "valid"], w=[("P", i % nb)])
                pid = ("P", i % nb)
            else:
                Pb = Eb
                pid = ("E", i % nb)

            def pv(e, kt=kt, Pb=Pb, i=i):
                e.matmul(bank(pso), V_fn(kt), Pb, start=(i == 0), stop=(i == n - 1))
                return e.matmul(bank(psd), ones_bf, Pb, start=(i == 0), stop=(i == n - 1))
            fw.op("pe", pv, r=[pid, vid, "ones"], w=[("ps", pso), ("ps", psd)])
            if hook is not None:
                hook(i)
        fw.op("dve", lambda e: e.reciprocal(rec, bank(psd)), r=[("ps", psd)], w=["rec" + tagc])
        fw.op("dve", lambda e: e.tensor_tensor(dst, bank(pso), rec, ALU.mult),
              r=[("ps", pso), "rec" + tagc], w=[dst_id])

    ar.p = P0
    hwin = ar.alloc([128, 16, WIN], BF16)
    PA = ar.p
    xst = [ar.alloc([128, 16, 256]) for _ in range(2)]
    sq = ar.alloc([128, 16, 256], BF16)
    rt = ar.alloc([128, 256])
    rstd = ar.alloc([128, 256])
    for ck in range(WIN // 256):
        s = ck % 2
        fw.dma("sp", xst[s].rearrange("p a b -> p (a b)"), xT[ck], w=[("xst", s)])
        rms_feature_major(xst[s], 256, ("xst", s), sq, rt, rstd,
                          lambda dk, ck=ck: hwin[:, dk, ck * 256:(ck + 1) * 256], 0, [], [("hwin", ck // 2)])
    fw.barrier()

    ar.p = PA
    WqB = ar.alloc([128, 16, 1024], BF16)
    ropeQ = ar.alloc([128, 8, 128])
    rawq = ar.alloc([128, 1024])
    tmpq = ar.alloc([128, 1024])
    knq = ar.alloc([128, 1024])
    krq = ar.alloc([128, 1024], BF16)
    tAq = ar.alloc([128, 512])
    tBq = ar.alloc([128, 512])
    ssq8 = ar.alloc([128, 8])
    rt8 = ar.alloc([128, 8])
    rs8 = ar.alloc([128, 8])
    fw.dma("gp", WqB, wqB.rearrange("(dk p) c -> p dk c", p=128), w=["WqB"])
    fw.dma("sp", ropeQ, ropeT[:, 8:16, :], w=["ropeQ"])
    win_ids = [("hwin", i) for i in range(6)]
    for tt in range(8):
        for half in range(2):
            def mm(e, tt=tt, half=half):
                ins = None
                for dk in range(16):
                    ins = e.matmul(bank(half), hwin[:, dk, 1024 + tt * 128:1024 + (tt + 1) * 128],
                                   WqB[:, dk, half * 512:(half + 1) * 512], start=(dk == 0), stop=(dk == 15))
                return ins
            fw.op("pe", mm, r=["WqB"] + win_ids, w=[("ps", half)])
            fw.op("act", lambda e, half=half: e.activation(out=rawq[:, half * 512:(half + 1) * 512], in_=bank(half),
                                                           func=AF.Copy), r=[("ps", half)], w=["qraw"])
        qk_post(rawq, 8, qg_b, "qg", ropeQ[:, tt, :], "ropeQ", tmpq, ssq8, rt8, rs8, knq, krq, tAq, tBq, 6,
                lambda h, tt=tt: QT_B[:, h, tt * 128:(tt + 1) * 128], [("QTB", h) for h in range(8)], "q")
    fw.barrier()

    ar.p = PA
    WA = [ar.alloc([128, 16, 384], BF16) for _ in range(2)]
    maskS = [ar.alloc([128, 23, 128], BF16) for _ in range(2)]
    KT_h = [ar.alloc([128, WIN], BF16) for _ in range(2)]
    V_h = [ar.alloc([128, 24, 128], BF16) for _ in range(2)]
    QT_h = [ar.alloc([128, TOK], BF16) for _ in range(2)]
    E2 = [ar.alloc([128, 512], BF16) for _ in range(2)]
    P2 = [ar.alloc([128, 512], BF16) for _ in range(2)]
    rec = ar.alloc([128, 512])

    def a2_loads(h):
        s = h % 2
        fw.dma("gp", WA[s], wA[h].rearrange("(dk p) c -> p dk c", p=128), w=[("WA", s)])
        fw.dma("sp", maskS[s].rearrange("p a b -> p (a b)"), maskA[h], w=[("mask", s)])

    def a2_proj_groups(h):
        s = h % 2
        pieces = []
        gcount = [0]

        def add_group(nmm, mk_mm, evac):
            per = 4 if nmm == 16 else 16
            pb = gcount[0] % 2
            gcount[0] += 1
            for p0 in range(0, nmm, per):
                last = (p0 + per >= nmm)

                def piece(p0=p0, pb=pb):
                    def mm(e):
                        ins = None
                        for q in range(p0, p0 + per):
                            ins = mk_mm(e, q, pb)
                        return ins
                    fw.op("pe", mm, r=[("WA", s)] + win_ids, w=[("ps", pb)])
                pieces.append((piece, (lambda pb=pb: evac(pb)) if last else None))

        for c6 in range(6):
            add_group(16,
                      lambda e, dk, pb, c6=c6: e.matmul(bank(pb), WA[s][:, dk, 128:256], hwin[:, dk, c6 * 512:(c6 + 1) * 512],
                                                        start=(dk == 0), stop=(dk == 15)),
                      lambda pb, c6=c6: fw.op("act", lambda e: e.activation(out=KT_h[s][:, c6 * 512:(c6 + 1) * 512], in_=bank(pb),
                                                                             func=AF.Copy), r=[("ps", pb)], w=[("KTh", s)]))
        for c2 in range(2):
            add_group(16,
                      lambda e, dk, pb, c2=c2: e.matmul(bank(pb), WA[s][:, dk, 0:128],
                                                        hwin[:, dk, 1024 + c2 * 512:1024 + (c2 + 1) * 512],
                                                        start=(dk == 0), stop=(dk == 15)),
                      lambda pb, c2=c2: fw.op("act", lambda e: e.activation(out=QT_h[s][:, c2 * 512:(c2 + 1) * 512], in_=bank(pb),
                                                                             func=AF.Copy), r=[("ps", pb)], w=[("QTh", s)]))
        for gg in range(6):
            add_group(64,
                      lambda e, q, pb, gg=gg: e.matmul(bank(pb)[:, (q // 16) * 128:(q // 16 + 1) * 128],
                                                       hwin[:, q % 16, (4 * gg + q // 16) * 128:(4 * gg + q // 16 + 1) * 128],
                                                       WA[s][:, q % 16, 256:384], start=(q % 16 == 0), stop=(q % 16 == 15)),
                      lambda pb, gg=gg: fw.op("dve", lambda e: e.tensor_copy(
                          V_h[s][:, 4 * gg:4 * gg + 4, :].rearrange("p a b -> p (a b)"), bank(pb)),
                          r=[("ps", pb)], w=[("Vh", s)]))
        return pieces

    a2_loads(0)
    for (pc, ev) in a2_proj_groups(0):
        pc()
        if ev is not None:
            ev()
    for h in range(8):
        s = h % 2
        if h + 1 < 8:
            a2_loads(h + 1)
            pend = a2_proj_groups(h + 1)
        else:
            pend = []
        cnt_t = [0]
        npend = len(pend)
        late = []

        def hook(i, pend=pend, cnt_t=cnt_t, npend=npend, late=late):
            cnt_t[0] += 1
            for (d, ev) in [x for x in late if x[0] <= cnt_t[0]]:
                ev()
            late[:] = [x for x in late if x[0] > cnt_t[0]]
            want = (npend * cnt_t[0] + 39) // 40
            while pend and (npend - len(pend)) < want:
                pc, ev = pend.pop(0)
                pc()
                if ev is not None:
                    late.append((cnt_t[0] + 2, ev))
        for qc in range(2):
            kts = list(range(4 * qc, 4 * qc + 20))
            qt0 = 8 + 4 * qc
            pso, psd = (4, 5) if qc == 0 else (6, 7)

            def mask_fn(i, kts=kts, qt0=qt0, s=s):
                m0 = 11 - (kts[i] - qt0)
                return maskS[s][:, m0:m0 + 4, :].rearrange("p a b -> p (a b)")
            attention(lambda kt, s=s: KT_h[s][:, kt * 128:(kt + 1) * 128], lambda kt, s=s: V_h[s][:, kt, :],
                      QT_h[s][:, qc * 512:(qc + 1) * 512], kts, ("KTh", s), ("Vh", s), ("QTh", s), mask_fn, ("mask", s),
                      E2, P2, rec, mixA[:, h, qc * 512:(qc + 1) * 512], ("mixT", h), pso, psd, "A",
                      sbanks=(2, 3), hook=hook)
        while pend:
            pc, ev = pend.pop(0)
            pc()
            if ev is not None:
                late.append((0, ev))
        for (d, ev) in late:
            ev()
        late[:] = []
    fw.barrier()

    ar.p = P0
    KT_B = ar.alloc([128, 2, S], BF16)
    V_B = ar.alloc([128, 64, 256], BF16)
    PB = ar.p
    Wst = ar.alloc([128, 16, 512])
    ar.p = PB
    xst = [ar.alloc([128, 16, 256]) for _ in range(2)]
    sq = ar.alloc([128, 16, 256], BF16)
    xb = [ar.alloc([128, 16, 256], BF16) for _ in range(2)]
    Wkv = ar.alloc([128, 16, 512], BF16)
    rope4 = ar.alloc([128, 4, 128])
    raw = [ar.alloc([128, 1024]) for _ in range(2)]
    kgt = ar.alloc([128, 1024])
    sqt = ar.alloc([128, 1024])
    tA = ar.alloc([128, 512])
    tB = ar.alloc([128, 512])
    ob = ar.alloc([128, 1024])
    kr = ar.alloc([128, 1024], BF16)
    rtk = [ar.alloc([128, 2]) for _ in range(2)]
    rsk = [ar.alloc([128, 2]) for _ in range(2)]
    ssq8 = ar.alloc([128, 8])
    rt8 = ar.alloc([128, 8])
    rs8 = ar.alloc([128, 8])
    tC = sqt[:, 0:512]
    tD = sqt[:, 512:1024]
    fw.dma("sp", Wst, wkvB.rearrange("(dk p) c -> p dk c", p=128), w=["Wst"])

    def foldw(e):
        ins = None
        for dk in range(16):
            ins = e.tensor_scalar(Wkv[:, dk, :], Wst[:, dk, :], g1T[:, dk:dk + 1], None, ALU.mult)
        return ins
    fw.op("dve", foldw, r=["Wst", "g1T"], w=["Wkv"])
    SSQB = (0, 6)
    sched = []
    step = [0]

    def run_due():
        due = [f for (d, f) in sched if d <= step[0]]
        rest = [(d, f) for (d, f) in sched if d > step[0]]
        sched[:] = rest
        for f in due:
            f()

    def b1_head(C, half):
        ck = 2 * C + half
        s = ck % 2
        fw.dma("sp", xst[s].rearrange("p a b -> p (a b)"), xT[ck], w=[("xst", s)] + (["Wst"] if ck < 2 else []))
        fw.op("act", lambda e, s=s: e.activation(out=sq, in_=xst[s], func=AF.Square), r=[("xst", s)], w=["sq"])
        fw.op("dve", lambda e, s=s: e.tensor_copy(xb[s], xst[s]), r=[("xst", s)], w=[("xb", s)])

        def mms(e, half=half):
            ins = None
            for sub in range(2):
                for dk in range(16):
                    ins = e.matmul(bank(SSQB[half])[:, sub:sub + 1], sq[:, dk, sub * 128:(sub + 1) * 128],
                                   ones_bf[:, 0:1], start=(dk == 0), stop=(dk == 15))
            return ins
        fw.op("pe", mms, r=["sq", "ones"], w=[("ps", SSQB[half])])
        for sub in range(2):
            bb = 1 + 2 * half + sub

            def mm(e, s=s, sub=sub, bb=bb):
                ins = None
                for dk in range(16):
                    ins = e.matmul(bank(bb), xb[s][:, dk, sub * 128:(sub + 1) * 128], Wkv[:, dk, :],
                                   start=(dk == 0), stop=(dk == 15))
                return ins
            fw.op("pe", mm, r=[("xb", s), "Wkv"], w=[("ps", bb)])

    def b1_tail(C, half):
        r_ = C % 2
        fw.op("act", lambda e, half=half: e.activation(out=rtk[half], in_=bank(SSQB[half])[:, 0:2], func=AF.Sqrt,
                                                       bias=EPS, scale=1.0 / D), r=[("ps", SSQB[half])], w=[("rtk", half)])
        fw.op("dve", lambda e, half=half: e.reciprocal(rsk[half], rtk[half]), r=[("rtk", half)], w=[("rsk", half)])
        for sub in range(2):
            j = 2 * half + sub
            bb = 1 + j
            fw.op("act", lambda e, bb=bb, C=C, j=j, half=half, sub=sub: e.activation(
                out=V_B[:, 4 * C + j, :], in_=bank(bb)[:, 256:512], func=AF.Copy, scale=rsk[half][:, sub:sub + 1]),
                r=[("ps", bb), ("rsk", half)], w=[("VB", C, j)])
            fw.op("act", lambda e, bb=bb, j=j, half=half, sub=sub, r_=r_: e.activation(
                out=raw[r_][:, j * 256:(j + 1) * 256], in_=bank(bb)[:, 0:256], func=AF.Copy,
                scale=rsk[half][:, sub:sub + 1]), r=[("ps", bb), ("rsk", half)], w=[("raw", r_, j)])

    def b1_postA(C):
        r_ = C % 2
        fw.dma("sp", rope4, ropeT[:, 4 * C:4 * C + 4, :], w=["rope4"])
        rw = raw[r_]
        raw_ids = [("raw", r_, j) for j in range(4)]
        rw3 = rw.rearrange("p (a d) -> p a d", a=8)
        kg3 = kgt.rearrange("p (a d) -> p a d", a=8)
        sq3 = sqt.rearrange("p (a d) -> p a d", a=8)
        kg5 = kgt.rearrange("p (j h i two) -> p j h i two", j=4, h=2, two=2)
        ob5 = ob.rearrange("p (j h i two) -> p j h i two", j=4, h=2, two=2)
        ob3 = ob.rearrange("p (a d) -> p a d", a=8)
        kr3 = kr.rearrange("p (a d) -> p a d", a=8)
        x0, x1 = kg5[:, :, :, :, 0], kg5[:, :, :, :, 1]
        cosb = rope4[:, :, 0:64].unsqueeze(2).to_broadcast([128, 4, 2, 64])
        sinb = rope4[:, :, 64:128].unsqueeze(2).to_broadcast([128, 4, 2, 64])
        v4 = lambda t: t.rearrange("p (j h i) -> p j h i", j=4, h=2)
        fw.op("dve", lambda e: e.tensor_tensor(kg3, rw3, kg_b.unsqueeze(1).to_broadcast([128, 8, 128]), ALU.mult),
              r=raw_ids + ["kg"], w=["kgt"])
        fw.op("dve", lambda e: e.tensor_tensor(sqt, rw, rw, ALU.mult), r=raw_ids, w=["sqt", "sqt2"])
        fw.op("dve", lambda e: e.tensor_tensor(v4(tA), x0, cosb, ALU.mult), r=["kgt", "rope4"], w=["tA"])
        fw.op("dve", lambda e: e.tensor_reduce(ssq8, sq3, AX.X, ALU.add), r=["sqt", "sqt2"], w=["ssq8"])
        fw.op("dve", lambda e: e.tensor_tensor(v4(tB), x1, sinb, ALU.mult), r=["kgt", "rope4"], w=["tB"])
        fw.op("dve", lambda e: e.tensor_tensor(v4(tC), x0, sinb, ALU.mult), r=["kgt", "rope4"], w=["sqt"])
        fw.op("dve", lambda e: e.tensor_tensor(v4(tD), x1, cosb, ALU.mult), r=["kgt", "rope4"], w=["sqt2"])
        fw.op("dve", lambda e: e.tensor_tensor(ob5[:, :, :, :, 0], v4(tA), v4(tB), ALU.subtract), r=["tA", "tB"], w=["ob0"])
        fw.op("dve", lambda e: e.tensor_tensor(ob5[:, :, :, :, 1], v4(tC), v4(tD), ALU.add), r=["sqt", "sqt2"], w=["ob1"])

    def b1_postB(C):
        ob3 = ob.rearrange("p (a d) -> p a d", a=8)
        kr3 = kr.rearrange("p (a d) -> p a d", a=8)
        fw.op("act", lambda e: e.activation(out=rt8, in_=ssq8, func=AF.Sqrt, bias=EPS, scale=1.0 / 128), r=["ssq8"], w=["rt8"])
        fw.op("dve", lambda e: e.reciprocal(rs8, rt8), r=["rt8"], w=["rs8"])
        fw.op("dve", lambda e: e.tensor_tensor(kr3, ob3, rs8.unsqueeze(2).to_broadcast([128, 8, 128]), ALU.mult),
              r=["ob0", "ob1", "rs8"], w=["kr"])
        def trk(e):
            ins = None
            for j in range(4):
                for hh in range(2):
                    ins = e.transpose(bank_bf(5)[:, (hh * 4 + j) * 128:(hh * 4 + j + 1) * 128],
                                      kr[:, (j * 2 + hh) * 128:(j * 2 + hh + 1) * 128], ident_bf)
            return ins
        fw.op("pe", trk, r=["kr", "ident"], w=[("ps", 5)])
        for hh in range(2):
            fw.op("dve", lambda e, hh=hh, C=C: e.tensor_copy(KT_B[:, hh, C * 512:(C + 1) * 512],
                                                             bank_bf(5)[:, hh * 512:(hh + 1) * 512]),
                  r=[("ps", 5)], w=[("KTB", C, hh)])

    for C in range(S // 512):
        for half in range(2):
            b1_head(C, half)
            run_due()
            sched.append((step[0] + 1, lambda C=C, half=half: b1_tail(C, half)))
            if half == 1:
                sched.append((step[0] + 1, lambda C=C: b1_postA(C)))
                sched.append((step[0] + 2, lambda C=C: b1_postB(C)))
            step[0] += 1
    step[0] += 10
    run_due()
    fw.barrier()

    ar.p = PB
    mixB = ar.alloc([128, 8, TOK], BF16)
    E2 = [ar.alloc([128, 512], BF16) for _ in range(4)]
    rec = ar.alloc([128, 512])
    Wo = [ar.alloc([128, 16, 512], BF16) for _ in range(2)]
    w_out3 = w_out.rearrange("(m p) c -> p m c", p=128)
    for oc_ in range(2):
        fw.dma("gp", Wo[oc_], w_out3[:, :, oc_ * 512:(oc_ + 1) * 512], w=[("Wo", oc_)])
    cnt = 0
    for h in range(8):
        kv = h // 4
        for qc in range(2):
            pso, psd = (4, 5) if cnt % 2 == 0 else (6, 7)
            cnt += 1
            attention(lambda kt, kv=kv: KT_B[:, kv, kt * 128:(kt + 1) * 128],
                      lambda kt, kv=kv: V_B[:, kt, kv * 128:(kv + 1) * 128],
                      QT_B[:, h, qc * 512:(qc + 1) * 512], list(range(64)), "KTB", "VB", ("QTB", h), None, None,
                      E2, None, rec, mixB[:, h, qc * 512:(qc + 1) * 512], ("mixT", 8 + h), pso, psd, "B", sbanks=(0, 1, 2, 3))
    fw.barrier()

    ar.p = P0
    acc = ar.alloc([128, 8, D])
    assert ar.p <= PB
    PO = PB + 4096
    ar.p = PO
    for tt in range(8):
        fw.dma("sp", acc[:, tt, :], x_own[tt * 128:(tt + 1) * 128, :], w=[("acc", tt)], key=("accld", tt))
    mix_ids = [("mixT", i) for i in range(16)]
    k = 0
    for oc in range(4):
        s = oc % 2
        if oc >= 2:
            fw.dma("gp", Wo[s], w_out3[:, :, oc * 512:(oc + 1) * 512], w=[("Wo", s)])
        for tt in range(8):
            b = k % 4
            k += 1

            def mm(e, tt=tt, s=s, b=b):
                ins = None
                for m in range(16):
                    ins = e.matmul(bank(b), (mixA[:, m, tt * 128:(tt + 1) * 128] if m < 8 else mixB[:, m - 8, tt * 128:(tt + 1) * 128]), Wo[s][:, m, :],
                                   start=(m == 0), stop=(m == 15))
                return ins
            fw.op("pe", mm, r=[("Wo", s)] + mix_ids, w=[("ps", b)])
            fw.op("dve", lambda e, tt=tt, oc=oc, b=b: e.tensor_tensor(
                acc[:, tt, oc * 512:(oc + 1) * 512], bank(b), acc[:, tt, oc * 512:(oc + 1) * 512], ALU.add),
                r=[("ps", b), ("acc", tt)], w=[("acc", tt)])
    fw.barrier()

    ar.p = MIX0
    h2T = ar.alloc([128, 16, TOK], BF16)
    PH = ar.p
    ar.p = PO
    g2_b = ar.alloc([128, D])
    junk = ar.alloc([128, D], BF16)
    h2 = [ar.alloc([128, D], BF16) for _ in range(2)]
    ssqn = ar.alloc([128, 8])
    rtn = ar.alloc([128, 8])
    rsn = ar.alloc([128, 8])
    fw.dma("sp", g2_b, g2_d, w=["g2"])
    acc_ids = [("acc", t_) for t_ in range(8)]
    fw.dma("sp", x1_scr, acc.rearrange("p a b -> p (a b)"), r=acc_ids, w=["x1scr"])
    fw.op("dve", lambda e: e.memset(ssqn, 0.0), w=["ssqn"])
    for tt in range(8):
        fw.op("act", lambda e, tt=tt: e.activation(out=junk, in_=acc[:, tt, :], func=AF.Square,
                                                   accum_out=ssqn[:, tt:tt + 1]), r=[("acc", tt), "ssqn"], w=["junk", ("ssqn", tt)])
        fw.op("act", lambda e, tt=tt: e.activation(out=rtn[:, tt:tt + 1], in_=ssqn[:, tt:tt + 1], func=AF.Sqrt,
                                                   bias=EPS, scale=1.0 / D), r=[("ssqn", tt)], w=[("rtn", tt)])
    for tt in range(8):
        fw.op("dve", lambda e, tt=tt: e.reciprocal(rsn[:, tt:tt + 1], rtn[:, tt:tt + 1]), r=[("rtn", tt)], w=[("rsn", tt)])
        s = tt % 2
        fw.op("dve", lambda e, tt=tt, s=s: e.scalar_tensor_tensor(out=h2[s], in0=acc[:, tt, :], scalar=rsn[:, tt:tt + 1],
                                                                  in1=g2_b, op0=ALU.mult, op1=ALU.mult),
              r=[("acc", tt), ("rsn", tt), "g2"], w=[("h2", s)])
        for g in range(4):
            b = g % 2

            def tr(e, g=g, s=s, b=b):
                ins = None
                for j in range(4):
                    dk = 4 * g + j
                    ins = e.transpose(bank_bf(b)[:, j * 128:(j + 1) * 128], h2[s][:, dk * 128:(dk + 1) * 128], ident_bf)
                return ins
            fw.op("pe", tr, r=[("h2", s), "ident"], w=[("ps", b)])
            fw.op("act", lambda e, g=g, tt=tt, b=b: e.activation(
                out=h2T[:, 4 * g:4 * g + 4, tt * 128:(tt + 1) * 128],
                in_=bank_bf(b)[:, 0:512].rearrange("p (a t) -> p a t", a=4), func=AF.Copy),
                r=[("ps", b)], w=["h2T"])
    fw.barrier()

    ar.p = PH
    qT = ar.alloc([128, 16, TOK], BF16)
    subkT = ar.alloc([128, 16, 128], BF16)
    PQ = ar.p
    Wq = [ar.alloc([128, 16, 512], BF16) for _ in range(2)]
    fw.dma("gp", subkT, subkT_d, w=["subk"])
    wq3 = wq.rearrange("(dk p) c -> p dk c", p=128)
    k = 0
    for piece in range(4):
        s = piece % 2
        fw.dma("gp", Wq[s], wq3[:, :, piece * 512:(piece + 1) * 512], w=[("Wq", s)])
        for bb in range(4):
            blk = piece * 4 + bb
            for half in range(2):
                b = k % 4
                k += 1

                def mm(e, bb=bb, half=half, s=s, b=b):
                    ins = None
                    for dk in range(16):
                        ins = e.matmul(bank(b), Wq[s][:, dk, bb * 128:(bb + 1) * 128], h2T[:, dk, half * 512:(half + 1) * 512],
                                       start=(dk == 0), stop=(dk == 15))
                    return ins
                fw.op("pe", mm, r=[("Wq", s), "h2T"], w=[("ps", b)])
                fw.op("act", lambda e, blk=blk, half=half, b=b: e.activation(
                    out=qT[:, blk, half * 512:(half + 1) * 512], in_=bank(b), func=AF.Copy), r=[("ps", b)], w=["qT"])
    fw.barrier()

    ar.p = PQ
    bufX = ar.alloc([128, 2048])
    bufY = ar.alloc([128, 2048])
    top = ar.alloc([128, 256])
    idx = ar.alloc([128, 256], U32)
    idxf = ar.alloc([128, 256])
    best = ar.alloc([128, 128])
    pos = ar.alloc([128, 128], U32)
    pa = ar.alloc([128, 128], U32)
    pb = ar.alloc([128, 128], U32)
    paf = ar.alloc([128, 128])
    pbf = ar.alloc([128, 128])
    bm = ar.alloc([128, 128])
    ex = ar.alloc([128, 128])
    Zs = ar.alloc([128, 8])
    rZ = ar.alloc([128, 8])
    R3 = ar.alloc([128, 3, 128], BF16)
    RT = ar.alloc([128, 3, 128])
    NT = 16
    A1t = [ar.alloc([128, NT, 128]) for _ in range(2)]
    A1 = [ar.alloc([128, NT, 128], BF16) for _ in range(2)]
    A2 = [ar.alloc([128, NT, 128], BF16) for _ in range(2)]
    Gs = [ar.alloc([128, 128, 128], BF16) for _ in range(2)]
    XS = [("X", i) for i in range(16)]
    YS = [("Y", i) for i in range(16)]
    sc3 = bufX.rearrange("p (s n) -> p s n", s=16)
    wk3 = bufY.rearrange("p (s n) -> p s n", s=16)
    top3 = top.rearrange("p (s k) -> p s k", s=16)
    idx3 = idx.rearrange("p (s k) -> p s k", s=16)
    top4 = top.rearrange("p (h c k) -> p h c k", h=8, c=2)
    idxf4 = idxf.rearrange("p (h c k) -> p h c k", h=8, c=2)
    cand3 = bufX.rearrange("p (h n) -> p h n", h=8)
    cand4 = bufX.rearrange("p (h a b) -> p h a b", h=8, a=16)
    cw3 = bufY.rearrange("p (h n) -> p h n", h=8)
    best3 = best.rearrange("p (h k) -> p h k", h=8)
    pos3 = pos.rearrange("p (h k) -> p h k", h=8)
    bm3 = bm.rearrange("p (h k) -> p h k", h=8)
    ex3 = ex.rearrange("p (h k) -> p h k", h=8)
    selY = bufY.rearrange("p (h k a) -> p h k a", h=8, k=16)
    selX = bufX.rearrange("p (h k a) -> p h k a", h=8, k=16)
    iota16b = iota[:, 0:16].unsqueeze(1).unsqueeze(1).to_broadcast([128, 8, 16, 16])
    all_top = [("top", sg, j) for sg in range(16) for j in range(2)]
    all_idx = [("idx", sg, j) for sg in range(16) for j in range(2)]
    all_best = [("best", hh, j) for hh in range(8) for j in range(2)]
    all_pos = [("pos", hh, j) for hh in range(8) for j in range(2)]

    def topk_stages(tt):
        def st0():
            def mm(e):
                ins = None
                for blk in range(16):
                    ins = e.matmul(psA[:, blk * 128:(blk + 1) * 128], qT[:, blk, tt * 128:(tt + 1) * 128], subkT[:, blk, :],
                                   start=True, stop=True)
                return ins
            fw.op("pe", mm, r=["qT", "subk"], w=[("ps", 0), ("ps", 1), ("ps", 2), ("ps", 3)])
            fw.op("act", lambda e: e.activation(out=bufX, in_=psA[:, :], func=AF.Copy),
                  r=[("ps", 0), ("ps", 1), ("ps", 2), ("ps", 3)], w=XS)
            for sg in range(16):
                fw.op("dve", lambda e, sg=sg: e.max(out=top3[:, sg, 0:8], in_=sc3[:, sg, :]), r=[("X", sg)], w=[("top", sg, 0)])

        def st1():
            for sg in range(16):
                fw.op("dve", lambda e, sg=sg: e.max_index(out=idx3[:, sg, 0:8], in_max=top3[:, sg, 0:8], in_values=sc3[:, sg, :]),
                      r=[("X", sg), ("top", sg, 0)], w=[("idx", sg, 0)])
                fw.op("dve", lambda e, sg=sg: e.match_replace(out=wk3[:, sg, :], in_to_replace=top3[:, sg, 0:8],
                                                              in_values=sc3[:, sg, :], imm_value=NEG),
                      r=[("X", sg), ("top", sg, 0)], w=[("Y", sg)])

        def st2():
            for sg in range(16):
                fw.op("dve", lambda e, sg=sg: e.max(out=top3[:, sg, 8:16], in_=wk3[:, sg, :]), r=[("Y", sg)], w=[("top", sg, 1)])

        def st3():
            for sg in range(16):
                fw.op("dve", lambda e, sg=sg: e.max_index(out=idx3[:, sg, 8:16], in_max=top3[:, sg, 8:16], in_values=wk3[:, sg, :]),
                      r=[("Y", sg), ("top", sg, 1)], w=[("idx", sg, 1)])
            fw.op("dve", lambda e: e.tensor_copy(idxf, idx), r=all_idx, w=["idxf"])

        def st4():
            fw.op("dve", lambda e: e.tensor_tensor(cand4, top4[:, :, 0, :].unsqueeze(3).to_broadcast([128, 8, 16, 16]),
                                                   top4[:, :, 1, :].unsqueeze(2).to_broadcast([128, 8, 16, 16]), ALU.add),
                  r=all_top, w=XS)
            for hh in range(8):
                fw.op("dve", lambda e, hh=hh: e.max(out=best3[:, hh, 0:8], in_=cand3[:, hh, :]),
                      r=[("X", 2 * hh), ("X", 2 * hh + 1)], w=[("best", hh, 0)])

        def st5():
            for hh in range(8):
                fw.op("dve", lambda e, hh=hh: e.max_index(out=pos3[:, hh, 0:8], in_max=best3[:, hh, 0:8], in_values=cand3[:, hh, :]),
                      r=[("X", 2 * hh), ("X", 2 * hh + 1), ("best", hh, 0)], w=[("pos", hh, 0)])
                fw.op("dve", lambda e, hh=hh: e.match_replace(out=cw3[:, hh, :], in_to_replace=best3[:, hh, 0:8],
                                                              in_values=cand3[:, hh, :], imm_value=NEG),
                      r=[("X", 2 * hh), ("X", 2 * hh + 1), ("best", hh, 0)], w=[("Y", 2 * hh), ("Y", 2 * hh + 1)])
            for hh in range(8):
                fw.op("dve", lambda e, hh=hh: e.max(out=best3[:, hh, 8:16], in_=cw3[:, hh, :]),
                      r=[("Y", 2 * hh), ("Y", 2 * hh + 1)], w=[("best", hh, 1)])

        def st6():
            for hh in range(8):
                fw.op("dve", lambda e, hh=hh: e.max_index(out=pos3[:, hh, 8:16], in_max=best3[:, hh, 8:16], in_values=cw3[:, hh, :]),
                      r=[("Y", 2 * hh), ("Y", 2 * hh + 1), ("best", hh, 1)], w=[("pos", hh, 1)])
            fw.op("dve", lambda e: e.tensor_tensor(bm3, best3, best3[:, :, 0:1].to_broadcast([128, 8, 16]), ALU.subtract),
                  r=all_best, w=["bm"])
            fw.op("act", lambda e: e.activation(out=ex, in_=bm, func=AF.Exp), r=["bm"], w=["ex"])
            fw.op("dve", lambda e: e.tensor_single_scalar(pa, pos, 4, ALU.logical_shift_right), r=all_pos, w=["pa"])
            fw.op("dve", lambda e: e.tensor_single_scalar(pb, pos, 15, ALU.bitwise_and), r=all_pos, w=["pb"])
            fw.op("dve", lambda e: e.tensor_copy(paf, pa), r=["pa"], w=["paf"])
            fw.op("dve", lambda e: e.tensor_copy(pbf, pb), r=["pb"], w=["pbf"])

        def st7():
            paf3 = paf.rearrange("p (h k) -> p h k", h=8)
            pbf3 = pbf.rearrange("p (h k) -> p h k", h=8)
            fw.op("dve", lambda e: e.tensor_tensor(selY, iota16b, paf3.unsqueeze(3).to_broadcast([128, 8, 16, 16]), ALU.is_equal),
                  r=["paf", "iota"], w=YS)
            fw.op("gp", lambda e: e.tensor_tensor(selY, selY, idxf4[:, :, 0, :].unsqueeze(2).to_broadcast([128, 8, 16, 16]), ALU.mult),
                  r=YS + ["idxf"], w=YS)
            fw.op("dve", lambda e: e.tensor_tensor(selX, iota16b, pbf3.unsqueeze(3).to_broadcast([128, 8, 16, 16]), ALU.is_equal),
                  r=["pbf", "iota"], w=XS)
            fw.op("gp", lambda e: e.tensor_tensor(selX, selX, idxf4[:, :, 1, :].unsqueeze(2).to_broadcast([128, 8, 16, 16]), ALU.mult),
                  r=XS + ["idxf"], w=XS)
            fw.op("dve", lambda e: e.tensor_reduce(Zs, ex3, AX.X, ALU.add), r=["ex"], w=["Zs"])
            fw.op("dve", lambda e: e.reciprocal(rZ, Zs), r=["Zs"], w=["rZ"])
            fw.op("dve", lambda e: e.tensor_tensor(R3[:, 2, :].rearrange("p (h k) -> p h k", h=8), ex3,
                                                   rZ.unsqueeze(2).to_broadcast([128, 8, 16]), ALU.mult),
                  r=["ex", "rZ"], w=["R3g"])
            fw.op("dve", lambda e: e.tensor_reduce(R3[:, 0, :].rearrange("p (h k) -> p h k", h=8), selY, AX.X, ALU.add),
                  r=YS, w=[("R3", 0)])
            fw.op("dve", lambda e: e.tensor_reduce(R3[:, 1, :].rearrange("p (h k) -> p h k", h=8), selX, AX.X, ALU.add),
                  r=XS, w=[("R3", 1)])
        return [st0, st1, st2, st3, st4, st5, st6, st7]

    def onehot_prelude(tt):
        def tr(e):
            ins = None
            for j in range(3):
                ins = e.transpose(bank_bf(4)[:, j * 128:(j + 1) * 128], R3[:, j, :], ident_bf)
            return ins
        fw.op("pe", tr, r=[("R3", 0), ("R3", 1), "R3g", "ident"], w=[("ps", 4)])
        fw.op("act", lambda e: e.activation(out=RT.rearrange("p a b -> p (a b)"), in_=bank_bf(4)[:, 0:384], func=AF.Copy),
              r=[("ps", 4)], w=["RT"])

    iotab = iota.unsqueeze(1).to_broadcast([128, NT, 128])

    def onehot_group(tt, g):
        gs = Gs[tt % 2]
        s = g % 2
        t0 = g * NT
        fw.op("dve", lambda e: e.tensor_tensor(A2[s], iotab, RT[:, 1, t0:t0 + NT].unsqueeze(2).to_broadcast([128, NT, 128]),
                                               ALU.is_equal), r=["RT", "iota"], w=[("A2", s)])
        fw.op("dve", lambda e: e.tensor_tensor(A1t[s], iotab, RT[:, 0, t0:t0 + NT].unsqueeze(2).to_broadcast([128, NT, 128]),
                                               ALU.is_equal), r=["RT", "iota"], w=[("A1t", s)])
        def gate(e):
            ins = None
            for j in range(NT):
                ins = e.activation(out=A1[s][:, j, :], in_=A1t[s][:, j, :], func=AF.Copy, scale=RT[:, 2, t0 + j:t0 + j + 1])
            return ins
        if g % 3 == 2:
            fw.op("gp", lambda e: e.tensor_tensor(A1[s], A1t[s], RT[:, 2, t0:t0 + NT].unsqueeze(2).to_broadcast([128, NT, 128]),
                                                  ALU.mult), r=["RT", ("A1t", s)], w=[("A1", s)])
        else:
            fw.op("act", gate, r=["RT", ("A1t", s)], w=[("A1", s)])
        for q4 in range(NT // 4):
            b = 5 + (q4 % 2)

            def gm(e, q4=q4, b=b):
                ins = None
                for j in range(4):
                    ins = e.matmul(bank(b)[:, j * 128:(j + 1) * 128], A1[s][:, 4 * q4 + j, :], A2[s][:, 4 * q4 + j, :],
                                   start=True, stop=True)
                return ins
            fw.op("pe", gm, r=[("A1", s), ("A2", s)], w=[("ps", b)])
            tq = t0 + 4 * q4
            fw.op("act", lambda e, b=b, tq=tq: e.activation(
                out=gs[:, :, tq:tq + 4], in_=bank(b).rearrange("p (t i) -> p i t", t=4), func=AF.Copy),
                r=[("ps", b)], w=[("Gs", tt % 2, tq)])

    for st in topk_stages(0):
        st()
    for tt in range(8):
        onehot_prelude(tt)
        nxt = topk_stages(tt + 1) if tt + 1 < 8 else []
        for g in range(128 // NT):
            if g < len(nxt):
                nxt[g]()
            onehot_group(tt, g)
        fw.dma("sp", Gd[tt].rearrange("p a b -> p (a b)"), Gs[tt % 2].rearrange("p a b -> p (a b)"),
               r=[("Gs", tt % 2, tq_) for tq_ in range(0, 128, 4)], w=[("Gd", tt)], key=("Gdk", tt % 2))
    fw.barrier()

    ar.p = PO
    GRP = 4
    UT = [ar.alloc([128, 16, 128], BF16) for _ in range(4)]
    Vc = [[ar.alloc([128, D], BF16) for _ in range(GRP)] for _ in range(2)]
    Gc = [ar.alloc([128, TOK], BF16) for _ in range(4)]
    AT = [ar.alloc([128, GRP, TOK], BF16) for _ in range(2)]
    ge = [ar.alloc([128, 512], BF16) for _ in range(2)]
    all_gd = [("Gd", t) for t in range(8)]
    kU = 0
    kV = 0
    for grp in range(128 // GRP):
        gsl = grp % 2
        for j in range(GRP):
            c = grp * GRP + j
            s3 = c % 4
            fw.dma("gp", UT[s3].rearrange("p a b -> p (a b)"), UT_l[c], w=[("UT", s3)])
            fw.dma("gp", Vc[gsl][j], V_l[c], w=[("Vc", gsl, j)])
            fw.dma("sp", Gc[s3].rearrange("p (a t) -> p a t", a=8), Gd[:, :, c, :].rearrange("a p t -> p a t"),
                   r=all_gd, w=[("Gc", s3)])
            for half in range(2):
                b = kU % 2
                kU += 1

                def mm(e, s3=s3, half=half, b=b):
                    ins = None
                    for dk in range(16):
                        ins = e.matmul(bank(b), UT[s3][:, dk, :], h2T[:, dk, half * 512:(half + 1) * 512],
                                       start=(dk == 0), stop=(dk == 15))
                    return ins
                fw.op("pe", mm, r=[("UT", s3), "h2T"], w=[("ps", b)])
                fw.op("act", lambda e, b=b: e.activation(out=ge[b], in_=bank(b), func=AF.Gelu_apprx_tanh), r=[("ps", b)], w=[("ge", b)])
                fw.op("dve", lambda e, b=b, gsl=gsl, j=j, half=half, s3=s3: e.tensor_tensor(
                    AT[gsl][:, j, half * 512:(half + 1) * 512], ge[b], Gc[s3][:, half * 512:(half + 1) * 512], ALU.mult),
                    r=[("ge", b), ("Gc", s3)], w=[("AT", gsl, j)])
        if grp == 0:
            fw.dma("sp", acc.rearrange("p a b -> p (a b)"), x1_scr, r=["x1scr"],
                   w=["acc"] + [("acc", t_, o_) for t_ in range(8) for o_ in range(4)], key="accld")
        for tt in range(8):
            for oc in range(4):
                b = 4 + (kV % 4)
                kV += 1

                def vm(e, gsl=gsl, tt=tt, oc=oc, b=b):
                    ins = None
                    for j in range(GRP):
                        ins = e.matmul(bank(b), AT[gsl][:, j, tt * 128:(tt + 1) * 128], Vc[gsl][j][:, oc * 512:(oc + 1) * 512],
                                       start=(j == 0), stop=(j == GRP - 1))
                    return ins
                fw.op("pe", vm, r=[("AT", gsl, j) for j in range(GRP)] + [("Vc", gsl, j) for j in range(GRP)], w=[("ps", b)])
                fw.op("dve", lambda e, tt=tt, oc=oc, b=b: e.tensor_tensor(
                    acc[:, tt, oc * 512:(oc + 1) * 512], bank(b), acc[:, tt, oc * 512:(oc + 1) * 512], ALU.add),
                    r=[("ps", b), ("acc", tt, oc)], w=[("acc", tt, oc)], extra=None)
    fw.barrier()

    ar.p = PO
    gf_b = ar.alloc([128, D])
    junk = ar.alloc([128, D], BF16)
    ost = [ar.alloc([128, D]) for _ in range(2)]
    ssqf = ar.alloc([128, 8])
    rtf = ar.alloc([128, 8])
    rsf = ar.alloc([128, 8])
    fw.dma("sp", gf_b, gf_d, w=["gf"])
    fw.op("dve", lambda e: e.memset(ssqf, 0.0), w=["ssqf"])
    for tt in range(8):
        s = tt % 2
        fw.op("act", lambda e, tt=tt: e.activation(out=junk, in_=acc[:, tt, :], func=AF.Square,
                                                   accum_out=ssqf[:, tt:tt + 1]), r=["ssqf"], w=["junkf", ("ssqf", tt)])
        fw.op("act", lambda e, tt=tt: e.activation(out=rtf[:, tt:tt + 1], in_=ssqf[:, tt:tt + 1], func=AF.Sqrt,
                                                   bias=EPS, scale=1.0 / D), r=[("ssqf", tt)], w=[("rtf", tt)])
        fw.op("dve", lambda e, tt=tt: e.reciprocal(rsf[:, tt:tt + 1], rtf[:, tt:tt + 1]), r=[("rtf", tt)], w=[("rsf", tt)])
        fw.op("dve", lambda e, tt=tt, s=s: e.scalar_tensor_tensor(out=ost[s], in0=acc[:, tt, :], scalar=rsf[:, tt:tt + 1],
                                                                  in1=gf_b, op0=ALU.mult, op1=ALU.mult),
              r=[("rsf", tt), "gf"], w=[("ost", s)])
        fw.dma("sp", out_d[tt * 128:(tt + 1) * 128, :], ost[s], r=[("ost", s)], w=[("out", tt)], key=("out", s))

    keys = fw.analyze()
    with ExitStack() as es:
        sems = {}
        for i, k in enumerate(keys):
            sems[k] = es.enter_context(nc.semaphore("s%d" % i))
        block = es.enter_context(nc.Block())
        with nc.allow_low_precision(reason="bf16 matmul operands, exact small ints"):
            fw.emit(nc, block, sems)
    return nc


def _constants():
    half = 64
    inv = (10000.0 ** (-np.arange(0, half, 2, dtype=np.float32) / half)).astype(np.float32)
    row = np.repeat(np.arange(S // 64, dtype=np.float32), 64)
    col = np.tile(np.arange(64, dtype=np.float32), S // 64)
    ang = np.concatenate([row[:, None] * inv, col[:, None] * inv], axis=-1).astype(np.float32)
    rope = np.concatenate([np.cos(ang), np.sin(ang)], axis=-1).astype(np.float32)
    slopes = 2.0 ** (-8.0 * (np.arange(8, dtype=np.float64) + 1.0) / 8)
    kk = np.arange(128)[:, None, None]
    mm_ = np.arange(23)[None, :, None]
    qq = np.arange(128)[None, None, :]
    delta = (11 - mm_) * 128 + kk - qq
    ad = np.abs(delta)
    cnt = (ad <= 64).astype(np.float64) + ((ad <= 256) & (delta % 4 == 0)) + ((ad <= 1024) & (delta % 16 == 0))
    mask = np.stack([cnt * np.exp(-slopes[h] * ad) for h in range(8)], axis=0)
    mask = mask.reshape(8, 128, 23 * 128).astype(np.float32).astype(ml_dtypes.bfloat16)
    iota = np.tile(np.arange(128, dtype=np.float32)[None, :], (128, 1))
    ident = np.eye(128, dtype=np.float32)
    return rope, mask, iota, ident


_NC_CACHE = {}


def kernel(x, norm1_g, w_in, q_norm_g, k_norm_g, w_out, norm2_g, peer_w_query, peer_sub_keys,
           peer_u, peer_v, final_norm_g):
    f32 = np.float32
    x = np.asarray(x, f32)
    w_in0 = np.asarray(w_in, f32)[0]
    rope, mask, iota, ident = _constants()
    xT_full = np.ascontiguousarray(x[0].T)
    wA = np.ascontiguousarray(np.stack([np.concatenate(
        [w_in0[:, h * 128:(h + 1) * 128], w_in0[:, 1024 + h * 128:1024 + (h + 1) * 128],
         w_in0[:, 2048 + h * 128:2048 + (h + 1) * 128]], axis=1) for h in range(8)], axis=0))
    wqB = np.ascontiguousarray(w_in0[:, 3072:4096])
    wkvB = np.ascontiguousarray(w_in0[:, 4096:4608])
    U = np.asarray(peer_u, f32)[0]
    V = np.asarray(peer_v, f32)[0]
    UT_l = np.ascontiguousarray(U.reshape(128, 128, 16, 128).transpose(1, 3, 2, 0)).reshape(128, 128, 16 * 128)
    V_l = np.ascontiguousarray(V.reshape(128, 128, D).transpose(1, 0, 2))
    subkT = np.ascontiguousarray(np.asarray(peer_sub_keys, f32)[0].reshape(16, 128, 128).transpose(2, 0, 1))
    common = {
        "maskA": mask, "g1T": np.ascontiguousarray(np.asarray(norm1_g, f32)[0].reshape(16, 128).T),
        "qg_b": np.ascontiguousarray(np.tile(np.asarray(q_norm_g, f32)[0][None, :], (128, 1))),
        "kg_b": np.ascontiguousarray(np.tile(np.asarray(k_norm_g, f32)[0][None, :], (128, 1))),
        "g2_b": np.ascontiguousarray(np.tile(np.asarray(norm2_g, f32)[0][None, :], (128, 1))),
        "gf_b": np.ascontiguousarray(np.tile(np.asarray(final_norm_g, f32)[None, :], (128, 1))),
        "iota": iota, "ident": ident, "wA": wA, "wqB": wqB, "wkvB": wkvB,
        "w_out": np.ascontiguousarray(np.asarray(w_out, f32)[0]),
        "wq": np.ascontiguousarray(np.asarray(peer_w_query, f32)[0]),
        "subkT": subkT, "UT_l": UT_l, "V_l": V_l,
    }
    in_maps = []
    for c in range(NCORES):
        shift = 1024 * c - 1024
        xr = np.roll(xT_full, -shift, axis=1).reshape(16, 128, S // 256, 256)
        xr = np.ascontiguousarray(xr.transpose(2, 1, 0, 3)).reshape(S // 256, 128, 16 * 256)
        rr = np.roll(rope, -shift, axis=0)
        ropeT = np.ascontiguousarray(rr.reshape(64, 128, 128).transpose(1, 0, 2))
        tokpos = shift + np.arange(WIN)
        valid = ((tokpos >= 0) & (tokpos < S)).astype(f32).reshape(24, 128).T
        m = dict(common)
        m.update({"xT": xr, "x_own": np.ascontiguousarray(x[0, 1024 * c:1024 * (c + 1), :]), "ropeT": ropeT,
                  "valid": np.ascontiguousarray(valid)})
        in_maps.append(m)
    if "nc" not in _NC_CACHE:
        _NC_CACHE["nc"] = build_program()
    res = run_bass_kernel_spmd(_NC_CACHE["nc"], in_maps, core_ids=list(range(NCORES)))
    out = np.concatenate([np.asarray(r["out"], f32) for r in res.results], axis=0)
    return out.reshape(1, S, D)
```
